# Optimizing a Trainium2 kernel written in Bass

```python
import jax, jax.numpy as jnp
from jax import lax
import numpy as np

D_MODEL = 1024
BATCH = 1
SEQ = 16384
DEPTH = 1

D_MIX = D_MODEL
SB_HEADS = 8
SB_HEAD_DIM = 64
D_SB = SB_HEADS * SB_HEAD_DIM
D_RG = D_MIX - D_SB
RG_BLOCKS = 8
RG_BLOCK_DIM = D_RG // RG_BLOCKS
CONV_WIDTH = 4
RG_C = 8.0
D_IN_PROJ = 3 * D_SB + 2 * D_RG
Q_BLOCK = 128
PEER_HEADS = 8
PEER_N_KEYS = 128
PEER_N_EXPERTS = PEER_N_KEYS * PEER_N_KEYS
PEER_D_KEY = 256
PEER_HALF = PEER_D_KEY // 2
PEER_TOPK = 16
TOKEN_CHUNK = 128
EPS = 1e-6

kernel_name = "hymba_stickbreak_rglru_peer"


def rms_norm(x, g):
    xf = x.astype(jnp.float32)
    y = xf * lax.rsqrt(jnp.mean(xf * xf, axis=-1, keepdims=True) + EPS)
    return (y * g.astype(jnp.float32)).astype(x.dtype)


def stick_breaking_attention(q, k, v):
    B, H, S, Dh = q.shape
    nb = S // Q_BLOCK
    scale = Dh ** -0.5
    kpos = jnp.arange(S)

    def block(i):
        q_blk = lax.dynamic_slice_in_dim(q, i * Q_BLOCK, Q_BLOCK, axis=2)
        qpos = i * Q_BLOCK + jnp.arange(Q_BLOCK)
        causal = kpos[None, :] < qpos[:, None]
        z = jnp.einsum('bhqd,bhkd->bhqk', q_blk, k).astype(jnp.float32) * scale
        log_beta = jax.nn.log_sigmoid(z)
        log_one_minus = jnp.where(causal, jax.nn.log_sigmoid(-z), 0.0)
        suffix = lax.cumsum(log_one_minus, axis=3, reverse=True) - log_one_minus
        w = jnp.where(causal, jnp.exp(log_beta + suffix), 0.0)
        return jnp.einsum('bhqk,bhkd->bhqd', w.astype(v.dtype), v)

    out = lax.map(block, jnp.arange(nb))
    return out.transpose(1, 2, 0, 3, 4).reshape(B, H, S, Dh)


def causal_depthwise_conv(x, w, bias):
    S = x.shape[1]
    xp = jnp.pad(x, ((0, 0), (CONV_WIDTH - 1, 0), (0, 0)))
    y = xp[:, 0:S, :] * w[0]
    for j in range(1, CONV_WIDTH):
        y = y + xp[:, j:j + S, :] * w[j]
    return y + bias


def rg_lru(x, w_a, b_a, w_x, b_x, lam):
    B, S, C = x.shape
    xb = x.reshape(B, S, RG_BLOCKS, RG_BLOCK_DIM)
    r = jax.nn.sigmoid(jnp.einsum('bsni,nij->bsnj', xb, w_a).reshape(B, S, C) + b_a)
    i_gate = jax.nn.sigmoid(jnp.einsum('bsni,nij->bsnj', xb, w_x).reshape(B, S, C) + b_x)
    log_a = -RG_C * r.astype(jnp.float32) * jax.nn.softplus(-lam.astype(jnp.float32))
    a = jnp.exp(log_a)
    b = jnp.sqrt(-jnp.expm1(2.0 * log_a)) * (i_gate * x).astype(jnp.float32)

    def step(h, ab):
        a_t, b_t = ab
        h = a_t * h + b_t
        return h, h

    h0 = jnp.zeros((B, C), jnp.float32)
    _, hs = lax.scan(step, h0, (a.transpose(1, 0, 2), b.transpose(1, 0, 2)))
    return hs.transpose(1, 0, 2).astype(x.dtype)


def peer_ffn(x, w_query, sub_keys, expert_u, expert_v):
    B, S, D = x.shape
    T = B * S
    xt = x.reshape(T // TOKEN_CHUNK, TOKEN_CHUNK, D)

    def chunk(xc):
        q = jnp.einsum('td,dhk->thk', xc, w_query)
        q = q.reshape(TOKEN_CHUNK, PEER_HEADS, 2, PEER_HALF)
        s = jnp.einsum('thpk,hpnk->thpn', q, sub_keys).astype(jnp.float32)
        top_s, top_i = lax.top_k(s, PEER_TOPK)
        cand_s = top_s[:, :, 0, :, None] + top_s[:, :, 1, None, :]
        cand_i = top_i[:, :, 0, :, None] * PEER_N_KEYS + top_i[:, :, 1, None, :]
        cand_s = cand_s.reshape(TOKEN_CHUNK, PEER_HEADS, PEER_TOPK * PEER_TOPK)
        cand_i = cand_i.reshape(TOKEN_CHUNK, PEER_HEADS, PEER_TOPK * PEER_TOPK)
        best_s, best_pos = lax.top_k(cand_s, PEER_TOPK)
        idx = jnp.take_along_axis(cand_i, best_pos, axis=-1)
        g = jax.nn.softmax(best_s, axis=-1)
        u = jnp.take(expert_u, idx, axis=0)
        v = jnp.take(expert_v, idx, axis=0)
        act = jax.nn.gelu(jnp.einsum('td,thkd->thk', xc, u))
        return jnp.einsum('thk,thkd->td', (g * act).astype(v.dtype), v)

    y = lax.map(chunk, xt)
    return y.reshape(B, S, D)


def setup_inputs(seed: int = 0) -> dict:
    key = jax.random.key(seed)
    ks = jax.random.split(key, 24)
    f32 = jnp.float32

    def nrm(k, shape, scale):
        return jax.random.normal(k, shape, f32) * scale

    def gain(k, shape):
        return 1.0 + 0.02 * jax.random.normal(k, shape, f32)

    u_a = jax.random.uniform(ks[11], (DEPTH, D_RG), f32, 0.9, 0.999)
    a_base = u_a ** (1.0 / RG_C)
    rg_lambda = jnp.log(a_base) - jnp.log1p(-a_base)
    return {
        "x": jax.random.normal(ks[0], (BATCH, SEQ, D_MODEL), f32),
        "norm_mix": gain(ks[1], (DEPTH, D_MODEL)),
        "w_in": nrm(ks[2], (DEPTH, D_MODEL, D_IN_PROJ), D_MODEL ** -0.5),
        "q_norm": gain(ks[3], (DEPTH, SB_HEAD_DIM)),
        "k_norm": gain(ks[4], (DEPTH, SB_HEAD_DIM)),
        "conv_w": nrm(ks[5], (DEPTH, CONV_WIDTH, D_RG), CONV_WIDTH ** -0.5),
        "conv_b": nrm(ks[6], (DEPTH, D_RG), 0.01),
        "rg_w_a": nrm(ks[7], (DEPTH, RG_BLOCKS, RG_BLOCK_DIM, RG_BLOCK_DIM), RG_BLOCK_DIM ** -0.5),
        "rg_b_a": nrm(ks[8], (DEPTH, D_RG), 0.01),
        "rg_w_x": nrm(ks[9], (DEPTH, RG_BLOCKS, RG_BLOCK_DIM, RG_BLOCK_DIM), RG_BLOCK_DIM ** -0.5),
        "rg_b_x": nrm(ks[10], (DEPTH, D_RG), 0.01),
        "rg_lambda": rg_lambda,
        "out_norm_sb": gain(ks[12], (DEPTH, D_SB)),
        "out_norm_rg": gain(ks[13], (DEPTH, D_RG)),
        "w_out": nrm(ks[14], (DEPTH, D_MIX, D_MODEL), D_MIX ** -0.5),
        "norm_ffn": gain(ks[15], (DEPTH, D_MODEL)),
        "peer_w_query": nrm(ks[16], (DEPTH, D_MODEL, PEER_HEADS, PEER_D_KEY), D_MODEL ** -0.5),
        "peer_sub_keys": nrm(ks[17], (DEPTH, PEER_HEADS, 2, PEER_N_KEYS, PEER_HALF), PEER_HALF ** -0.5),
        "peer_u": nrm(ks[18], (DEPTH, PEER_N_EXPERTS, D_MODEL), D_MODEL ** -0.5),
        "peer_v": nrm(ks[19], (DEPTH, PEER_N_EXPERTS, D_MODEL), PEER_HEADS ** -0.5),
    }


def reference(x, norm_mix, w_in, q_norm, k_norm, conv_w, conv_b, rg_w_a, rg_b_a,
              rg_w_x, rg_b_x, rg_lambda, out_norm_sb, out_norm_rg, w_out, norm_ffn,
              peer_w_query, peer_sub_keys, peer_u, peer_v):
    B, S, _ = x.shape
    for l in range(DEPTH):
        h = rms_norm(x, norm_mix[l])
        proj = jnp.einsum('bsd,de->bse', h, w_in[l])
        q = proj[:, :, 0:D_SB]
        k = proj[:, :, D_SB:2 * D_SB]
        v = proj[:, :, 2 * D_SB:3 * D_SB]
        x_rg = proj[:, :, 3 * D_SB:3 * D_SB + D_RG]
        g_rg = proj[:, :, 3 * D_SB + D_RG:D_IN_PROJ]

        def to_heads(t):
            return t.reshape(B, S, SB_HEADS, SB_HEAD_DIM).transpose(0, 2, 1, 3)

        qh = rms_norm(to_heads(q), q_norm[l])
        kh = rms_norm(to_heads(k), k_norm[l])
        o_sb = stick_breaking_attention(qh, kh, to_heads(v))
        o_sb = o_sb.transpose(0, 2, 1, 3).reshape(B, S, D_SB)

        x_rg = causal_depthwise_conv(x_rg, conv_w[l], conv_b[l])
        o_rg = rg_lru(x_rg, rg_w_a[l], rg_b_a[l], rg_w_x[l], rg_b_x[l], rg_lambda[l])
        o_rg = o_rg * jax.nn.gelu(g_rg)

        mixed = jnp.concatenate(
            [rms_norm(o_sb, out_norm_sb[l]), rms_norm(o_rg, out_norm_rg[l])], axis=-1)
        x = x + jnp.einsum('bse,ed->bsd', mixed, w_out[l])

        h = rms_norm(x, norm_ffn[l])
        x = x + peer_ffn(h, peer_w_query[l], peer_sub_keys[l], peer_u[l], peer_v[l])
    return x
```

```python
import numpy as np
from contextlib import ExitStack
import concourse.bass as bass
import concourse.mybir as mybir
from concourse.bass_utils import run_bass_kernel_spmd

F32 = mybir.dt.float32
BF16 = mybir.dt.bfloat16
U32 = mybir.dt.uint32
I32 = mybir.dt.int32
AF = mybir.ActivationFunctionType
ALU = mybir.AluOpType
AX = mybir.AxisListType

S_LEN = 16384
D = 1024
NB = 128
NSLOT = 16
EPS = 1e-6
GC = 0.7978845608028654


SAME_ENGINE_WAIT = True
NO_SELF_WAIT = set()


class Res:
    __slots__ = ("w", "r")

    def __init__(self):
        self.w = None
        self.r = {}


class Sch:
    CHUNK = 30000
    NDMA = 24

    def __init__(self, nc, stack):
        self.nc = nc
        self.stack = stack
        self.eng = {"pe": nc.tensor, "act": nc.scalar, "dve": nc.vector,
                    "pool": nc.gpsimd, "sp": nc.sync}
        self.sems = {}
        self.cnt = {k: 0 for k in self.eng}
        self.waited = {}
        self.dma_pool = {k: [] for k in self.eng}
        self.dma_rr = {k: 0 for k in self.eng}
        self.nsem = 0

    def _sem(self, key):
        s = self.sems.get(key)
        if s is None:
            s = self.stack.enter_context(self.nc.semaphore("s%d" % self.nsem))
            self.nsem += 1
            self.sems[key] = s
        return s

    def _wait(self, eng, deps):
        e = self.eng[eng]
        for key, val in deps.items():
            if key[0] == eng and (eng == "pe" or eng in NO_SELF_WAIT):
                continue
            k = (eng, key)
            if self.waited.get(k, 0) >= val:
                continue
            self.waited[k] = val
            e.wait_ge(self._sem(key), val)

    @staticmethod
    def _deps(reads, writes):
        deps = {}

        def add(k, v):
            if deps.get(k, 0) < v:
                deps[k] = v
        for r in reads:
            if r.w is not None:
                add(*r.w)
        for w in writes:
            if w.w is not None:
                add(*w.w)
            for k, v in w.r.items():
                add(k, v)
        return deps

    @staticmethod
    def _commit(tok, reads, writes):
        k, v = tok
        for r in reads:
            if r.r.get(k, 0) < v:
                r.r[k] = v
        for w in writes:
            w.w = tok
            w.r = {}

    def op(self, eng, fn, reads=(), writes=()):
        deps = self._deps(reads, writes)
        self._wait(eng, deps)
        ins = fn(self.eng[eng])
        n = self.cnt[eng]
        self.cnt[eng] = n + 1
        key = (eng, n // self.CHUNK)
        val = n % self.CHUNK + 1
        ins.then_inc(self._sem(key), 1)
        self._commit((key, val), reads, writes)
        return ins

    def dma(self, eng, fn, reads=(), writes=()):
        deps = self._deps(reads, writes)
        pool = self.dma_pool[eng]
        i = self.dma_rr[eng] % self.NDMA
        self.dma_rr[eng] += 1
        if i >= len(pool):
            pool.append([("dma", eng, len(pool)), 0])
            i = len(pool) - 1
        key, val = pool[i]
        if val > 0 and deps.get(key, 0) < val:
            deps[key] = val
        self._wait(eng, deps)
        ins = fn(self.eng[eng])
        val += 16
        pool[i][1] = val
        ins.then_inc(self._sem(key), 16)
        self._commit((key, val), reads, writes)
        return ins

    def barrier(self):
        deps = {}
        for e, pool in self.dma_pool.items():
            for key, val in pool:
                if val:
                    deps[key] = val
        for e, n in self.cnt.items():
            if n:
                deps[(e, (n - 1) // self.CHUNK)] = (n - 1) % self.CHUNK + 1
        for eng in self.eng:
            e = self.eng[eng]
            for key, val in deps.items():
                k = (eng, key)
                if self.waited.get(k, 0) >= val:
                    continue
                self.waited[k] = val
                e.wait_ge(self._sem(key), val)

    def finish(self, eng="sp"):
        deps = {}
        for e, pool in self.dma_pool.items():
            for key, val in pool:
                if val:
                    deps[key] = val
        for e, n in self.cnt.items():
            if n:
                deps[(e, (n - 1) // self.CHUNK)] = (n - 1) % self.CHUNK + 1
        e = self.eng[eng]
        for key, val in deps.items():
            e.wait_ge(self._sem(key), val)


class Buf:
    __slots__ = ("t", "r")

    def __init__(self, t):
        self.t = t
        self.r = Res()


class Rot:
    def __init__(self, bufs):
        self.bufs = bufs
        self.i = 0

    def next(self):
        b = self.bufs[self.i % len(self.bufs)]
        self.i += 1
        return b


def build(debug=None):
    nc = bass.Bass("TRN2", target_bir_lowering=False)

    def din(name, shape, dt=F32):
        return nc.dram_tensor(name, list(shape), dt, kind="ExternalInput").ap()

    x_all = din("x_all", [S_LEN, D])
    x_own = din("x_own", [2048, D])
    sel_d = din("sel", [128, NB])
    masks_d = din("masks", [128, 2 * 8 * 128])
    w_in = din("w_in", [D, 2560])
    nmix_d = din("nmix", [128, 8])
    qk_d = din("qk", [1, 128])
    rgp_d = din("rgp", [128, 4 * 12])
    rgwa_d = din("rg_w_a", [8, 64, 64])
    rgwx_d = din("rg_w_x", [8, 64, 64])
    gsb_d = din("gsb", [128, 4])
    w_out = din("w_out", [D, D])
    nffn_d = din("nffn", [1, D])
    wq_d = din("wq", [D, 2048])
    sk_d = din("sk", [16, 128, 128])
    puv_d = din("peer_uv", [16384, 2 * D])
    y_own = nc.dram_tensor("y_own", [2048, D], F32, kind="ExternalOutput").ap()
    KT_d = nc.dram_tensor("KT_scr", [4, 128, S_LEN], BF16, kind="Internal").ap()
    V_d = nc.dram_tensor("V_scr", [S_LEN, 512], BF16, kind="Internal").ap()
    UVb_d = nc.dram_tensor("UVb_scr", [16384, 2 * D], BF16, kind="Internal").ap()
    rKT = Res()
    rV = Res()
    dbg = {}
    if debug:
        for nm, shp in debug.items():
            if nm in ("nsb", "nheads", "nslots", "stop"):
                continue
            dbg[nm] = nc.dram_tensor(nm, list(shp), F32, kind="ExternalOutput").ap()

    with ExitStack() as top:
        S = Sch(nc, top)
        cnt = [0]

        def sbuf(st, shape, dt):
            cnt[0] += 1
            return Buf(st.enter_context(nc.sbuf_tensor("t%d" % cnt[0], list(shape), dt)))

        def psum(st, shape, dt):
            cnt[0] += 1
            return Buf(st.enter_context(nc.psum_tensor("p%d" % cnt[0], list(shape), dt)))

        dmaq = ["sp", "act"]
        dq = [0]

        def ld(out, in_, reads, writes, q=None):
            if q is None:
                q = dmaq[dq[0] % 2]
                dq[0] += 1
            S.dma(q, lambda e: e.dma_start(out=out, in_=in_), reads, writes)

        ident_f = sbuf(top, [128, 128], F32)
        ident = sbuf(top, [128, 128], BF16)
        ones_b = sbuf(top, [128, 128], BF16)
        zeros_b = sbuf(top, [128, 512], BF16)
        ssrg = sbuf(top, [128, NSLOT], F32)
        sssb = sbuf(top, [128, NSLOT], F32)
        mixrg = sbuf(top, [128, 4, 2048], BF16)
        mixsb = sbuf(top, [128, 4, 2048], BF16)
        pab = top.enter_context(ExitStack())
        QT = sbuf(pab, [128, 4, 2048], BF16)

        S.op("pool", lambda e: e.memset(ident_f.t[:], 0.0), [], [ident_f.r])
        S.op("pool", lambda e: e.affine_select(out=ident_f.t[:], in_=ident_f.t[:], pattern=[[-1, 128]],
                                                compare_op=ALU.not_equal, fill=1.0, base=0,
                                                channel_multiplier=1), [ident_f.r], [ident_f.r])
        S.op("dve", lambda e: e.tensor_copy(out=ident.t[:], in_=ident_f.t[:]), [ident_f.r], [ident.r])
        S.op("pool", lambda e: e.memset(ones_b.t[:], 1.0), [], [ones_b.r])
        S.op("pool", lambda e: e.memset(zeros_b.t[:], 0.0), [], [zeros_b.r])
        S.op("pool", lambda e: e.memset(sssb.t[:], 0.0), [], [sssb.r])

        pbT = [psum(top, [128, 1024], BF16) for _ in range(2)]
        pbF = [psum(top, [128, 512], F32) for _ in range(6)]

        def rstd_from(ms_ap, out_ap, res_in, res_out, n, scale, tmp):
            S.op("dve", lambda e: e.tensor_scalar(out=tmp.t[:, 0:n], in0=ms_ap, scalar1=scale, scalar2=EPS,
                                                   op0=ALU.mult, op1=ALU.add), [res_in], [tmp.r])
            S.op("act", lambda e: e.activation(out=tmp.t[:, 0:n], in_=tmp.t[:, 0:n], func=AF.Ln), [tmp.r], [tmp.r])
            S.op("act", lambda e: e.activation(out=out_ap, in_=tmp.t[:, 0:n], func=AF.Exp, scale=-0.5),
                 [tmp.r], [res_out])

        with ExitStack() as pa:
            hso = sbuf(pa, [128, 4, 2048], F32)
            nmix = sbuf(pa, [128, 8], F32)
            ld(nmix.t[:], nmix_d, [], [nmix.r])
            w_in_v = w_in.rearrange("(dc p) e -> p dc e", p=128)

            def load_w(dst_list):
                with ExitStack() as ws:
                    stg = Rot([sbuf(ws, [128, 8, 512], F32) for _ in range(1)])
                    load_w_inner(stg, dst_list)
                S.barrier()

            def load_w_inner(stg, dst_list):
                for ci, (dst, dcol, scol) in enumerate(dst_list):
                    sg = stg.next()
                    ld(sg.t[:], w_in_v[:, :, scol:scol + 512], [], [sg.r])
                    for dc in range(8):
                        eng = "dve"
                        S.op(eng, lambda e: e.tensor_scalar(out=dst.t[:, dc, dcol:dcol + 512], in0=sg.t[:, dc, :],
                                                            scalar1=nmix.t[:, dc:dc + 1], scalar2=None,
                                                            op0=ALU.mult), [sg.r, nmix.r], [dst.r])
            qk = sbuf(pa, [128, 128], F32)
            ld(qk.t[:], qk_d.to_broadcast([128, 128]), [], [qk.r])
            gqk1 = sbuf(pa, [128, 64], F32)
            S.op("dve", lambda e: e.scalar_tensor_tensor(out=gqk1.t[:], in0=qk.t[:, 0:64], scalar=0.125,
                                                         in1=qk.t[:, 64:128], op0=ALU.mult, op1=ALU.mult),
                 [qk.r], [gqk1.r])
            gqk = sbuf(pa, [128, 8, 64], F32)
            S.op("dve", lambda e: e.tensor_copy(out=gqk.t[:], in_=gqk1.t[:].unsqueeze(1).to_broadcast([128, 8, 64])),
                 [gqk1.r], [gqk.r])
            rgp = sbuf(pa, [128, 4, 12], F32)
            ld(rgp.t[:].rearrange("p a b -> p (a b)"), rgp_d, [], [rgp.r])
            rgc = sbuf(pa, [128, 4, 4], F32)
            tmpc = sbuf(pa, [128, 4], F32)
            S.op("act", lambda e: e.activation(out=tmpc.t[:], in_=rgp.t[:, :, 7], func=AF.Exp, scale=-1.0),
                 [rgp.r], [tmpc.r])
            S.op("act", lambda e: e.activation(out=tmpc.t[:], in_=tmpc.t[:], func=AF.Ln, bias=1.0),
                 [tmpc.r], [tmpc.r])
            S.op("dve", lambda e: e.tensor_scalar(out=rgc.t[:, :, 0], in0=tmpc.t[:], scalar1=-8.0, scalar2=None,
                                                  op0=ALU.mult), [tmpc.r], [rgc.r])
            S.op("dve", lambda e: e.tensor_scalar(out=rgc.t[:, :, 1], in0=tmpc.t[:], scalar1=-16.0, scalar2=None,
                                                  op0=ALU.mult), [tmpc.r], [rgc.r])
            S.op("dve", lambda e: e.tensor_scalar(out=rgc.t[:, :, 2], in0=rgp.t[:, :, 5], scalar1=-1.0, scalar2=None,
                                                  op0=ALU.mult), [rgp.r], [rgc.r])
            S.op("dve", lambda e: e.tensor_scalar(out=rgc.t[:, :, 3], in0=rgp.t[:, :, 6], scalar1=-1.0, scalar2=None,
                                                  op0=ALU.mult), [rgp.r], [rgc.r])
            WaBD = sbuf(pa, [128, 4, 128], BF16)
            WxBD = sbuf(pa, [128, 4, 128], BF16)
            with ExitStack() as ws:
                for (dst, src) in ((WaBD, rgwa_d), (WxBD, rgwx_d)):
                    sg = sbuf(ws, [128, 4, 128], F32)
                    S.op("pool", lambda e: e.memset(sg.t[:], 0.0), [], [sg.r])
                    for ct in range(4):
                        ld(sg.t[0:64, ct, 0:64], src[2 * ct], [], [sg.r])
                        ld(sg.t[64:128, ct, 64:128], src[2 * ct + 1], [], [sg.r])
                    S.op("dve", lambda e: e.tensor_copy(out=dst.t[:], in_=sg.t[:]), [sg.r], [dst.r])
            S.barrier()
            sel = sbuf(pa, [128, NB], F32)
            ld(sel.t[:], sel_d, [], [sel.r])

            uvstg = Rot([sbuf(pa, [128, 2 * D], BF16) for _ in range(2)])
            uv_chunk = [0]

            def convert_uv(nchunks):
                for _ in range(nchunks):
                    c = uv_chunk[0]
                    if c >= 128:
                        return
                    uv_chunk[0] += 1
                    sg = uvstg.next()
                    S.dma("pool", lambda e: e.dma_start(out=sg.t[:], in_=puv_d[c * 128:(c + 1) * 128, :]), [], [sg.r])
                    ld(UVb_d[c * 128:(c + 1) * 128, :], sg.t[:], [sg.r], [])

            xt = Rot([sbuf(pa, [128, D], F32) for _ in range(3)])
            junk = sbuf(pa, [128, D], BF16)
            ss = Rot([sbuf(pa, [128, 1], F32) for _ in range(2)])
            rstd = Rot([sbuf(pa, [128, 1], F32) for _ in range(2)])
            tmp1 = Rot([sbuf(pa, [128, 8], F32) for _ in range(2)])
            tmp1k = Rot([sbuf(pa, [128, 8], F32) for _ in range(2)])
            xn = Rot([sbuf(pa, [128, D], BF16) for _ in range(2)])
            xnT = Rot([sbuf(pa, [128, 8, 512], BF16) for _ in range(2)])
            ksq = sbuf(pa, [128, 8, 64], F32)
            kms = Rot([sbuf(pa, [128, 8], F32) for _ in range(2)])
            ksc = Rot([sbuf(pa, [128, 8], F32) for _ in range(2)])
            kn = Rot([sbuf(pa, [128, 512], BF16) for _ in range(2)])

            pT, pT2 = pbT
            pK, pV, pX, pZa, pZi, pQ = pbF

            def P0(src_rows):
                x_ = xt.next()
                ld(x_.t[:], src_rows, [], [x_.r])
                return x_

            def P1a(x_):
                s_ = ss.next()
                S.op("act", lambda e: e.activation(out=junk.t[:], in_=x_.t[:], func=AF.Square, accum_out=s_.t[:]),
                     [x_.r], [s_.r])
                return s_

            def P1b(x_, s_):
                r_ = rstd.next()
                t_ = tmp1.next()
                rstd_from(s_.t[:], r_.t[:], s_.r, r_.r, 1, 1.0 / D, t_)
                n_ = xn.next()
                S.op("dve", lambda e: e.tensor_scalar(out=n_.t[:], in0=x_.t[:], scalar1=r_.t[:, 0:1], scalar2=None,
                                                      op0=ALU.mult), [x_.r, r_.r], [n_.r])
                return n_

            def P1(src_rows, x_=None):
                if x_ is None:
                    x_ = P0(src_rows)
                return P1b(x_, P1a(x_))

            def P2(n_, xnT_b, b):
                for dc in range(8):
                    S.op("pe", lambda e: e.transpose(out=pT.t[:, dc * 128:(dc + 1) * 128],
                                                     in_=n_.t[:, dc * 128:(dc + 1) * 128], identity=ident.t[:]),
                         [n_.r, ident.r], [pT.r])
                S.op("act", lambda e: e.activation(out=xnT_b.t[:, :, b * 128:(b + 1) * 128],
                                                   in_=pT.t[:].rearrange("p (a b) -> p a b", a=8), func=AF.Copy),
                     [pT.r], [xnT_b.r])

            def norm_block(src_rows, xnT_b, b):
                P2(P1(src_rows), xnT_b, b)

            def qk_norm_a(pk):
                S.op("act", lambda e: e.activation(out=ksq.t[:].rearrange("p a b -> p (a b)"), in_=pk.t[:],
                                                   func=AF.Square), [pk.r], [ksq.r])
                ms = kms.next()
                S.op("dve", lambda e: e.tensor_reduce(out=ms.t[:], in_=ksq.t[:], axis=AX.X, op=ALU.add),
                     [ksq.r], [ms.r])
                return ms

            def qk_norm_b(pk, ms, gains):
                sc = ksc.next()
                t_ = tmp1k.next()
                rstd_from(ms.t[:], sc.t[:], ms.r, sc.r, 8, 1.0 / 64, t_)
                k_ = kn.next()
                if gains:
                    S.op("dve", lambda e: e.tensor_tensor(out=ksq.t[:], in0=pk.t[:].rearrange("p (a b) -> p a b", a=8),
                                                          in1=sc.t[:].unsqueeze(2).to_broadcast([128, 8, 64]),
                                                          op=ALU.mult), [pk.r, sc.r], [ksq.r])
                    S.op("dve", lambda e: e.tensor_tensor(out=k_.t[:].rearrange("p (a b) -> p a b", a=8),
                                                          in0=ksq.t[:], in1=gqk.t[:], op=ALU.mult),
                         [ksq.r, gqk.r], [k_.r])
                else:
                    S.op("dve", lambda e: e.tensor_tensor(out=k_.t[:].rearrange("p (a b) -> p a b", a=8),
                                                          in0=pk.t[:].rearrange("p (a b) -> p a b", a=8),
                                                          in1=sc.t[:].unsqueeze(2).to_broadcast([128, 8, 64]),
                                                          op=ALU.mult), [pk.r, sc.r], [k_.r])
                return k_

            def qk_norm(pk, gains):
                return qk_norm_b(pk, qk_norm_a(pk), gains)

            pa1 = ExitStack()
            Wkvx = sbuf(pa1, [128, 8, 1536], BF16)
            load_w([(Wkvx, 0, 512), (Wkvx, 512, 1024), (Wkvx, 1024, 1536)])
            KTs = Rot([sbuf(pa1, [128, 4, 512], BF16) for _ in range(1)])
            Vs = Rot([sbuf(pa1, [128, 4, 512], BF16) for _ in range(2)])
            xr = [sbuf(pa1, [128, 515], F32) for _ in range(4)]
            hprev = [sbuf(pa1, [128, 1], F32) for _ in range(4)]
            for ct in range(4):
                S.op("pool", lambda e: e.memset(xr[ct].t[:, 0:3], 0.0), [], [xr[ct].r])
                S.op("pool", lambda e: e.memset(hprev[ct].t[:], 0.0), [], [hprev[ct].r])
            NR = 2
            ry = Rot([sbuf(pa1, [128, 512], F32) for _ in range(3)])
            ryb = Rot([sbuf(pa1, [128, 512], BF16) for _ in range(NR)])
            rea = Rot([sbuf(pa1, [128, 512], F32) for _ in range(NR)])
            rei = Rot([sbuf(pa1, [128, 512], F32) for _ in range(NR)])
            ra_ = Rot([sbuf(pa1, [128, 512], F32) for _ in range(NR)])
            rsq = Rot([sbuf(pa1, [128, 512], F32) for _ in range(NR)])
            rhs_ = Rot([sbuf(pa1, [128, 512], F32) for _ in range(NR)])

            n_sb = 32 if debug is None or "nsb" not in debug else debug["nsb"][0]
            NBLK = 4 * n_sb
            pKr = Rot([pK, pQ])
            sbst = {}
            blkst = {}
            rgst = {}

            def get_sb(sb):
                if sb not in sbst:
                    sbst[sb] = (xnT.next(), KTs.next(), Vs.next())
                return sbst[sb]

            def sP0(n):
                blkst[n] = {"x": P0(x_all[n * 128:(n + 1) * 128, :])}

            def sP1(n):
                blkst[n]["ss"] = P1a(blkst[n]["x"])

            def sP1b(n):
                blkst[n]["xn"] = P1b(blkst[n]["x"], blkst[n]["ss"])

            def sP2(n):
                xT_, kts, vs = get_sb(n // 4)
                P2(blkst[n]["xn"], xT_, n % 4)

            def sP3(n):
                sb, b = n // 4, n % 4
                xT_, kts, vs = get_sb(sb)
                pk = pKr.next()
                for dc in range(8):
                    S.op("pe", lambda e: e.matmul(pk.t[:], lhsT=xT_.t[:, dc, b * 128:(b + 1) * 128],
                                                  rhs=Wkvx.t[:, dc, 0:512], start=(dc == 0), stop=(dc == 7)),
                         [xT_.r, Wkvx.r], [pk.r])
                for dc in range(8):
                    S.op("pe", lambda e: e.matmul(pV.t[:], lhsT=xT_.t[:, dc, b * 128:(b + 1) * 128],
                                                  rhs=Wkvx.t[:, dc, 512:1024], start=(dc == 0), stop=(dc == 7)),
                         [xT_.r, Wkvx.r], [pV.r])
                blkst[n]["pk"] = pk
                blkst[n]["ms"] = qk_norm_a(pk)
                S.op("act", lambda e: e.activation(out=vs.t[:, b, :], in_=pV.t[:], func=AF.Copy), [pV.r], [vs.r])

            def sP3b(n):
                blkst[n]["kn"] = qk_norm_b(blkst[n]["pk"], blkst[n]["ms"], True)

            def sP4(n):
                sb, b = n // 4, n % 4
                xT_, kts, vs = get_sb(sb)
                k_ = blkst[n]["kn"]
                for hp in range(4):
                    S.op("pe", lambda e: e.transpose(out=pT2.t[:, hp * 128:(hp + 1) * 128],
                                                     in_=k_.t[:, hp * 128:(hp + 1) * 128], identity=ident.t[:]),
                         [k_.r, ident.r], [pT2.r])
                S.op("act", lambda e: e.activation(out=kts.t[:, :, b * 128:(b + 1) * 128],
                                                   in_=pT2.t[:, 0:512].rearrange("p (a b) -> p a b", a=4),
                                                   func=AF.Copy), [pT2.r], [kts.r])
                if b == 3:
                    for hp in range(4):
                        ld(KT_d[hp, :, sb * 512:(sb + 1) * 512], kts.t[:, hp, :], [kts.r], [rKT])
                    ld(V_d[sb * 512:(sb + 1) * 512, :].rearrange("(b p) e -> p b e", p=128), vs.t[:], [vs.r], [rV])
                    convert_uv(4)
                del blkst[n]

            def sR1(k):
                sb, ct = k // 4, k % 4
                xT_, kts, vs = get_sb(sb)
                for dc in range(8):
                    S.op("pe", lambda e: e.matmul(pX.t[:], lhsT=Wkvx.t[:, dc, 1024 + ct * 128:1024 + (ct + 1) * 128],
                                                  rhs=xT_.t[:, dc, :], start=(dc == 0), stop=(dc == 7)),
                         [xT_.r, Wkvx.r], [pX.r])
                xr_ = xr[ct]
                S.op("act", lambda e: e.activation(out=xr_.t[:, 3:515], in_=pX.t[:], func=AF.Copy),
                     [pX.r], [xr_.r])

            def sR1b(k):
                sb, ct = k // 4, k % 4
                xr_ = xr[ct]
                y_ = ry.next()
                S.op("dve", lambda e: e.tensor_scalar(out=y_.t[:], in0=xr_.t[:, 3:515], scalar1=rgp.t[:, ct, 3:4],
                                                      scalar2=rgp.t[:, ct, 4:5], op0=ALU.mult, op1=ALU.add),
                     [xr_.r, rgp.r], [y_.r])
                for j in range(3):
                    S.op("dve", lambda e: e.scalar_tensor_tensor(out=y_.t[:], in0=xr_.t[:, j:j + 512],
                                                                 scalar=rgp.t[:, ct, j:j + 1], in1=y_.t[:],
                                                                 op0=ALU.mult, op1=ALU.add),
                         [xr_.r, rgp.r, y_.r], [y_.r])
                S.op("pool", lambda e: e.tensor_copy(out=xr_.t[:, 0:3], in_=xr_.t[:, 512:515]), [xr_.r], [xr_.r])
                yb = ryb.next()
                S.op("pool", lambda e: e.tensor_copy(out=yb.t[:], in_=y_.t[:]), [y_.r], [yb.r])
                rgst[k] = {"y": y_, "yb": yb}

            def sR2(k):
                sb, ct = k // 4, k % 4
                st = rgst[k]
                yb = st["yb"]
                S.op("pe", lambda e: e.matmul(pZa.t[:], lhsT=WaBD.t[:, ct, :], rhs=yb.t[:], start=True, stop=True),
                     [yb.r, WaBD.r], [pZa.r])
                S.op("pe", lambda e: e.matmul(pZi.t[:], lhsT=WxBD.t[:, ct, :], rhs=yb.t[:], start=True, stop=True),
                     [yb.r, WxBD.r], [pZi.r])
                ea = rea.next()
                ei = rei.next()
                S.op("act", lambda e: e.activation(out=ea.t[:], in_=pZa.t[:], func=AF.Sigmoid,
                                                   bias=rgp.t[:, ct, 5:6]), [pZa.r, rgp.r], [ea.r])
                S.op("act", lambda e: e.activation(out=ei.t[:], in_=pZi.t[:], func=AF.Sigmoid,
                                                   bias=rgp.t[:, ct, 6:7]), [pZi.r, rgp.r], [ei.r])
                a_ = ra_.next()
                sq_ = rsq.next()
                S.op("act", lambda e: e.activation(out=a_.t[:], in_=ea.t[:], func=AF.Exp, scale=rgc.t[:, ct, 0:1]),
                     [ea.r, rgc.r], [a_.r])
                S.op("act", lambda e: e.activation(out=sq_.t[:], in_=ea.t[:], func=AF.Exp, scale=rgc.t[:, ct, 1:2]),
                     [ea.r, rgc.r], [sq_.r])
                S.op("act", lambda e: e.activation(out=sq_.t[:], in_=sq_.t[:], func=AF.Ln, scale=-1.0, bias=1.0),
                     [sq_.r], [sq_.r])
                S.op("act", lambda e: e.activation(out=sq_.t[:], in_=sq_.t[:], func=AF.Exp, scale=0.5),
                     [sq_.r], [sq_.r])
                st.update({"ei": ei, "a": a_, "sq": sq_})

            def sR3(k):
                sb, ct = k // 4, k % 4
                st = rgst.pop(k)
                ei, y_, sq_, a_ = st["ei"], st["y"], st["sq"], st["a"]
                b_ = ei
                S.op("dve", lambda e: e.tensor_tensor(out=b_.t[:], in0=ei.t[:], in1=y_.t[:], op=ALU.mult),
                     [ei.r, y_.r], [b_.r])
                S.op("dve", lambda e: e.tensor_tensor(out=b_.t[:], in0=b_.t[:], in1=sq_.t[:], op=ALU.mult),
                     [b_.r, sq_.r], [b_.r])
                h_ = rhs_.next()
                hp_ = hprev[ct]
                S.op("dve", lambda e: e.tensor_tensor_scan(out=h_.t[:], data0=a_.t[:], data1=b_.t[:],
                                                           initial=hp_.t[:, 0:1], op0=ALU.mult, op1=ALU.add),
                     [a_.r, b_.r, hp_.r], [h_.r])
                S.op("pool", lambda e: e.tensor_copy(out=hp_.t[:], in_=h_.t[:, 511:512]), [h_.r], [hp_.r])
                for b in range(4):
                    blk = 4 * sb + b
                    slot = blk // 8
                    dst = hso.t[:, ct, slot * 128:(slot + 1) * 128]
                    if blk % 8 == 0:
                        S.op("dve", lambda e: e.tensor_scalar(out=dst, in0=h_.t[:, b * 128:(b + 1) * 128],
                                                              scalar1=sel.t[:, blk:blk + 1], scalar2=None,
                                                              op0=ALU.mult), [h_.r, sel.r], [hso.r])
                    else:
                        S.op("dve", lambda e: e.scalar_tensor_tensor(out=dst, in0=h_.t[:, b * 128:(b + 1) * 128],
                                                                     scalar=sel.t[:, blk:blk + 1], in1=dst,
                                                                     op0=ALU.mult, op1=ALU.add),
                             [h_.r, sel.r, hso.r], [hso.r])
                if debug and "hs" in dbg and sb < dbg["hs"].shape[1] // 512:
                    ld(dbg["hs"][ct * 128:(ct + 1) * 128, sb * 512:(sb + 1) * 512], h_.t[:], [h_.r], [])

            stages = [(sP0, 0), (sP1, 1), (sP1b, 2), (sP2, 3), (sP3, 4), (sP3b, 5), (sP4, 6), (sR1, 7), (sR1b, 8), (sR2, 9),
                      (sR3, 10)]
            for i in range(NBLK + 11):
                for fn, lag in reversed(stages):
                    if 0 <= i - lag < NBLK:
                        fn(i - lag)

            convert_uv(128)
            pa1.close()
            S.barrier()
            Wqg = sbuf(pa, [128, 8, 1024], BF16)
            load_w([(Wqg, 0, 0), (Wqg, 512, 2048)])
            gl = Rot([sbuf(pa, [128, 512], F32) for _ in range(2)])
            gt = Rot([sbuf(pa, [128, 512], F32) for _ in range(2)])
            osq = Rot([sbuf(pa, [128, 512], BF16) for _ in range(2)])
            ost = {}
            osb_ = {}

            def get_og(g):
                if g not in osb_:
                    osb_[g] = xnT.next()
                return osb_[g]

            def oQ0(n):
                ost[n] = {"x": P0(x_own[n * 128:(n + 1) * 128, :])}

            def oQ1(n):
                ost[n]["ss"] = P1a(ost[n]["x"])

            def oQ1b(n):
                ost[n]["xn"] = P1b(ost[n]["x"], ost[n]["ss"])

            def oQ2(n):
                P2(ost[n]["xn"], get_og(n // 4), n % 4)

            def oQ3(n):
                xT_ = get_og(n // 4)
                b = n % 4
                for dc in range(8):
                    S.op("pe", lambda e: e.matmul(pQ.t[:], lhsT=xT_.t[:, dc, b * 128:(b + 1) * 128],
                                                  rhs=Wqg.t[:, dc, 0:512], start=(dc == 0), stop=(dc == 7)),
                         [xT_.r, Wqg.r], [pQ.r])
                ost[n]["ms"] = qk_norm_a(pQ)

            def oQ3b(n):
                ost[n]["q"] = qk_norm_b(pQ, ost[n]["ms"], False)

            def oQ4(n):
                slot = n
                q_ = ost.pop(n)["q"]
                for hp in range(4):
                    S.op("pe", lambda e: e.transpose(out=pT2.t[:, hp * 128:(hp + 1) * 128],
                                                     in_=q_.t[:, hp * 128:(hp + 1) * 128], identity=ident.t[:]),
                         [q_.r, ident.r], [pT2.r])
                S.op("act", lambda e: e.activation(out=QT.t[:, :, slot * 128:(slot + 1) * 128],
                                                   in_=pT2.t[:, 0:512].rearrange("p (a b) -> p a b", a=4),
                                                   func=AF.Copy), [pT2.r], [QT.r])

            def oG(k):
                g, ct = k // 4, k % 4
                xT_ = get_og(g)
                if ct == 0:
                    S.op("pe", lambda e: e.matmul(pZa.t[:, 0:4], lhsT=zeros_b.t[:, 0:128], rhs=zeros_b.t[:, 0:4],
                                                  start=True, stop=False), [zeros_b.r], [pZa.r])
                for dc in range(8):
                    S.op("pe", lambda e: e.matmul(pX.t[:], lhsT=Wqg.t[:, dc, 512 + ct * 128:512 + (ct + 1) * 128],
                                                  rhs=xT_.t[:, dc, :], start=(dc == 0), stop=(dc == 7)),
                         [xT_.r, Wqg.r], [pX.r])
                g_ = gl.next()
                t_ = gt.next()
                S.op("act", lambda e: e.activation(out=g_.t[:], in_=pX.t[:], func=AF.Copy), [pX.r], [g_.r])
                S.op("dve", lambda e: e.tensor_tensor(out=t_.t[:], in0=g_.t[:], in1=g_.t[:], op=ALU.mult),
                     [g_.r], [t_.r])
                S.op("dve", lambda e: e.tensor_scalar(out=t_.t[:], in0=t_.t[:], scalar1=0.044715, scalar2=1.0,
                                                      op0=ALU.mult, op1=ALU.add), [t_.r], [t_.r])
                S.op("dve", lambda e: e.tensor_tensor(out=t_.t[:], in0=t_.t[:], in1=g_.t[:], op=ALU.mult),
                     [t_.r, g_.r], [t_.r])
                S.op("act", lambda e: e.activation(out=t_.t[:], in_=t_.t[:], func=AF.Sigmoid, scale=2.0 * GC),
                     [t_.r], [t_.r])
                S.op("dve", lambda e: e.tensor_tensor(out=g_.t[:], in0=g_.t[:], in1=t_.t[:], op=ALU.mult),
                     [g_.r, t_.r], [g_.r])
                og = hso.t[:, ct, g * 512:(g + 1) * 512]
                S.op("dve", lambda e: e.tensor_tensor(out=og, in0=og, in1=g_.t[:], op=ALU.mult),
                     [hso.r, g_.r], [hso.r])
                sq_ = osq.next()
                S.op("dve", lambda e: e.tensor_tensor(out=sq_.t[:], in0=og, in1=og, op=ALU.mult),
                     [hso.r], [sq_.r])
                for b in range(4):
                    S.op("pe", lambda e: e.matmul(pZa.t[:, b:b + 1], lhsT=sq_.t[:, b * 128:(b + 1) * 128],
                                                  rhs=ones_b.t[:, 0:1], start=False, stop=(ct == 3 and b == 3)),
                         [sq_.r, ones_b.r], [pZa.r])
                S.op("dve", lambda e: e.tensor_scalar(out=mixrg.t[:, ct, g * 512:(g + 1) * 512], in0=og,
                                                      scalar1=rgp.t[:, ct, 8:9], scalar2=None, op0=ALU.mult),
                     [hso.r, rgp.r], [mixrg.r])
                if ct == 3:
                    S.op("dve", lambda e: e.tensor_copy(out=ssrg.t[:, 4 * g:4 * g + 4], in_=pZa.t[:, 0:4]),
                         [pZa.r], [ssrg.r])

            ostages = [(oQ0, 0), (oQ1, 1), (oQ1b, 2), (oQ2, 3), (oQ3, 4), (oQ3b, 5), (oQ4, 6), (oG, 8)]
            for i in range(NSLOT + 9):
                for fn, lag in reversed(ostages):
                    if 0 <= i - lag < NSLOT:
                        fn(i - lag)
            if debug and "org" in dbg:
                for ct in range(4):
                    ld(dbg["org"][ct * 128:(ct + 1) * 128, :], hso.t[:, ct, :], [hso.r], [])
            if debug and "ssrg" in dbg:
                ld(dbg["ssrg"], ssrg.t[:], [ssrg.r], [])

        S.barrier()
        if debug and debug.get("stop") == "A":
            S.finish()
            return nc

        with ExitStack() as pb:
            masks = sbuf(pb, [128, 2, 8, 128], F32)
            ld(masks.t[:].rearrange("p a b c -> p (a b c)"), masks_d, [], [masks.r])
            ntri_f = sbuf(pb, [128, 128], F32)
            ntri = sbuf(pb, [128, 128], BF16)
            nones = sbuf(pb, [128, 128], BF16)
            S.op("pool", lambda e: e.memset(ntri_f.t[:], -1.0), [], [ntri_f.r])
            S.op("pool", lambda e: e.affine_select(out=ntri_f.t[:], in_=ntri_f.t[:], pattern=[[-1, 128]],
                                                    compare_op=ALU.is_ge, fill=0.0, base=0, channel_multiplier=1),
                 [ntri_f.r], [ntri_f.r])
            S.op("dve", lambda e: e.tensor_copy(out=ntri.t[:], in_=ntri_f.t[:]), [ntri_f.r], [ntri.r])
            S.op("pool", lambda e: e.memset(nones.t[:], -1.0), [], [nones.r])
            gsb = sbuf(pb, [128, 4], F32)
            ld(gsb.t[:], gsb_d, [], [gsb.r])
            KTc2 = [[sbuf(pb, [128, 4096], BF16) for _ in range(4)] for _ in range(2)]
            Vc = [sbuf(pb, [128, 32, 128], BF16) for _ in range(4)]
            NW = 4
            be = Rot([sbuf(pb, [128, 512], F32) for _ in range(NW)])
            bsp = Rot([sbuf(pb, [128, 512], BF16) for _ in range(NW)])
            bwb = Rot([sbuf(pb, [128, 512], BF16) for _ in range(NW)])
            S32 = Rot([sbuf(pb, [128, 512], F32) for _ in range(2)])
            Sb = Rot([sbuf(pb, [128, 512], BF16) for _ in range(4)])
            osb = Rot([sbuf(pb, [128, 512], F32) for _ in range(2)])
            osq2 = Rot([sbuf(pb, [128, 512], BF16) for _ in range(2)])
            pZ = Rot([pbF[0], pbF[1], pbF[2], pbF[3]])
            pO = Rot([pbF[4], pbF[5]])
            pS = Buf(pbT[0].t[:].bitcast(F32))
            n_heads = 8 if not debug or "nheads" not in debug else debug["nheads"][0]

            class Step:
                pass
            steps = []
            for h in range(n_heads):
                for g in range(4):
                    jmax = 8 * (4 * g + 3) + 8
                    for j in range(jmax - 1, -1, -1):
                        st_ = Step()
                        st_.h, st_.g, st_.j = h, g, j
                        st_.first = (j == jmax - 1)
                        st_.last = (j == 0)
                        steps.append(st_)
            chain = {}

            def load_K(hp):
                KTc = KTc2[hp % 2]
                for c4 in range(4):
                    ld(KTc[c4].t[:], KT_d[hp, :, c4 * 4096:(c4 + 1) * 4096], [rKT], [KTc[c4].r])

            def load_V(hp):
                for c4 in range(4):
                    ld(Vc[c4].t[:],
                       V_d[c4 * 4096:(c4 + 1) * 4096, hp * 128:(hp + 1) * 128].rearrange("(j p) e -> p j e", p=128),
                       [rV], [Vc[c4].r])

            def geom(st_):
                h, g, j = st_.h, st_.g, st_.j
                s0 = max(4 * g, j // 8)
                c0 = (s0 - 4 * g) * 128
                msk = []
                for s in range(s0, 4 * g + 4):
                    if j >= 8 * s:
                        msk.append((slice((s - 4 * g) * 128, (s - 4 * g + 1) * 128), 0 if s < 8 else 1, j - 8 * s))
                return h // 2, h % 2, c0, msk

            def stageA(st_):
                h, g, j = st_.h, st_.g, st_.j
                hp, hh, c0, msk = geom(st_)
                p0 = hh * 64
                if st_.first and g == 0 and hh == 0 and hp == 0:
                    load_K(0)
                if st_.first and g == 0 and hh == 1 and hp + 1 < (n_heads + 1) // 2:
                    load_K(hp + 1)
                KTc = KTc2[hp % 2]
                if st_.first:
                    ch = Step()
                    ch.O = pO.next()
                    ch.S32 = S32.next()
                    ch.Sb = None
                    chain[(h, g)] = ch
                    S.op("pe", lambda e: e.matmul(ch.O.t[:, :], lhsT=zeros_b.t[:, 0:128], rhs=zeros_b.t[:, :],
                                                  start=True, stop=False), [zeros_b.r], [ch.O.r])
                    S.op("pool", lambda e: e.memset(ch.S32.t[:], 0.0), [], [ch.S32.r])
                st_.Z = pZ.next()
                kc = KTc[j // 32]
                jo = (j % 32) * 128
                qcols = slice(4 * g * 128 + c0, (4 * g + 4) * 128)
                S.op("pe", lambda e: e.matmul(st_.Z.t[:, c0:512], lhsT=kc.t[p0:p0 + 64, jo:jo + 128],
                                              rhs=QT.t[p0:p0 + 64, hp, qcols], start=True, stop=False),
                     [kc.r, QT.r], [st_.Z.r])
                st_.e = be.next()
                S.op("act", lambda e: e.activation(out=st_.e.t[:, c0:512], in_=st_.Z.t[:, c0:512], func=AF.Exp),
                     [st_.Z.r], [st_.e.r])

            def stageB(st_):
                hp, hh, c0, msk = geom(st_)
                st_.sp = bsp.next()
                S.op("act", lambda e: e.activation(out=st_.sp.t[:, c0:512], in_=st_.e.t[:, c0:512], func=AF.Ln,
                                                   bias=1.0), [st_.e.r], [st_.sp.r])
                for (cs, hf, jj) in msk:
                    S.op("pool", lambda e: e.tensor_tensor(out=st_.sp.t[:, cs], in0=st_.sp.t[:, cs],
                                                           in1=masks.t[:, hf, jj, :], op=ALU.mult),
                         [st_.sp.r, masks.r], [st_.sp.r])
                ch = chain[(st_.h, st_.g)]
                st_.Sb_in = ch.Sb
                st_.cp = getattr(ch, "c0_prev", None)
                if not st_.last:
                    sp = st_.sp
                    S.op("dve", lambda e: e.tensor_tensor(out=ch.S32.t[:, c0:512], in0=ch.S32.t[:, c0:512],
                                                          in1=sp.t[:, c0:512], op=ALU.add),
                         [ch.S32.r, sp.r], [ch.S32.r])
                    nsb_ = Sb.next()
                    S.op("dve", lambda e: e.tensor_copy(out=nsb_.t[:, c0:512], in_=ch.S32.t[:, c0:512]),
                         [ch.S32.r], [nsb_.r])
                    ch.Sb = nsb_
                    ch.c0_prev = c0

            def stageC(st_):
                h, g, j = st_.h, st_.g, st_.j
                hp, hh, c0, msk = geom(st_)
                p0 = hh * 64
                ch = chain[(h, g)]
                Z, sp = st_.Z, st_.sp
                has_carry = st_.Sb_in is not None
                S.op("pe", lambda e: e.matmul(Z.t[:, c0:512], lhsT=ntri.t[:], rhs=sp.t[:, c0:512],
                                              start=False, stop=not has_carry), [sp.r, ntri.r], [Z.r])
                if has_carry:
                    sbp = st_.Sb_in
                    cp = st_.cp
                    S.op("pe", lambda e: e.matmul(Z.t[:, cp:512], lhsT=nones.t[:], rhs=sbp.t[:, cp:512],
                                                  start=False, stop=True), [sbp.r, nones.r], [Z.r])
                wb = bwb.next()
                S.op("act", lambda e: e.activation(out=wb.t[:, c0:512], in_=Z.t[:, c0:512], func=AF.Exp),
                     [Z.r], [wb.r])
                for (cs, hf, jj) in msk:
                    S.op("pool", lambda e: e.tensor_tensor(out=wb.t[:, cs], in0=wb.t[:, cs],
                                                           in1=masks.t[:, hf, jj, :], op=ALU.mult),
                         [wb.r, masks.r], [wb.r])
                st_.wb = wb

            def stageD(st_):
                h, g, j = st_.h, st_.g, st_.j
                hp, hh, c0, msk = geom(st_)
                p0 = hh * 64
                ch = chain[(h, g)]
                wb = st_.wb
                if st_.first and g == 0 and hh == 0:
                    load_V(hp)
                vc = Vc[j // 32]
                MO = 64 * (hh + 1)
                O = ch.O
                S.op("pe", lambda e: e.matmul(O.t[0:MO, c0:512], lhsT=vc.t[:, j % 32, 0:MO], rhs=wb.t[:, c0:512],
                                              start=False, stop=st_.last), [vc.r, wb.r], [O.r])
                if st_.last:
                    o_ = osb.next()
                    S.op("act", lambda e: e.activation(out=o_.t[p0:p0 + 64, :], in_=O.t[p0:p0 + 64, :], func=AF.Copy),
                         [O.r], [o_.r])
                    S.op("dve", lambda e: e.tensor_scalar(out=mixsb.t[p0:p0 + 64, hp, g * 512:(g + 1) * 512],
                                                          in0=o_.t[p0:p0 + 64, :], scalar1=gsb.t[p0:p0 + 64, hp:hp + 1],
                                                          scalar2=None, op0=ALU.mult), [o_.r, gsb.r], [mixsb.r])
                    q2 = osq2.next()
                    S.op("dve", lambda e: e.tensor_tensor(out=q2.t[p0:p0 + 64, :], in0=o_.t[p0:p0 + 64, :],
                                                           in1=o_.t[p0:p0 + 64, :], op=ALU.mult), [o_.r], [q2.r])
                    for b in range(4):
                        S.op("pe", lambda e: e.matmul(pS.t[:, b:b + 1], lhsT=q2.t[p0:p0 + 64, b * 128:(b + 1) * 128],
                                                      rhs=ones_b.t[p0:p0 + 64, 0:1], start=True, stop=True),
                             [q2.r, ones_b.r], [pS.r])
                    S.op("dve", lambda e: e.tensor_tensor(out=sssb.t[:, 4 * g:4 * g + 4], in0=sssb.t[:, 4 * g:4 * g + 4],
                                                          in1=pS.t[:, 0:4], op=ALU.add), [sssb.r, pS.r], [sssb.r])
                    if debug and "osb" in dbg:
                        ld(dbg["osb"][h * 64:(h + 1) * 64, g * 512:(g + 1) * 512], o_.t[p0:p0 + 64, :], [o_.r], [])

            n = len(steps)
            for i in range(n + 3):
                if i < n:
                    stageA(steps[i])
                if 0 <= i - 1 < n:
                    stageB(steps[i - 1])
                if 0 <= i - 2 < n:
                    stageC(steps[i - 2])
                if 0 <= i - 3 < n:
                    stageD(steps[i - 3])

        pab.close()
        S.barrier()
        if debug and debug.get("stop") == "B":
            S.finish()
            return nc

        with ExitStack() as pc:
            Wo = sbuf(pc, [128, 8, D], BF16)
            Wq = sbuf(pc, [128, 8, 2048], BF16)
            SKT = sbuf(pc, [128, 16, 128], BF16)
            gffn = sbuf(pc, [128, D], F32)
            ld(gffn.t[:], nffn_d.to_broadcast([128, D]), [], [gffn.r])
            iota16 = sbuf(pc, [128, 16], F32)
            lo16 = sbuf(pc, [128, 16], F32)
            hi16 = sbuf(pc, [128, 16], F32)
            S.op("pool", lambda e: e.iota(iota16.t[:], pattern=[[1, 16]], base=0, channel_multiplier=0,
                                           allow_small_or_imprecise_dtypes=True), [], [iota16.r])
            S.op("dve", lambda e: e.tensor_scalar(out=lo16.t[:], in0=iota16.t[:], scalar1=16.0, scalar2=None,
                                                  op0=ALU.mult), [iota16.r], [lo16.r])
            S.op("dve", lambda e: e.tensor_scalar(out=hi16.t[:], in0=iota16.t[:], scalar1=16.0, scalar2=16.0,
                                                  op0=ALU.mult, op1=ALU.add), [iota16.r], [hi16.r])
            with ExitStack() as ws:
                wo_v = w_out.rearrange("(c p) e -> p c e", p=128)
                wq_v = wq_d.rearrange("(dc p) e -> p dc e", p=128)
                for half in range(2):
                    S.dma("pool", lambda e: e.dma_start(out=Wo.t[:, :, half * 512:(half + 1) * 512],
                                                        in_=wo_v[:, :, half * 512:(half + 1) * 512]), [], [Wo.r])
                for c4 in range(4):
                    S.dma("pool", lambda e: e.dma_start(out=Wq.t[:, :, c4 * 512:(c4 + 1) * 512],
                                                        in_=wq_v[:, :, c4 * 512:(c4 + 1) * 512]), [], [Wq.r])
                skf = sbuf(ws, [128, 16, 128], F32)
                skb = sbuf(ws, [128, 16, 128], BF16)
                ld(skf.t[:], sk_d.rearrange("a n k -> n a k"), [], [skf.r])
                S.op("dve", lambda e: e.tensor_copy(out=skb.t[:], in_=skf.t[:]), [skf.r], [skb.r])
                for half in range(2):
                    for a8 in range(8):
                        S.op("pe", lambda e: e.transpose(out=pbT[0].t[:, a8 * 128:(a8 + 1) * 128],
                                                         in_=skb.t[:, half * 8 + a8, :], identity=ident.t[:]),
                             [skb.r, ident.r], [pbT[0].r])
                    S.op("act", lambda e: e.activation(out=SKT.t[:, half * 8:(half + 1) * 8, :],
                                                       in_=pbT[0].t[:].rearrange("p (a b) -> p a b", a=8),
                                                       func=AF.Copy), [pbT[0].r], [SKT.r])
            S.barrier()

            rs_sb = sbuf(pc, [128, NSLOT], F32)
            rs_rg = sbuf(pc, [128, NSLOT], F32)
            tmp16 = sbuf(pc, [128, NSLOT], F32)
            rstd_from(sssb.t[:], rs_sb.t[:], sssb.r, rs_sb.r, NSLOT, 1.0 / 512, tmp16)
            rstd_from(ssrg.t[:], rs_rg.t[:], ssrg.r, rs_rg.r, NSLOT, 1.0 / 512, tmp16)

            x2 = Rot([sbuf(pc, [128, D], F32) for _ in range(2)])
            hqb_rot = Rot([sbuf(pc, [128, D], BF16) for _ in range(2)])
            hqT = sbuf(pc, [128, 8, 128], BF16)
            junk2 = sbuf(pc, [128, D], BF16)
            ss2 = sbuf(pc, [128, 1], F32)
            r2 = sbuf(pc, [128, 1], F32)
            t2 = sbuf(pc, [128, 8], F32)
            qb = sbuf(pc, [128, 2048], BF16)
            qT = sbuf(pc, [128, 16, 128], BF16)
            W1 = sbuf(pc, [128, 2048], F32)
            W2 = sbuf(pc, [128, 2048], F32)
            cand = sbuf(pc, [128, 8, 256], F32)
            sc3 = W1.t[:].rearrange("p (a n) -> p a n", a=16)
            scw3 = W2.t[:].rearrange("p (a n) -> p a n", a=16)
            candw3 = W2.t[:].rearrange("p (h n) -> p h n", h=8)
            oh3 = W1.t[:].rearrange("p (k a) -> p k a", a=16)
            oh4 = W1.t[:].rearrange("p (h k a) -> p h k a", h=8, a=16)
            oh2_3 = W2.t[:].rearrange("p (k a) -> p k a", a=16)
            tops = sbuf(pc, [128, 16, 16], F32)
            topi = sbuf(pc, [128, 16, 16], U32)
            topif = sbuf(pc, [128, 16, 16], F32)
            best = sbuf(pc, [128, 8, 16], F32)
            bpos = sbuf(pc, [128, 8, 16], U32)
            posf = sbuf(pc, [128, 128], F32)
            af = sbuf(pc, [128, 128], F32)
            bf = sbuf(pc, [128, 128], F32)
            i1f = sbuf(pc, [128, 128], F32)
            i2f = sbuf(pc, [128, 128], F32)
            idxf = sbuf(pc, [128, 128], F32)
            idx = Rot([sbuf(pc, [128, 128], I32) for _ in range(2)])
            gate = Rot([sbuf(pc, [128, 8, 16], F32) for _ in range(2)])
            gsum = sbuf(pc, [128, 8], F32)
            actv = sbuf(pc, [128, 128], F32)
            tg = sbuf(pc, [128, 128], F32)
            coef = Rot([sbuf(pc, [128, 128], F32) for _ in range(1)])
            JC = 2
            uvg = Rot([sbuf(pc, [128, 2 * D], BF16) for _ in range(11)])
            prod = Rot([sbuf(pc, [128, D], BF16) for _ in range(5)])
            dgr = Rot([sbuf(pc, [128, 128], BF16) for _ in range(4)])
            tgR = [Res() for _ in range(128 // JC)]
            actvR = [Res() for _ in range(128 // JC)]
            cfR = [Res() for _ in range(128 // JC)]
            accP = [pbF[2], pbF[3]]
            pP = [pbF[0], pbF[1], pbF[4], pbF[5]]
            pQ4 = [pbF[0], pbF[1], pbF[4], pbF[5]]
            pSc = [pbF[4], pbF[5], pbF[0], pbF[1]]
            n_slots = NSLOT if not debug or "nslots" not in debug else debug["nslots"][0]
            slot_state = {}

            def front(s):
                if True:
                    pass
                    ts = slice(s * 128, (s + 1) * 128)
                    x2_ = x2.next()
                    stt = {'x2': x2_}
                    gate_ = gate.next()
                    stt['gate'] = gate_
                    slot_state[s] = stt
                    ld(x2_.t[:], x_own[ts, :], [], [x2_.r])
                    yield
                    for half in range(2):
                        for c in range(4):
                            S.op("pe", lambda e: e.matmul(pP[half].t[:], lhsT=mixsb.t[:, c, ts],
                                                          rhs=Wo.t[:, c, half * 512:(half + 1) * 512],
                                                          start=(c == 0), stop=(c == 3)), [mixsb.r, Wo.r], [pP[half].r])
                            yield
                        for c in range(4):
                            S.op("pe", lambda e: e.matmul(pP[2 + half].t[:], lhsT=mixrg.t[:, c, ts],
                                                          rhs=Wo.t[:, 4 + c, half * 512:(half + 1) * 512],
                                                          start=(c == 0), stop=(c == 3)), [mixrg.r, Wo.r],
                                 [pP[2 + half].r])
                            yield
                    for half in range(2):
                        hs_ = slice(half * 512, (half + 1) * 512)
                        S.op("dve", lambda e: e.scalar_tensor_tensor(out=x2_.t[:, hs_], in0=pP[half].t[:],
                                                                     scalar=rs_sb.t[:, s:s + 1], in1=x2_.t[:, hs_],
                                                                     op0=ALU.mult, op1=ALU.add),
                             [pP[half].r, rs_sb.r, x2_.r], [x2_.r])
                        yield
                        S.op("dve", lambda e: e.scalar_tensor_tensor(out=x2_.t[:, hs_], in0=pP[2 + half].t[:],
                                                                     scalar=rs_rg.t[:, s:s + 1], in1=x2_.t[:, hs_],
                                                                     op0=ALU.mult, op1=ALU.add),
                             [pP[2 + half].r, rs_rg.r, x2_.r], [x2_.r])
                        yield
                    if debug and "x2" in dbg:
                        ld(dbg["x2"][ts, :], x2_.t[:], [x2_.r], [])
                        yield
                    S.op("act", lambda e: e.activation(out=junk2.t[:], in_=x2_.t[:], func=AF.Square, accum_out=ss2.t[:]),
                         [x2_.r], [ss2.r])
                    yield
                    rstd_from(ss2.t[:], r2.t[:], ss2.r, r2.r, 1, 1.0 / D, t2)
                    yield
                    hqb = hqb_rot.next()
                    stt['hqb'] = hqb
                    stt['hq'] = hqb
                    S.op("dve", lambda e: e.scalar_tensor_tensor(out=hqb.t[:], in0=x2_.t[:], scalar=r2.t[:, 0:1],
                                                                 in1=gffn.t[:], op0=ALU.mult, op1=ALU.mult),
                         [x2_.r, r2.r, gffn.r], [hqb.r])
                    yield
                    for dc in range(8):
                        S.op("pe", lambda e: e.transpose(out=pbT[0].t[:, dc * 128:(dc + 1) * 128],
                                                         in_=hqb.t[:, dc * 128:(dc + 1) * 128], identity=ident.t[:]),
                             [hqb.r, ident.r], [pbT[0].r])
                        yield
                    S.op("act", lambda e: e.activation(out=hqT.t[:], in_=pbT[0].t[:].rearrange("p (a b) -> p a b", a=8),
                                                       func=AF.Copy), [pbT[0].r], [hqT.r])
                    yield
                    for c4 in range(4):
                        for dc in range(8):
                            S.op("pe", lambda e: e.matmul(pQ4[c4].t[:], lhsT=hqT.t[:, dc, :],
                                                          rhs=Wq.t[:, dc, c4 * 512:(c4 + 1) * 512],
                                                          start=(dc == 0), stop=(dc == 7)), [hqT.r, Wq.r], [pQ4[c4].r])
                            yield
                        if c4 % 2 == 0:
                            S.op("act", lambda e: e.activation(out=qb.t[:, c4 * 512:(c4 + 1) * 512], in_=pQ4[c4].t[:],
                                                               func=AF.Copy), [pQ4[c4].r], [qb.r])
                            yield
                        else:
                            S.op("dve", lambda e: e.tensor_copy(out=qb.t[:, c4 * 512:(c4 + 1) * 512], in_=pQ4[c4].t[:]),
                                 [pQ4[c4].r], [qb.r])
                            yield
                    for half in range(2):
                        pt = pbT[half]
                        for a8 in range(8):
                            S.op("pe", lambda e: e.transpose(out=pt.t[:, a8 * 128:(a8 + 1) * 128],
                                                             in_=qb.t[:, (half * 8 + a8) * 128:(half * 8 + a8 + 1) * 128],
                                                             identity=ident.t[:]), [qb.r, ident.r], [pt.r])
                            yield
                        S.op("act", lambda e: e.activation(out=qT.t[:, half * 8:(half + 1) * 8, :],
                                                           in_=pt.t[:].rearrange("p (a b) -> p a b", a=8), func=AF.Copy),
                             [pt.r], [qT.r])
                        yield
                    for c4 in range(4):
                        for a4 in range(4):
                            hpi = c4 * 4 + a4
                            S.op("pe", lambda e: e.matmul(pSc[c4].t[:, a4 * 128:(a4 + 1) * 128], lhsT=qT.t[:, hpi, :],
                                                          rhs=SKT.t[:, hpi, :], start=True, stop=True),
                                 [qT.r, SKT.r], [pSc[c4].r])
                            yield
                        S.op("act", lambda e: e.activation(out=W1.t[:, c4 * 512:(c4 + 1) * 512], in_=pSc[c4].t[:],
                                                           func=AF.Copy), [pSc[c4].r], [W1.r])
                        yield
                    for a in range(16):
                        S.op("dve", lambda e: e.max(out=tops.t[:, a, 0:8], in_=sc3[:, a, :]), [W1.r], [tops.r])
                        yield
                        S.op("dve", lambda e: e.max_index(out=topi.t[:, a, 0:8], in_max=tops.t[:, a, 0:8],
                                                          in_values=sc3[:, a, :]), [W1.r, tops.r], [topi.r])
                        yield
                        S.op("dve", lambda e: e.match_replace(out=scw3[:, a, :], in_to_replace=tops.t[:, a, 0:8],
                                                              in_values=sc3[:, a, :], imm_value=-1e30),
                             [W1.r, tops.r], [W2.r])
                        yield
                        S.op("dve", lambda e: e.max(out=tops.t[:, a, 8:16], in_=scw3[:, a, :]), [W2.r], [tops.r])
                        yield
                        S.op("dve", lambda e: e.max_index(out=topi.t[:, a, 8:16], in_max=tops.t[:, a, 8:16],
                                                          in_values=scw3[:, a, :]), [W2.r, tops.r], [topi.r])
                        yield
                    S.op("dve", lambda e: e.tensor_copy(out=topif.t[:], in_=topi.t[:]), [topi.r], [topif.r])
                    yield
                    for h in range(8):
                        S.op("dve", lambda e: e.tensor_tensor(
                            out=cand.t[:, h, :].rearrange("p (a b) -> p a b", a=16),
                            in0=tops.t[:, 2 * h, :].unsqueeze(2).to_broadcast([128, 16, 16]),
                            in1=tops.t[:, 2 * h + 1, :].unsqueeze(1).to_broadcast([128, 16, 16]), op=ALU.add),
                            [tops.r], [cand.r])
                        yield
                    for h in range(8):
                        S.op("dve", lambda e: e.max(out=best.t[:, h, 0:8], in_=cand.t[:, h, :]), [cand.r], [best.r])
                        yield
                        S.op("dve", lambda e: e.max_index(out=bpos.t[:, h, 0:8], in_max=best.t[:, h, 0:8],
                                                          in_values=cand.t[:, h, :]), [cand.r, best.r], [bpos.r])
                        yield
                        S.op("dve", lambda e: e.match_replace(out=candw3[:, h, :], in_to_replace=best.t[:, h, 0:8],
                                                              in_values=cand.t[:, h, :], imm_value=-1e30),
                             [cand.r, best.r], [W2.r])
                        yield
                        S.op("dve", lambda e: e.max(out=best.t[:, h, 8:16], in_=candw3[:, h, :]), [W2.r], [best.r])
                        yield
                        S.op("dve", lambda e: e.max_index(out=bpos.t[:, h, 8:16], in_max=best.t[:, h, 8:16],
                                                          in_values=candw3[:, h, :]), [W2.r, best.r], [bpos.r])
                        yield
                    S.op("dve", lambda e: e.tensor_copy(out=posf.t[:], in_=bpos.t[:].rearrange("p h k -> p (h k)")),
                         [bpos.r], [posf.r])
                    yield
                    pos_b = posf.t[:].unsqueeze(2).to_broadcast([128, 128, 16])
                    S.op("dve", lambda e: e.tensor_tensor(out=oh3, in0=pos_b,
                                                          in1=lo16.t[:].unsqueeze(1).to_broadcast([128, 128, 16]),
                                                          op=ALU.is_ge), [posf.r, lo16.r], [W1.r])
                    yield
                    S.op("dve", lambda e: e.tensor_tensor(out=oh2_3, in0=pos_b,
                                                          in1=hi16.t[:].unsqueeze(1).to_broadcast([128, 128, 16]),
                                                          op=ALU.is_lt), [posf.r, hi16.r], [W2.r])
                    yield
                    S.op("dve", lambda e: e.tensor_tensor(out=W1.t[:], in0=W1.t[:], in1=W2.t[:], op=ALU.mult),
                         [W1.r, W2.r], [W1.r])
                    yield
                    S.op("dve", lambda e: e.tensor_tensor(out=oh2_3, in0=oh3,
                                                           in1=iota16.t[:].unsqueeze(1).to_broadcast([128, 128, 16]),
                                                           op=ALU.mult), [W1.r, iota16.r], [W2.r])
                    yield
                    S.op("dve", lambda e: e.tensor_reduce(out=af.t[:], in_=oh2_3, axis=AX.X, op=ALU.add), [W2.r], [af.r])
                    yield
                    for h in range(8):
                        S.op("dve", lambda e: e.tensor_tensor(
                            out=oh4[:, h, :, :], in0=oh4[:, h, :, :],
                            in1=topif.t[:, 2 * h, :].unsqueeze(1).to_broadcast([128, 16, 16]), op=ALU.mult),
                            [W1.r, topif.r], [W1.r])
                        yield
                    S.op("dve", lambda e: e.tensor_reduce(out=i1f.t[:], in_=oh3, axis=AX.X, op=ALU.add), [W1.r], [i1f.r])
                    yield
                    S.op("dve", lambda e: e.scalar_tensor_tensor(out=bf.t[:], in0=af.t[:], scalar=-16.0, in1=posf.t[:],
                                                                 op0=ALU.mult, op1=ALU.add), [af.r, posf.r], [bf.r])
                    yield
                    S.op("dve", lambda e: e.tensor_tensor(out=oh3, in0=bf.t[:].unsqueeze(2).to_broadcast([128, 128, 16]),
                                                          in1=iota16.t[:].unsqueeze(1).to_broadcast([128, 128, 16]),
                                                          op=ALU.is_equal), [bf.r, iota16.r], [W1.r])
                    yield
                    for h in range(8):
                        S.op("dve", lambda e: e.tensor_tensor(
                            out=oh4[:, h, :, :], in0=oh4[:, h, :, :],
                            in1=topif.t[:, 2 * h + 1, :].unsqueeze(1).to_broadcast([128, 16, 16]), op=ALU.mult),
                            [W1.r, topif.r], [W1.r])
                        yield
                    S.op("dve", lambda e: e.tensor_reduce(out=i2f.t[:], in_=oh3, axis=AX.X, op=ALU.add), [W1.r], [i2f.r])
                    yield
                    S.op("dve", lambda e: e.scalar_tensor_tensor(out=idxf.t[:], in0=i1f.t[:], scalar=128.0, in1=i2f.t[:],
                                                                 op0=ALU.mult, op1=ALU.add), [i1f.r, i2f.r], [idxf.r])
                    yield
                    idx_ = idx.next()
                    stt['idx'] = idx_
                    S.op("dve", lambda e: e.tensor_copy(out=idx_.t[:], in_=idxf.t[:]), [idxf.r], [idx_.r])
                    yield
                    if debug and "idx" in dbg:
                        ld(dbg["idx"][ts, :], idxf.t[:], [idxf.r], [])
                        yield
                    S.op("dve", lambda e: e.tensor_tensor(out=gate_.t[:], in0=best.t[:],
                                                          in1=best.t[:, :, 0:1].to_broadcast([128, 8, 16]),
                                                          op=ALU.subtract), [best.r], [gate_.r])
                    yield
                    S.op("act", lambda e: e.activation(out=gate_.t[:], in_=gate_.t[:], func=AF.Exp), [gate_.r], [gate_.r])
                    yield
                    S.op("dve", lambda e: e.tensor_reduce(out=gsum.t[:], in_=gate_.t[:], axis=AX.X, op=ALU.add),
                         [gate_.r], [gsum.r])
                    yield
                    S.op("dve", lambda e: e.reciprocal(out=gsum.t[:], in_=gsum.t[:]), [gsum.r], [gsum.r])
                    yield
                    S.op("dve", lambda e: e.tensor_tensor(out=gate_.t[:], in0=gate_.t[:],
                                                          in1=gsum.t[:].unsqueeze(2).to_broadcast([128, 8, 16]),
                                                          op=ALU.mult), [gate_.r, gsum.r], [gate_.r])
                    yield

            def experts(s, nxt):
                if True:
                    ts = slice(s * 128, (s + 1) * 128)
                    stt = slot_state[s]
                    x2_, hq_, idx_, gate_ = stt['x2'], stt['hq'], stt['idx'], stt['gate']
                    hqb = stt['hqb']
                    ac = x2_
                    cf = coef.next()
                    NG = 128 // JC
                    grp = [None] * NG

                    def acc_group(gi, first):
                        cs = slice(gi * JC, (gi + 1) * JC)
                        S.op("dve", lambda e: e.tensor_tensor(out=cf.t[:, cs], in0=tg.t[:, cs], in1=actv.t[:, cs],
                                                              op=ALU.mult), [tgR[gi], actvR[gi]], [cfR[gi]])
                        for jj in range(JC):
                            j = gi * JC + jj
                            b_ = grp[gi][jj]
                            dg_ = dgr.next()
                            S.op("act", lambda e: e.activation(out=dg_.t[:], in_=ident.t[:], func=AF.Copy,
                                                               scale=cf.t[:, j:j + 1]), [ident.r, cfR[gi]], [dg_.r])
                            for half in range(2):
                                S.op("pe", lambda e: e.matmul(accP[half].t[:], lhsT=dg_.t[:],
                                                              rhs=b_.t[:, D + half * 512:D + (half + 1) * 512],
                                                              start=(j == 0), stop=(j == 127)),
                                     [dg_.r, b_.r], [accP[half].r])

                    def gelu1(gi):
                        cs = slice(gi * JC, (gi + 1) * JC)
                        S.op("dve", lambda e: e.scalar_tensor_tensor(out=tg.t[:, cs], in0=tg.t[:, cs], scalar=1.0,
                                                                     in1=actv.t[:, cs], op0=ALU.add, op1=ALU.mult),
                             [tgR[gi], actvR[gi]], [tgR[gi]])
                        S.op("act", lambda e: e.activation(out=tg.t[:, cs], in_=tg.t[:, cs], func=AF.Sigmoid,
                                                           scale=2.0 * GC), [tgR[gi]], [tgR[gi]])
                        S.op("dve", lambda e: e.tensor_tensor(out=actv.t[:, cs], in0=actv.t[:, cs],
                                                              in1=gate_.t[:].rearrange("p h k -> p (h k)")[:, cs],
                                                              op=ALU.mult), [actvR[gi], gate_.r], [actvR[gi]])

                    for gi in range(NG):
                        grp[gi] = [uvg.next() for _ in range(JC)]
                        cs = slice(gi * JC, (gi + 1) * JC)
                        for jj in range(JC):
                            j = gi * JC + jj
                            b_ = grp[gi][jj]
                            S.dma("pool", lambda e: e.indirect_dma_start(
                                out=b_.t[:, :], out_offset=None, in_=UVb_d,
                                in_offset=bass.IndirectOffsetOnAxis(ap=idx_.t[:, j:j + 1], axis=0)),
                                [idx_.r], [b_.r])
                        for jj in range(JC):
                            j = gi * JC + jj
                            b_ = grp[gi][jj]
                            pr_ = prod.next()
                            S.op("dve", lambda e: e.tensor_tensor(out=pr_.t[:], in0=b_.t[:, 0:D], in1=hqb.t[:], op=ALU.mult),
                                 [b_.r, hqb.r], [pr_.r])
                            S.op("act", lambda e: e.activation(out=pr_.t[:], in_=pr_.t[:], func=AF.Copy,
                                                               accum_out=actv.t[:, j:j + 1]), [pr_.r], [pr_.r, actvR[gi]])
                        S.op("act", lambda e: e.activation(out=tg.t[:, cs], in_=actv.t[:, cs], func=AF.Square,
                                                           scale=0.044715 ** 0.5), [actvR[gi]], [tgR[gi]])
                        if gi >= 1:
                            gelu1(gi - 1)
                        if gi >= 2:
                            acc_group(gi - 2, gi == 2)
                        if nxt is not None:
                            for _ in range(FRONT_PER_GROUP):
                                next(nxt, None)
                    gelu1(NG - 1)
                    acc_group(NG - 2, False)
                    acc_group(NG - 1, False)
                    for half in range(2):
                        hs_ = slice(half * 512, (half + 1) * 512)
                        S.op("dve", lambda e: e.tensor_tensor(out=ac.t[:, hs_], in0=accP[half].t[:], in1=x2_.t[:, hs_],
                                                              op=ALU.add), [accP[half].r, x2_.r], [ac.r])
                    ld(y_own[ts, :], ac.t[:], [ac.r], [])

            FRONT_PER_GROUP = 6
            for _ in front(0):
                pass
            for s in range(n_slots):
                nxt = front(s + 1) if s + 1 < n_slots else None
                experts(s, nxt)
                if nxt is not None:
                    for _ in nxt:
                        pass
        S.finish()
    return nc


def own_blocks(c):
    return [8 * s + (c if s < 8 else 7 - c) for s in range(NSLOT)]


def make_core_inputs(c, inp):
    x = np.ascontiguousarray(inp["x"][0])
    blocks = own_blocks(c)
    rows = np.concatenate([np.arange(b * 128, (b + 1) * 128) for b in blocks])
    sel = np.zeros((128, NB), np.float32)
    sel[:, blocks] = 1.0
    masks = np.zeros((128, 2, 8, 128), np.float32)
    k = np.arange(128)[:, None]
    q = np.arange(128)[None, :]
    for hf, off in ((0, c), (1, 7 - c)):
        for jj in range(8):
            masks[:, hf, jj, :] = ((jj * 128 + k) < (off * 128 + q))
    rgp = np.zeros((512, 12), np.float32)
    rgp[:, 0:4] = inp["conv_w"][0].T
    rgp[:, 4] = inp["conv_b"][0]
    rgp[:, 5] = inp["rg_b_a"][0]
    rgp[:, 6] = inp["rg_b_x"][0]
    rgp[:, 7] = inp["rg_lambda"][0]
    rgp[:, 8] = inp["out_norm_rg"][0]
    rgp = rgp.reshape(4, 128, 12).transpose(1, 0, 2).reshape(128, 48)
    m = {
        "x_all": x,
        "x_own": np.ascontiguousarray(x[rows]),
        "sel": sel,
        "masks": masks.reshape(128, -1),
        "w_in": np.ascontiguousarray(inp["w_in"][0]),
        "nmix": np.ascontiguousarray(inp["norm_mix"][0].reshape(8, 128).T),
        "qk": np.concatenate([inp["q_norm"][0], inp["k_norm"][0]])[None, :].astype(np.float32),
        "rgp": np.ascontiguousarray(rgp),
        "rg_w_a": np.ascontiguousarray(inp["rg_w_a"][0]),
        "rg_w_x": np.ascontiguousarray(inp["rg_w_x"][0]),
        "gsb": np.ascontiguousarray(inp["out_norm_sb"][0].reshape(4, 128).T),
        "w_out": np.ascontiguousarray(inp["w_out"][0]),
        "nffn": np.ascontiguousarray(inp["norm_ffn"]),
        "wq": np.ascontiguousarray(inp["peer_w_query"][0].reshape(D, 2048)),
        "sk": np.ascontiguousarray(inp["peer_sub_keys"][0].reshape(16, 128, 128)),
        "peer_uv": inp["peer_uv"],
    }
    return m, rows


def kernel(**inputs):
    inp = {k: np.asarray(v, dtype=np.float32) for k, v in inputs.items()}
    inp["peer_uv"] = np.ascontiguousarray(np.concatenate([inp["peer_u"][0], inp["peer_v"][0]], axis=1))
    nc = build()
    in_maps, rows_all = [], []
    for c in range(8):
        m, rows = make_core_inputs(c, inp)
        in_maps.append(m)
        rows_all.append(rows)
    res = run_bass_kernel_spmd(nc, in_maps, core_ids=list(range(8)))
    out = np.zeros((1, S_LEN, D), np.float32)
    for c in range(8):
        out[0, rows_all[c]] = res.results[c]["y_own"]
    return out
```

```python
import numpy as np
from contextlib import ExitStack
import concourse.bass as bass
import concourse.mybir as mybir
from concourse.bass_utils import run_bass_kernel_spmd

F32 = mybir.dt.float32
BF16 = mybir.dt.bfloat16
U32 = mybir.dt.uint32
I32 = mybir.dt.int32
AF = mybir.ActivationFunctionType
ALU = mybir.AluOpType
AX = mybir.AxisListType

S_LEN = 16384
D = 1024
NB = 128
NSLOT = 16
EPS = 1e-6
GC = 0.7978845608028654


SAME_ENGINE_WAIT = True
NO_SELF_WAIT = set()


class Res:
    __slots__ = ("w", "r")

    def __init__(self):
        self.w = None
        self.r = {}


class Sch:
    CHUNK = 30000
    NDMA = 24

    def __init__(self, nc, stack):
        self.nc = nc
        self.stack = stack
        self.eng = {"pe": nc.tensor, "act": nc.scalar, "dve": nc.vector,
                    "pool": nc.gpsimd, "sp": nc.sync}
        self.sems = {}
        self.cnt = {k: 0 for k in self.eng}
        self.waited = {}
        self.dma_pool = {k: [] for k in self.eng}
        self.dma_rr = {k: 0 for k in self.eng}
        self.nsem = 0

    def _sem(self, key):
        s = self.sems.get(key)
        if s is None:
            s = self.stack.enter_context(self.nc.semaphore("s%d" % self.nsem))
            self.nsem += 1
            self.sems[key] = s
        return s

    def _wait(self, eng, deps):
        e = self.eng[eng]
        for key, val in deps.items():
            if key[0] == eng and (eng == "pe" or eng in NO_SELF_WAIT):
                continue
            k = (eng, key)
            if self.waited.get(k, 0) >= val:
                continue
            self.waited[k] = val
            e.wait_ge(self._sem(key), val)

    @staticmethod
    def _deps(reads, writes):
        deps = {}

        def add(k, v):
            if deps.get(k, 0) < v:
                deps[k] = v
        for r in reads:
            if r.w is not None:
                add(*r.w)
        for w in writes:
            if w.w is not None:
                add(*w.w)
            for k, v in w.r.items():
                add(k, v)
        return deps

    @staticmethod
    def _commit(tok, reads, writes):
        k, v = tok
        for r in reads:
            if r.r.get(k, 0) < v:
                r.r[k] = v
        for w in writes:
            w.w = tok
            w.r = {}

    def op(self, eng, fn, reads=(), writes=()):
        deps = self._deps(reads, writes)
        self._wait(eng, deps)
        ins = fn(self.eng[eng])
        n = self.cnt[eng]
        self.cnt[eng] = n + 1
        key = (eng, n // self.CHUNK)
        val = n % self.CHUNK + 1
        ins.then_inc(self._sem(key), 1)
        self._commit((key, val), reads, writes)
        return ins

    def dma(self, eng, fn, reads=(), writes=()):
        deps = self._deps(reads, writes)
        pool = self.dma_pool[eng]
        i = self.dma_rr[eng] % self.NDMA
        self.dma_rr[eng] += 1
        if i >= len(pool):
            pool.append([("dma", eng, len(pool)), 0])
            i = len(pool) - 1
        key, val = pool[i]
        if val > 0 and deps.get(key, 0) < val:
            deps[key] = val
        self._wait(eng, deps)
        ins = fn(self.eng[eng])
        val += 16
        pool[i][1] = val
        ins.then_inc(self._sem(key), 16)
        self._commit((key, val), reads, writes)
        return ins

    def barrier(self):
        deps = {}
        for e, pool in self.dma_pool.items():
            for key, val in pool:
                if val:
                    deps[key] = val
        for e, n in self.cnt.items():
            if n:
                deps[(e, (n - 1) // self.CHUNK)] = (n - 1) % self.CHUNK + 1
        for eng in self.eng:
            e = self.eng[eng]
            for key, val in deps.items():
                k = (eng, key)
                if self.waited.get(k, 0) >= val:
                    continue
                self.waited[k] = val
                e.wait_ge(self._sem(key), val)

    def finish(self, eng="sp"):
        deps = {}
        for e, pool in self.dma_pool.items():
            for key, val in pool:
                if val:
                    deps[key] = val
        for e, n in self.cnt.items():
            if n:
                deps[(e, (n - 1) // self.CHUNK)] = (n - 1) % self.CHUNK + 1
        e = self.eng[eng]
        for key, val in deps.items():
            e.wait_ge(self._sem(key), val)


class Buf:
    __slots__ = ("t", "r")

    def __init__(self, t):
        self.t = t
        self.r = Res()


class Rot:
    def __init__(self, bufs):
        self.bufs = bufs
        self.i = 0

    def next(self):
        b = self.bufs[self.i % len(self.bufs)]
        self.i += 1
        return b


def build(debug=None):
    nc = bass.Bass("TRN2", target_bir_lowering=False)

    def din(name, shape, dt=F32):
        return nc.dram_tensor(name, list(shape), dt, kind="ExternalInput").ap()

    x_all = din("x_all", [S_LEN, D])
    x_own = din("x_own", [2048, D])
    sel_d = din("sel", [128, NB])
    masks_d = din("masks", [128, 2 * 8 * 128])
    w_in = din("w_in", [D, 2560])
    nmix_d = din("nmix", [128, 8])
    qk_d = din("qk", [1, 128])
    rgp_d = din("rgp", [128, 4 * 12])
    rgwa_d = din("rg_w_a", [8, 64, 64])
    rgwx_d = din("rg_w_x", [8, 64, 64])
    gsb_d = din("gsb", [128, 4])
    w_out = din("w_out", [D, D])
    nffn_d = din("nffn", [1, D])
    wq_d = din("wq", [D, 2048])
    sk_d = din("sk", [16, 128, 128])
    puv_d = din("peer_uv", [16384, 2 * D])
    y_own = nc.dram_tensor("y_own", [2048, D], F32, kind="ExternalOutput").ap()
    KT_d = nc.dram_tensor("KT_scr", [4, 128, S_LEN], BF16, kind="Internal").ap()
    V_d = nc.dram_tensor("V_scr", [S_LEN, 512], BF16, kind="Internal").ap()
    UVb_d = nc.dram_tensor("UVb_scr", [16384, 2 * D], BF16, kind="Internal").ap()
    rKT = Res()
    rV = Res()
    dbg = {}
    if debug:
        for nm, shp in debug.items():
            if nm in ("nsb", "nheads", "nslots", "stop"):
                continue
            dbg[nm] = nc.dram_tensor(nm, list(shp), F32, kind="ExternalOutput").ap()

    with ExitStack() as top:
        S = Sch(nc, top)
        cnt = [0]

        def sbuf(st, shape, dt):
            cnt[0] += 1
            return Buf(st.enter_context(nc.sbuf_tensor("t%d" % cnt[0], list(shape), dt)))

        def psum(st, shape, dt):
            cnt[0] += 1
            return Buf(st.enter_context(nc.psum_tensor("p%d" % cnt[0], list(shape), dt)))

        dmaq = ["sp", "act"]
        dq = [0]

        def ld(out, in_, reads, writes, q=None):
            if q is None:
                q = dmaq[dq[0] % 2]
                dq[0] += 1
            S.dma(q, lambda e: e.dma_start(out=out, in_=in_), reads, writes)

        ident_f = sbuf(top, [128, 128], F32)
        ident = sbuf(top, [128, 128], BF16)
        ones_b = sbuf(top, [128, 128], BF16)
        zeros_b = sbuf(top, [128, 512], BF16)
        ssrg = sbuf(top, [128, NSLOT], F32)
        sssb = sbuf(top, [128, NSLOT], F32)
        mixrg = sbuf(top, [128, 4, 2048], BF16)
        mixsb = sbuf(top, [128, 4, 2048], BF16)
        pab = top.enter_context(ExitStack())
        QT = sbuf(pab, [128, 4, 2048], BF16)

        S.op("pool", lambda e: e.memset(ident_f.t[:], 0.0), [], [ident_f.r])
        S.op("pool", lambda e: e.affine_select(out=ident_f.t[:], in_=ident_f.t[:], pattern=[[-1, 128]],
                                                compare_op=ALU.not_equal, fill=1.0, base=0,
                                                channel_multiplier=1), [ident_f.r], [ident_f.r])
        S.op("dve", lambda e: e.tensor_copy(out=ident.t[:], in_=ident_f.t[:]), [ident_f.r], [ident.r])
        S.op("pool", lambda e: e.memset(ones_b.t[:], 1.0), [], [ones_b.r])
        S.op("pool", lambda e: e.memset(zeros_b.t[:], 0.0), [], [zeros_b.r])
        S.op("pool", lambda e: e.memset(sssb.t[:], 0.0), [], [sssb.r])

        pbT = [psum(top, [128, 1024], BF16) for _ in range(2)]
        pbF = [psum(top, [128, 512], F32) for _ in range(6)]

        def rstd_from(ms_ap, out_ap, res_in, res_out, n, scale, tmp):
            S.op("dve", lambda e: e.tensor_scalar(out=tmp.t[:, 0:n], in0=ms_ap, scalar1=scale, scalar2=EPS,
                                                   op0=ALU.mult, op1=ALU.add), [res_in], [tmp.r])
            S.op("act", lambda e: e.activation(out=tmp.t[:, 0:n], in_=tmp.t[:, 0:n], func=AF.Ln), [tmp.r], [tmp.r])
            S.op("act", lambda e: e.activation(out=out_ap, in_=tmp.t[:, 0:n], func=AF.Exp, scale=-0.5),
                 [tmp.r], [res_out])

        with ExitStack() as pa:
            hso = sbuf(pa, [128, 4, 2048], F32)
            nmix = sbuf(pa, [128, 8], F32)
            ld(nmix.t[:], nmix_d, [], [nmix.r])
            w_in_v = w_in.rearrange("(dc p) e -> p dc e", p=128)

            def load_w(dst_list):
                with ExitStack() as ws:
                    stg = Rot([sbuf(ws, [128, 8, 512], F32) for _ in range(1)])
                    load_w_inner(stg, dst_list)
                S.barrier()

            def load_w_inner(stg, dst_list):
                for ci, (dst, dcol, scol) in enumerate(dst_list):
                    sg = stg.next()
                    ld(sg.t[:], w_in_v[:, :, scol:scol + 512], [], [sg.r])
                    for dc in range(8):
                        eng = "dve"
                        S.op(eng, lambda e: e.tensor_scalar(out=dst.t[:, dc, dcol:dcol + 512], in0=sg.t[:, dc, :],
                                                            scalar1=nmix.t[:, dc:dc + 1], scalar2=None,
                                                            op0=ALU.mult), [sg.r, nmix.r], [dst.r])
            qk = sbuf(pa, [128, 128], F32)
            ld(qk.t[:], qk_d.to_broadcast([128, 128]), [], [qk.r])
            gqk1 = sbuf(pa, [128, 64], F32)
            S.op("dve", lambda e: e.scalar_tensor_tensor(out=gqk1.t[:], in0=qk.t[:, 0:64], scalar=0.125,
                                                         in1=qk.t[:, 64:128], op0=ALU.mult, op1=ALU.mult),
                 [qk.r], [gqk1.r])
            gqk = sbuf(pa, [128, 8, 64], F32)
            S.op("dve", lambda e: e.tensor_copy(out=gqk.t[:], in_=gqk1.t[:].unsqueeze(1).to_broadcast([128, 8, 64])),
                 [gqk1.r], [gqk.r])
            rgp = sbuf(pa, [128, 4, 12], F32)
            ld(rgp.t[:].rearrange("p a b -> p (a b)"), rgp_d, [], [rgp.r])
            rgc = sbuf(pa, [128, 4, 4], F32)
            tmpc = sbuf(pa, [128, 4], F32)
            S.op("act", lambda e: e.activation(out=tmpc.t[:], in_=rgp.t[:, :, 7], func=AF.Exp, scale=-1.0),
                 [rgp.r], [tmpc.r])
            S.op("act", lambda e: e.activation(out=tmpc.t[:], in_=tmpc.t[:], func=AF.Ln, bias=1.0),
                 [tmpc.r], [tmpc.r])
            S.op("dve", lambda e: e.tensor_scalar(out=rgc.t[:, :, 0], in0=tmpc.t[:], scalar1=-8.0, scalar2=None,
                                                  op0=ALU.mult), [tmpc.r], [rgc.r])
            S.op("dve", lambda e: e.tensor_scalar(out=rgc.t[:, :, 1], in0=tmpc.t[:], scalar1=-16.0, scalar2=None,
                                                  op0=ALU.mult), [tmpc.r], [rgc.r])
            S.op("dve", lambda e: e.tensor_scalar(out=rgc.t[:, :, 2], in0=rgp.t[:, :, 5], scalar1=-1.0, scalar2=None,
                                                  op0=ALU.mult), [rgp.r], [rgc.r])
            S.op("dve", lambda e: e.tensor_scalar(out=rgc.t[:, :, 3], in0=rgp.t[:, :, 6], scalar1=-1.0, scalar2=None,
                                                  op0=ALU.mult), [rgp.r], [rgc.r])
            WaBD = sbuf(pa, [128, 4, 128], BF16)
            WxBD = sbuf(pa, [128, 4, 128], BF16)
            with ExitStack() as ws:
                for (dst, src) in ((WaBD, rgwa_d), (WxBD, rgwx_d)):
                    sg = sbuf(ws, [128, 4, 128], F32)
                    S.op("pool", lambda e: e.memset(sg.t[:], 0.0), [], [sg.r])
                    for ct in range(4):
                        ld(sg.t[0:64, ct, 0:64], src[2 * ct], [], [sg.r])
                        ld(sg.t[64:128, ct, 64:128], src[2 * ct + 1], [], [sg.r])
                    S.op("dve", lambda e: e.tensor_copy(out=dst.t[:], in_=sg.t[:]), [sg.r], [dst.r])
            S.barrier()
            sel = sbuf(pa, [128, NB], F32)
            ld(sel.t[:], sel_d, [], [sel.r])

            uvstg = Rot([sbuf(pa, [128, 2 * D], BF16) for _ in range(2)])
            uv_chunk = [0]

            def convert_uv(nchunks):
                for _ in range(nchunks):
                    c = uv_chunk[0]
                    if c >= 128:
                        return
                    uv_chunk[0] += 1
                    sg = uvstg.next()
                    S.dma("pool", lambda e: e.dma_start(out=sg.t[:], in_=puv_d[c * 128:(c + 1) * 128, :]), [], [sg.r])
                    ld(UVb_d[c * 128:(c + 1) * 128, :], sg.t[:], [sg.r], [])

            xt = Rot([sbuf(pa, [128, D], F32) for _ in range(3)])
            junk = sbuf(pa, [128, D], BF16)
            ss = Rot([sbuf(pa, [128, 1], F32) for _ in range(2)])
            rstd = Rot([sbuf(pa, [128, 1], F32) for _ in range(2)])
            tmp1 = Rot([sbuf(pa, [128, 8], F32) for _ in range(2)])
            tmp1k = Rot([sbuf(pa, [128, 8], F32) for _ in range(2)])
            xn = Rot([sbuf(pa, [128, D], BF16) for _ in range(2)])
            xnT = Rot([sbuf(pa, [128, 8, 512], BF16) for _ in range(2)])
            ksq = sbuf(pa, [128, 8, 64], F32)
            kms = Rot([sbuf(pa, [128, 8], F32) for _ in range(2)])
            ksc = Rot([sbuf(pa, [128, 8], F32) for _ in range(2)])
            kn = Rot([sbuf(pa, [128, 512], BF16) for _ in range(2)])

            pT, pT2 = pbT
            pK, pV, pX, pZa, pZi, pQ = pbF

            def P0(src_rows):
                x_ = xt.next()
                ld(x_.t[:], src_rows, [], [x_.r])
                return x_

            def P1a(x_):
                s_ = ss.next()
                S.op("act", lambda e: e.activation(out=junk.t[:], in_=x_.t[:], func=AF.Square, accum_out=s_.t[:]),
                     [x_.r], [s_.r])
                return s_

            def P1b(x_, s_):
                r_ = rstd.next()
                t_ = tmp1.next()
                rstd_from(s_.t[:], r_.t[:], s_.r, r_.r, 1, 1.0 / D, t_)
                n_ = xn.next()
                S.op("dve", lambda e: e.tensor_scalar(out=n_.t[:], in0=x_.t[:], scalar1=r_.t[:, 0:1], scalar2=None,
                                                      op0=ALU.mult), [x_.r, r_.r], [n_.r])
                return n_

            def P1(src_rows, x_=None):
                if x_ is None:
                    x_ = P0(src_rows)
                return P1b(x_, P1a(x_))

            def P2(n_, xnT_b, b):
                for dc in range(8):
                    S.op("pe", lambda e: e.transpose(out=pT.t[:, dc * 128:(dc + 1) * 128],
                                                     in_=n_.t[:, dc * 128:(dc + 1) * 128], identity=ident.t[:]),
                         [n_.r, ident.r], [pT.r])
                S.op("act", lambda e: e.activation(out=xnT_b.t[:, :, b * 128:(b + 1) * 128],
                                                   in_=pT.t[:].rearrange("p (a b) -> p a b", a=8), func=AF.Copy),
                     [pT.r], [xnT_b.r])

            def norm_block(src_rows, xnT_b, b):
                P2(P1(src_rows), xnT_b, b)

            def qk_norm_a(pk):
                S.op("act", lambda e: e.activation(out=ksq.t[:].rearrange("p a b -> p (a b)"), in_=pk.t[:],
                                                   func=AF.Square), [pk.r], [ksq.r])
                ms = kms.next()
                S.op("dve", lambda e: e.tensor_reduce(out=ms.t[:], in_=ksq.t[:], axis=AX.X, op=ALU.add),
                     [ksq.r], [ms.r])
                return ms

            def qk_norm_b(pk, ms, gains):
                sc = ksc.next()
                t_ = tmp1k.next()
                rstd_from(ms.t[:], sc.t[:], ms.r, sc.r, 8, 1.0 / 64, t_)
                k_ = kn.next()
                if gains:
                    S.op("dve", lambda e: e.tensor_tensor(out=ksq.t[:], in0=pk.t[:].rearrange("p (a b) -> p a b", a=8),
                                                          in1=sc.t[:].unsqueeze(2).to_broadcast([128, 8, 64]),
                                                          op=ALU.mult), [pk.r, sc.r], [ksq.r])
                    S.op("dve", lambda e: e.tensor_tensor(out=k_.t[:].rearrange("p (a b) -> p a b", a=8),
                                                          in0=ksq.t[:], in1=gqk.t[:], op=ALU.mult),
                         [ksq.r, gqk.r], [k_.r])
                else:
                    S.op("dve", lambda e: e.tensor_tensor(out=k_.t[:].rearrange("p (a b) -> p a b", a=8),
                                                          in0=pk.t[:].rearrange("p (a b) -> p a b", a=8),
                                                          in1=sc.t[:].unsqueeze(2).to_broadcast([128, 8, 64]),
                                                          op=ALU.mult), [pk.r, sc.r], [k_.r])
                return k_

            def qk_norm(pk, gains):
                return qk_norm_b(pk, qk_norm_a(pk), gains)

            pa1 = ExitStack()
            Wkvx = sbuf(pa1, [128, 8, 1536], BF16)
            load_w([(Wkvx, 0, 512), (Wkvx, 512, 1024), (Wkvx, 1024, 1536)])
            KTs = Rot([sbuf(pa1, [128, 4, 512], BF16) for _ in range(1)])
            Vs = Rot([sbuf(pa1, [128, 4, 512], BF16) for _ in range(2)])
            xr = [sbuf(pa1, [128, 515], F32) for _ in range(4)]
            hprev = [sbuf(pa1, [128, 1], F32) for _ in range(4)]
            for ct in range(4):
                S.op("pool", lambda e: e.memset(xr[ct].t[:, 0:3], 0.0), [], [xr[ct].r])
                S.op("pool", lambda e: e.memset(hprev[ct].t[:], 0.0), [], [hprev[ct].r])
            NR = 2
            ry = Rot([sbuf(pa1, [128, 512], F32) for _ in range(3)])
            ryb = Rot([sbuf(pa1, [128, 512], BF16) for _ in range(NR)])
            rea = Rot([sbuf(pa1, [128, 512], F32) for _ in range(NR)])
            rei = Rot([sbuf(pa1, [128, 512], F32) for _ in range(NR)])
            ra_ = Rot([sbuf(pa1, [128, 512], F32) for _ in range(NR)])
            rsq = Rot([sbuf(pa1, [128, 512], F32) for _ in range(NR)])
            rhs_ = Rot([sbuf(pa1, [128, 512], F32) for _ in range(NR)])

            n_sb = 32 if debug is None or "nsb" not in debug else debug["nsb"][0]
            NBLK = 4 * n_sb
            pKr = Rot([pK, pQ])
            sbst = {}
            blkst = {}
            rgst = {}

            def get_sb(sb):
                if sb not in sbst:
                    sbst[sb] = (xnT.next(), KTs.next(), Vs.next())
                return sbst[sb]

            def sP0(n):
                blkst[n] = {"x": P0(x_all[n * 128:(n + 1) * 128, :])}

            def sP1(n):
                blkst[n]["ss"] = P1a(blkst[n]["x"])

            def sP1b(n):
                blkst[n]["xn"] = P1b(blkst[n]["x"], blkst[n]["ss"])

            def sP2(n):
                xT_, kts, vs = get_sb(n // 4)
                P2(blkst[n]["xn"], xT_, n % 4)

            def sP3(n):
                sb, b = n // 4, n % 4
                xT_, kts, vs = get_sb(sb)
                pk = pKr.next()
                for dc in range(8):
                    S.op("pe", lambda e: e.matmul(pk.t[:], lhsT=xT_.t[:, dc, b * 128:(b + 1) * 128],
                                                  rhs=Wkvx.t[:, dc, 0:512], start=(dc == 0), stop=(dc == 7)),
                         [xT_.r, Wkvx.r], [pk.r])
                for dc in range(8):
                    S.op("pe", lambda e: e.matmul(pV.t[:], lhsT=xT_.t[:, dc, b * 128:(b + 1) * 128],
                                                  rhs=Wkvx.t[:, dc, 512:1024], start=(dc == 0), stop=(dc == 7)),
                         [xT_.r, Wkvx.r], [pV.r])
                blkst[n]["pk"] = pk
                blkst[n]["ms"] = qk_norm_a(pk)
                S.op("act", lambda e: e.activation(out=vs.t[:, b, :], in_=pV.t[:], func=AF.Copy), [pV.r], [vs.r])

            def sP3b(n):
                blkst[n]["kn"] = qk_norm_b(blkst[n]["pk"], blkst[n]["ms"], True)

            def sP4(n):
                sb, b = n // 4, n % 4
                xT_, kts, vs = get_sb(sb)
                k_ = blkst[n]["kn"]
                for hp in range(4):
                    S.op("pe", lambda e: e.transpose(out=pT2.t[:, hp * 128:(hp + 1) * 128],
                                                     in_=k_.t[:, hp * 128:(hp + 1) * 128], identity=ident.t[:]),
                         [k_.r, ident.r], [pT2.r])
                S.op("act", lambda e: e.activation(out=kts.t[:, :, b * 128:(b + 1) * 128],
                                                   in_=pT2.t[:, 0:512].rearrange("p (a b) -> p a b", a=4),
                                                   func=AF.Copy), [pT2.r], [kts.r])
                if b == 3:
                    for hp in range(4):
                        ld(KT_d[hp, :, sb * 512:(sb + 1) * 512], kts.t[:, hp, :], [kts.r], [rKT])
                    ld(V_d[sb * 512:(sb + 1) * 512, :].rearrange("(b p) e -> p b e", p=128), vs.t[:], [vs.r], [rV])
                    convert_uv(4)
                del blkst[n]

            def sR1(k):
                sb, ct = k // 4, k % 4
                xT_, kts, vs = get_sb(sb)
                for dc in range(8):
                    S.op("pe", lambda e: e.matmul(pX.t[:], lhsT=Wkvx.t[:, dc, 1024 + ct * 128:1024 + (ct + 1) * 128],
                                                  rhs=xT_.t[:, dc, :], start=(dc == 0), stop=(dc == 7)),
                         [xT_.r, Wkvx.r], [pX.r])
                xr_ = xr[ct]
                S.op("act", lambda e: e.activation(out=xr_.t[:, 3:515], in_=pX.t[:], func=AF.Copy),
                     [pX.r], [xr_.r])

            def sR1b(k):
                sb, ct = k // 4, k % 4
                xr_ = xr[ct]
                y_ = ry.next()
                S.op("dve", lambda e: e.tensor_scalar(out=y_.t[:], in0=xr_.t[:, 3:515], scalar1=rgp.t[:, ct, 3:4],
                                                      scalar2=rgp.t[:, ct, 4:5], op0=ALU.mult, op1=ALU.add),
                     [xr_.r, rgp.r], [y_.r])
                for j in range(3):
                    S.op("dve", lambda e: e.scalar_tensor_tensor(out=y_.t[:], in0=xr_.t[:, j:j + 512],
                                                                 scalar=rgp.t[:, ct, j:j + 1], in1=y_.t[:],
                                                                 op0=ALU.mult, op1=ALU.add),
                         [xr_.r, rgp.r, y_.r], [y_.r])
                S.op("pool", lambda e: e.tensor_copy(out=xr_.t[:, 0:3], in_=xr_.t[:, 512:515]), [xr_.r], [xr_.r])
                yb = ryb.next()
                S.op("pool", lambda e: e.tensor_copy(out=yb.t[:], in_=y_.t[:]), [y_.r], [yb.r])
                rgst[k] = {"y": y_, "yb": yb}

            def sR2(k):
                sb, ct = k // 4, k % 4
                st = rgst[k]
                yb = st["yb"]
                S.op("pe", lambda e: e.matmul(pZa.t[:], lhsT=WaBD.t[:, ct, :], rhs=yb.t[:], start=True, stop=True),
                     [yb.r, WaBD.r], [pZa.r])
                S.op("pe", lambda e: e.matmul(pZi.t[:], lhsT=WxBD.t[:, ct, :], rhs=yb.t[:], start=True, stop=True),
                     [yb.r, WxBD.r], [pZi.r])
                ea = rea.next()
                ei = rei.next()
                S.op("act", lambda e: e.activation(out=ea.t[:], in_=pZa.t[:], func=AF.Sigmoid,
                                                   bias=rgp.t[:, ct, 5:6]), [pZa.r, rgp.r], [ea.r])
                S.op("act", lambda e: e.activation(out=ei.t[:], in_=pZi.t[:], func=AF.Sigmoid,
                                                   bias=rgp.t[:, ct, 6:7]), [pZi.r, rgp.r], [ei.r])
                a_ = ra_.next()
                sq_ = rsq.next()
                S.op("act", lambda e: e.activation(out=a_.t[:], in_=ea.t[:], func=AF.Exp, scale=rgc.t[:, ct, 0:1]),
                     [ea.r, rgc.r], [a_.r])
                S.op("act", lambda e: e.activation(out=sq_.t[:], in_=ea.t[:], func=AF.Exp, scale=rgc.t[:, ct, 1:2]),
                     [ea.r, rgc.r], [sq_.r])
                S.op("act", lambda e: e.activation(out=sq_.t[:], in_=sq_.t[:], func=AF.Ln, scale=-1.0, bias=1.0),
                     [sq_.r], [sq_.r])
                S.op("act", lambda e: e.activation(out=sq_.t[:], in_=sq_.t[:], func=AF.Exp, scale=0.5),
                     [sq_.r], [sq_.r])
                st.update({"ei": ei, "a": a_, "sq": sq_})

            def sR3(k):
                sb, ct = k // 4, k % 4
                st = rgst.pop(k)
                ei, y_, sq_, a_ = st["ei"], st["y"], st["sq"], st["a"]
                b_ = ei
                S.op("dve", lambda e: e.tensor_tensor(out=b_.t[:], in0=ei.t[:], in1=y_.t[:], op=ALU.mult),
                     [ei.r, y_.r], [b_.r])
                S.op("dve", lambda e: e.tensor_tensor(out=b_.t[:], in0=b_.t[:], in1=sq_.t[:], op=ALU.mult),
                     [b_.r, sq_.r], [b_.r])
                h_ = rhs_.next()
                hp_ = hprev[ct]
                S.op("dve", lambda e: e.tensor_tensor_scan(out=h_.t[:], data0=a_.t[:], data1=b_.t[:],
                                                           initial=hp_.t[:, 0:1], op0=ALU.mult, op1=ALU.add),
                     [a_.r, b_.r, hp_.r], [h_.r])
                S.op("pool", lambda e: e.tensor_copy(out=hp_.t[:], in_=h_.t[:, 511:512]), [h_.r], [hp_.r])
                for b in range(4):
                    blk = 4 * sb + b
                    slot = blk // 8
                    dst = hso.t[:, ct, slot * 128:(slot + 1) * 128]
                    if blk % 8 == 0:
                        S.op("dve", lambda e: e.tensor_scalar(out=dst, in0=h_.t[:, b * 128:(b + 1) * 128],
                                                              scalar1=sel.t[:, blk:blk + 1], scalar2=None,
                                                              op0=ALU.mult), [h_.r, sel.r], [hso.r])
                    else:
                        S.op("dve", lambda e: e.scalar_tensor_tensor(out=dst, in0=h_.t[:, b * 128:(b + 1) * 128],
                                                                     scalar=sel.t[:, blk:blk + 1], in1=dst,
                                                                     op0=ALU.mult, op1=ALU.add),
                             [h_.r, sel.r, hso.r], [hso.r])
                if debug and "hs" in dbg and sb < dbg["hs"].shape[1] // 512:
                    ld(dbg["hs"][ct * 128:(ct + 1) * 128, sb * 512:(sb + 1) * 512], h_.t[:], [h_.r], [])

            stages = [(sP0, 0), (sP1, 1), (sP1b, 2), (sP2, 3), (sP3, 4), (sP3b, 5), (sP4, 6), (sR1, 7), (sR1b, 8), (sR2, 9),
                      (sR3, 10)]
            for i in range(NBLK + 11):
                for fn, lag in reversed(stages):
                    if 0 <= i - lag < NBLK:
                        fn(i - lag)

            convert_uv(128)
            pa1.close()
            S.barrier()
            Wqg = sbuf(pa, [128, 8, 1024], BF16)
            load_w([(Wqg, 0, 0), (Wqg, 512, 2048)])
            gl = Rot([sbuf(pa, [128, 512], F32) for _ in range(2)])
            gt = Rot([sbuf(pa, [128, 512], F32) for _ in range(2)])
            osq = Rot([sbuf(pa, [128, 512], BF16) for _ in range(2)])
            ost = {}
            osb_ = {}

            def get_og(g):
                if g not in osb_:
                    osb_[g] = xnT.next()
                return osb_[g]

            def oQ0(n):
                ost[n] = {"x": P0(x_own[n * 128:(n + 1) * 128, :])}

            def oQ1(n):
                ost[n]["ss"] = P1a(ost[n]["x"])

            def oQ1b(n):
                ost[n]["xn"] = P1b(ost[n]["x"], ost[n]["ss"])

            def oQ2(n):
                P2(ost[n]["xn"], get_og(n // 4), n % 4)

            def oQ3(n):
                xT_ = get_og(n // 4)
                b = n % 4
                for dc in range(8):
                    S.op("pe", lambda e: e.matmul(pQ.t[:], lhsT=xT_.t[:, dc, b * 128:(b + 1) * 128],
                                                  rhs=Wqg.t[:, dc, 0:512], start=(dc == 0), stop=(dc == 7)),
                         [xT_.r, Wqg.r], [pQ.r])
                ost[n]["ms"] = qk_norm_a(pQ)

            def oQ3b(n):
                ost[n]["q"] = qk_norm_b(pQ, ost[n]["ms"], False)

            def oQ4(n):
                slot = n
                q_ = ost.pop(n)["q"]
                for hp in range(4):
                    S.op("pe", lambda e: e.transpose(out=pT2.t[:, hp * 128:(hp + 1) * 128],
                                                     in_=q_.t[:, hp * 128:(hp + 1) * 128], identity=ident.t[:]),
                         [q_.r, ident.r], [pT2.r])
                S.op("act", lambda e: e.activation(out=QT.t[:, :, slot * 128:(slot + 1) * 128],
                                                   in_=pT2.t[:, 0:512].rearrange("p (a b) -> p a b", a=4),
                                                   func=AF.Copy), [pT2.r], [QT.r])

            def oG(k):
                g, ct = k // 4, k % 4
                xT_ = get_og(g)
                if ct == 0:
                    S.op("pe", lambda e: e.matmul(pZa.t[:, 0:4], lhsT=zeros_b.t[:, 0:128], rhs=zeros_b.t[:, 0:4],
                                                  start=True, stop=False), [zeros_b.r], [pZa.r])
                for dc in range(8):
                    S.op("pe", lambda e: e.matmul(pX.t[:], lhsT=Wqg.t[:, dc, 512 + ct * 128:512 + (ct + 1) * 128],
                                                  rhs=xT_.t[:, dc, :], start=(dc == 0), stop=(dc == 7)),
                         [xT_.r, Wqg.r], [pX.r])
                g_ = gl.next()
                t_ = gt.next()
                S.op("act", lambda e: e.activation(out=g_.t[:], in_=pX.t[:], func=AF.Copy), [pX.r], [g_.r])
                S.op("dve", lambda e: e.tensor_tensor(out=t_.t[:], in0=g_.t[:], in1=g_.t[:], op=ALU.mult),
                     [g_.r], [t_.r])
                S.op("dve", lambda e: e.tensor_scalar(out=t_.t[:], in0=t_.t[:], scalar1=0.044715, scalar2=1.0,
                                                      op0=ALU.mult, op1=ALU.add), [t_.r], [t_.r])
                S.op("dve", lambda e: e.tensor_tensor(out=t_.t[:], in0=t_.t[:], in1=g_.t[:], op=ALU.mult),
                     [t_.r, g_.r], [t_.r])
                S.op("act", lambda e: e.activation(out=t_.t[:], in_=t_.t[:], func=AF.Sigmoid, scale=2.0 * GC),
                     [t_.r], [t_.r])
                S.op("dve", lambda e: e.tensor_tensor(out=g_.t[:], in0=g_.t[:], in1=t_.t[:], op=ALU.mult),
                     [g_.r, t_.r], [g_.r])
                og = hso.t[:, ct, g * 512:(g + 1) * 512]
                S.op("dve", lambda e: e.tensor_tensor(out=og, in0=og, in1=g_.t[:], op=ALU.mult),
                     [hso.r, g_.r], [hso.r])
                sq_ = osq.next()
                S.op("dve", lambda e: e.tensor_tensor(out=sq_.t[:], in0=og, in1=og, op=ALU.mult),
                     [hso.r], [sq_.r])
                for b in range(4):
                    S.op("pe", lambda e: e.matmul(pZa.t[:, b:b + 1], lhsT=sq_.t[:, b * 128:(b + 1) * 128],
                                                  rhs=ones_b.t[:, 0:1], start=False, stop=(ct == 3 and b == 3)),
                         [sq_.r, ones_b.r], [pZa.r])
                S.op("dve", lambda e: e.tensor_scalar(out=mixrg.t[:, ct, g * 512:(g + 1) * 512], in0=og,
                                                      scalar1=rgp.t[:, ct, 8:9], scalar2=None, op0=ALU.mult),
                     [hso.r, rgp.r], [mixrg.r])
                if ct == 3:
                    S.op("dve", lambda e: e.tensor_copy(out=ssrg.t[:, 4 * g:4 * g + 4], in_=pZa.t[:, 0:4]),
                         [pZa.r], [ssrg.r])

            ostages = [(oQ0, 0), (oQ1, 1), (oQ1b, 2), (oQ2, 3), (oQ3, 4), (oQ3b, 5), (oQ4, 6), (oG, 8)]
            for i in range(NSLOT + 9):
                for fn, lag in reversed(ostages):
                    if 0 <= i - lag < NSLOT:
                        fn(i - lag)
            if debug and "org" in dbg:
                for ct in range(4):
                    ld(dbg["org"][ct * 128:(ct + 1) * 128, :], hso.t[:, ct, :], [hso.r], [])
            if debug and "ssrg" in dbg:
                ld(dbg["ssrg"], ssrg.t[:], [ssrg.r], [])

        S.barrier()
        if debug and debug.get("stop") == "A":
            S.finish()
            return nc

        with ExitStack() as pb:
            masks = sbuf(pb, [128, 2, 8, 128], F32)
            ld(masks.t[:].rearrange("p a b c -> p (a b c)"), masks_d, [], [masks.r])
            ntri_f = sbuf(pb, [128, 128], F32)
            ntri = sbuf(pb, [128, 128], BF16)
            nones = sbuf(pb, [128, 128], BF16)
            S.op("pool", lambda e: e.memset(ntri_f.t[:], -1.0), [], [ntri_f.r])
            S.op("pool", lambda e: e.affine_select(out=ntri_f.t[:], in_=ntri_f.t[:], pattern=[[-1, 128]],
                                                    compare_op=ALU.is_ge, fill=0.0, base=0, channel_multiplier=1),
                 [ntri_f.r], [ntri_f.r])
            S.op("dve", lambda e: e.tensor_copy(out=ntri.t[:], in_=ntri_f.t[:]), [ntri_f.r], [ntri.r])
            S.op("pool", lambda e: e.memset(nones.t[:], -1.0), [], [nones.r])
            gsb = sbuf(pb, [128, 4], F32)
            ld(gsb.t[:], gsb_d, [], [gsb.r])
            KTc2 = [[sbuf(pb, [128, 4096], BF16) for _ in range(4)] for _ in range(2)]
            Vc = [sbuf(pb, [128, 32, 128], BF16) for _ in range(4)]
            NW = 4
            be = Rot([sbuf(pb, [128, 512], F32) for _ in range(NW)])
            bsp = Rot([sbuf(pb, [128, 512], BF16) for _ in range(NW)])
            bwb = Rot([sbuf(pb, [128, 512], BF16) for _ in range(NW)])
            S32 = Rot([sbuf(pb, [128, 512], F32) for _ in range(2)])
            Sb = Rot([sbuf(pb, [128, 512], BF16) for _ in range(4)])
            osb = Rot([sbuf(pb, [128, 512], F32) for _ in range(2)])
            osq2 = Rot([sbuf(pb, [128, 512], BF16) for _ in range(2)])
            pZ = Rot([pbF[0], pbF[1], pbF[2], pbF[3]])
            pO = Rot([pbF[4], pbF[5]])
            pS = Buf(pbT[0].t[:].bitcast(F32))
            n_heads = 8 if not debug or "nheads" not in debug else debug["nheads"][0]

            class Step:
                pass
            steps = []
            for h in range(n_heads):
                for g in range(4):
                    jmax = 8 * (4 * g + 3) + 8
                    for j in range(jmax - 1, -1, -1):
                        st_ = Step()
                        st_.h, st_.g, st_.j = h, g, j
                        st_.first = (j == jmax - 1)
                        st_.last = (j == 0)
                        steps.append(st_)
            chain = {}

            def load_K(hp):
                KTc = KTc2[hp % 2]
                for c4 in range(4):
                    ld(KTc[c4].t[:], KT_d[hp, :, c4 * 4096:(c4 + 1) * 4096], [rKT], [KTc[c4].r])

            def load_V(hp):
                for c4 in range(4):
                    ld(Vc[c4].t[:],
                       V_d[c4 * 4096:(c4 + 1) * 4096, hp * 128:(hp + 1) * 128].rearrange("(j p) e -> p j e", p=128),
                       [rV], [Vc[c4].r])

            def geom(st_):
                h, g, j = st_.h, st_.g, st_.j
                s0 = max(4 * g, j // 8)
                c0 = (s0 - 4 * g) * 128
                msk = []
                for s in range(s0, 4 * g + 4):
                    if j >= 8 * s:
                        msk.append((slice((s - 4 * g) * 128, (s - 4 * g + 1) * 128), 0 if s < 8 else 1, j - 8 * s))
                return h // 2, h % 2, c0, msk

            def stageA(st_):
                h, g, j = st_.h, st_.g, st_.j
                hp, hh, c0, msk = geom(st_)
                p0 = hh * 64
                if st_.first and g == 0 and hh == 0 and hp == 0:
                    load_K(0)
                if st_.first and g == 0 and hh == 1 and hp + 1 < (n_heads + 1) // 2:
                    load_K(hp + 1)
                KTc = KTc2[hp % 2]
                if st_.first:
                    ch = Step()
                    ch.O = pO.next()
                    ch.S32 = S32.next()
                    ch.Sb = None
                    chain[(h, g)] = ch
                    S.op("pe", lambda e: e.matmul(ch.O.t[:, :], lhsT=zeros_b.t[:, 0:128], rhs=zeros_b.t[:, :],
                                                  start=True, stop=False), [zeros_b.r], [ch.O.r])
                    S.op("pool", lambda e: e.memset(ch.S32.t[:], 0.0), [], [ch.S32.r])
                st_.Z = pZ.next()
                kc = KTc[j // 32]
                jo = (j % 32) * 128
                qcols = slice(4 * g * 128 + c0, (4 * g + 4) * 128)
                S.op("pe", lambda e: e.matmul(st_.Z.t[:, c0:512], lhsT=kc.t[p0:p0 + 64, jo:jo + 128],
                                              rhs=QT.t[p0:p0 + 64, hp, qcols], start=True, stop=False),
                     [kc.r, QT.r], [st_.Z.r])
                st_.e = be.next()
                S.op("act", lambda e: e.activation(out=st_.e.t[:, c0:512], in_=st_.Z.t[:, c0:512], func=AF.Exp),
                     [st_.Z.r], [st_.e.r])

            def stageB(st_):
                hp, hh, c0, msk = geom(st_)
                st_.sp = bsp.next()
                S.op("act", lambda e: e.activation(out=st_.sp.t[:, c0:512], in_=st_.e.t[:, c0:512], func=AF.Ln,
                                                   bias=1.0), [st_.e.r], [st_.sp.r])
                for (cs, hf, jj) in msk:
                    S.op("pool", lambda e: e.tensor_tensor(out=st_.sp.t[:, cs], in0=st_.sp.t[:, cs],
                                                           in1=masks.t[:, hf, jj, :], op=ALU.mult),
                         [st_.sp.r, masks.r], [st_.sp.r])
                ch = chain[(st_.h, st_.g)]
                st_.Sb_in = ch.Sb
                st_.cp = getattr(ch, "c0_prev", None)
                if not st_.last:
                    sp = st_.sp
                    S.op("dve", lambda e: e.tensor_tensor(out=ch.S32.t[:, c0:512], in0=ch.S32.t[:, c0:512],
                                                          in1=sp.t[:, c0:512], op=ALU.add),
                         [ch.S32.r, sp.r], [ch.S32.r])
                    nsb_ = Sb.next()
                    S.op("dve", lambda e: e.tensor_copy(out=nsb_.t[:, c0:512], in_=ch.S32.t[:, c0:512]),
                         [ch.S32.r], [nsb_.r])
                    ch.Sb = nsb_
                    ch.c0_prev = c0

            def stageC(st_):
                h, g, j = st_.h, st_.g, st_.j
                hp, hh, c0, msk = geom(st_)
                p0 = hh * 64
                ch = chain[(h, g)]
                Z, sp = st_.Z, st_.sp
                has_carry = st_.Sb_in is not None
                S.op("pe", lambda e: e.matmul(Z.t[:, c0:512], lhsT=ntri.t[:], rhs=sp.t[:, c0:512],
                                              start=False, stop=not has_carry), [sp.r, ntri.r], [Z.r])
                if has_carry:
                    sbp = st_.Sb_in
                    cp = st_.cp
                    S.op("pe", lambda e: e.matmul(Z.t[:, cp:512], lhsT=nones.t[:], rhs=sbp.t[:, cp:512],
                                                  start=False, stop=True), [sbp.r, nones.r], [Z.r])
                wb = bwb.next()
                S.op("act", lambda e: e.activation(out=wb.t[:, c0:512], in_=Z.t[:, c0:512], func=AF.Exp),
                     [Z.r], [wb.r])
                for (cs, hf, jj) in msk:
                    S.op("pool", lambda e: e.tensor_tensor(out=wb.t[:, cs], in0=wb.t[:, cs],
                                                           in1=masks.t[:, hf, jj, :], op=ALU.mult),
                         [wb.r, masks.r], [wb.r])
                st_.wb = wb

            def stageD(st_):
                h, g, j = st_.h, st_.g, st_.j
                hp, hh, c0, msk = geom(st_)
                p0 = hh * 64
                ch = chain[(h, g)]
                wb = st_.wb
                if st_.first and g == 0 and hh == 0:
                    load_V(hp)
                vc = Vc[j // 32]
                MO = 64 * (hh + 1)
                O = ch.O
                S.op("pe", lambda e: e.matmul(O.t[0:MO, c0:512], lhsT=vc.t[:, j % 32, 0:MO], rhs=wb.t[:, c0:512],
                                              start=False, stop=st_.last), [vc.r, wb.r], [O.r])
                if st_.last:
                    o_ = osb.next()
                    S.op("act", lambda e: e.activation(out=o_.t[p0:p0 + 64, :], in_=O.t[p0:p0 + 64, :], func=AF.Copy),
                         [O.r], [o_.r])
                    S.op("dve", lambda e: e.tensor_scalar(out=mixsb.t[p0:p0 + 64, hp, g * 512:(g + 1) * 512],
                                                          in0=o_.t[p0:p0 + 64, :], scalar1=gsb.t[p0:p0 + 64, hp:hp + 1],
                                                          scalar2=None, op0=ALU.mult), [o_.r, gsb.r], [mixsb.r])
                    q2 = osq2.next()
                    S.op("dve", lambda e: e.tensor_tensor(out=q2.t[p0:p0 + 64, :], in0=o_.t[p0:p0 + 64, :],
                                                           in1=o_.t[p0:p0 + 64, :], op=ALU.mult), [o_.r], [q2.r])
                    for b in range(4):
                        S.op("pe", lambda e: e.matmul(pS.t[:, b:b + 1], lhsT=q2.t[p0:p0 + 64, b * 128:(b + 1) * 128],
                                                      rhs=ones_b.t[p0:p0 + 64, 0:1], start=True, stop=True),
                             [q2.r, ones_b.r], [pS.r])
                    S.op("dve", lambda e: e.tensor_tensor(out=sssb.t[:, 4 * g:4 * g + 4], in0=sssb.t[:, 4 * g:4 * g + 4],
                                                          in1=pS.t[:, 0:4], op=ALU.add), [sssb.r, pS.r], [sssb.r])
                    if debug and "osb" in dbg:
                        ld(dbg["osb"][h * 64:(h + 1) * 64, g * 512:(g + 1) * 512], o_.t[p0:p0 + 64, :], [o_.r], [])

            n = len(steps)
            for i in range(n + 3):
                if i < n:
                    stageA(steps[i])
                if 0 <= i - 1 < n:
                    stageB(steps[i - 1])
                if 0 <= i - 2 < n:
                    stageC(steps[i - 2])
                if 0 <= i - 3 < n:
                    stageD(steps[i - 3])

        pab.close()
        S.barrier()
        if debug and debug.get("stop") == "B":
            S.finish()
            return nc

        with ExitStack() as pc:
            Wo = sbuf(pc, [128, 8, D], BF16)
            Wq = sbuf(pc, [128, 8, 2048], BF16)
            SKT = sbuf(pc, [128, 16, 128], BF16)
            gffn = sbuf(pc, [128, D], F32)
            ld(gffn.t[:], nffn_d.to_broadcast([128, D]), [], [gffn.r])
            iota16 = sbuf(pc, [128, 16], F32)
            lo16 = sbuf(pc, [128, 16], F32)
            hi16 = sbuf(pc, [128, 16], F32)
            S.op("pool", lambda e: e.iota(iota16.t[:], pattern=[[1, 16]], base=0, channel_multiplier=0,
                                           allow_small_or_imprecise_dtypes=True), [], [iota16.r])
            S.op("dve", lambda e: e.tensor_scalar(out=lo16.t[:], in0=iota16.t[:], scalar1=16.0, scalar2=None,
                                                  op0=ALU.mult), [iota16.r], [lo16.r])
            S.op("dve", lambda e: e.tensor_scalar(out=hi16.t[:], in0=iota16.t[:], scalar1=16.0, scalar2=16.0,
                                                  op0=ALU.mult, op1=ALU.add), [iota16.r], [hi16.r])
            with ExitStack() as ws:
                wo_v = w_out.rearrange("(c p) e -> p c e", p=128)
                wq_v = wq_d.rearrange("(dc p) e -> p dc e", p=128)
                for half in range(2):
                    S.dma("pool", lambda e: e.dma_start(out=Wo.t[:, :, half * 512:(half + 1) * 512],
                                                        in_=wo_v[:, :, half * 512:(half + 1) * 512]), [], [Wo.r])
                for c4 in range(4):
                    S.dma("pool", lambda e: e.dma_start(out=Wq.t[:, :, c4 * 512:(c4 + 1) * 512],
                                                        in_=wq_v[:, :, c4 * 512:(c4 + 1) * 512]), [], [Wq.r])
                skf = sbuf(ws, [128, 16, 128], F32)
                skb = sbuf(ws, [128, 16, 128], BF16)
                ld(skf.t[:], sk_d.rearrange("a n k -> n a k"), [], [skf.r])
                S.op("dve", lambda e: e.tensor_copy(out=skb.t[:], in_=skf.t[:]), [skf.r], [skb.r])
                for half in range(2):
                    for a8 in range(8):
                        S.op("pe", lambda e: e.transpose(out=pbT[0].t[:, a8 * 128:(a8 + 1) * 128],
                                                         in_=skb.t[:, half * 8 + a8, :], identity=ident.t[:]),
                             [skb.r, ident.r], [pbT[0].r])
                    S.op("act", lambda e: e.activation(out=SKT.t[:, half * 8:(half + 1) * 8, :],
                                                       in_=pbT[0].t[:].rearrange("p (a b) -> p a b", a=8),
                                                       func=AF.Copy), [pbT[0].r], [SKT.r])
            S.barrier()

            rs_sb = sbuf(pc, [128, NSLOT], F32)
            rs_rg = sbuf(pc, [128, NSLOT], F32)
            tmp16 = sbuf(pc, [128, NSLOT], F32)
            rstd_from(sssb.t[:], rs_sb.t[:], sssb.r, rs_sb.r, NSLOT, 1.0 / 512, tmp16)
            rstd_from(ssrg.t[:], rs_rg.t[:], ssrg.r, rs_rg.r, NSLOT, 1.0 / 512, tmp16)

            x2 = Rot([sbuf(pc, [128, D], F32) for _ in range(2)])
            hqb_rot = Rot([sbuf(pc, [128, D], BF16) for _ in range(2)])
            hqT = sbuf(pc, [128, 8, 128], BF16)
            junk2 = sbuf(pc, [128, D], BF16)
            ss2 = sbuf(pc, [128, 1], F32)
            r2 = sbuf(pc, [128, 1], F32)
            t2 = sbuf(pc, [128, 8], F32)
            qb = sbuf(pc, [128, 2048], BF16)
            qT = sbuf(pc, [128, 16, 128], BF16)
            W1 = sbuf(pc, [128, 2048], F32)
            W2 = sbuf(pc, [128, 2048], F32)
            cand = sbuf(pc, [128, 8, 256], F32)
            sc3 = W1.t[:].rearrange("p (a n) -> p a n", a=16)
            scw3 = W2.t[:].rearrange("p (a n) -> p a n", a=16)
            candw3 = W2.t[:].rearrange("p (h n) -> p h n", h=8)
            oh3 = W1.t[:].rearrange("p (k a) -> p k a", a=16)
            oh4 = W1.t[:].rearrange("p (h k a) -> p h k a", h=8, a=16)
            oh2_3 = W2.t[:].rearrange("p (k a) -> p k a", a=16)
            tops = sbuf(pc, [128, 16, 16], F32)
            topi = sbuf(pc, [128, 16, 16], U32)
            topif = sbuf(pc, [128, 16, 16], F32)
            best = sbuf(pc, [128, 8, 16], F32)
            bpos = sbuf(pc, [128, 8, 16], U32)
            posf = sbuf(pc, [128, 128], F32)
            af = sbuf(pc, [128, 128], F32)
            bf = sbuf(pc, [128, 128], F32)
            i1f = sbuf(pc, [128, 128], F32)
            i2f = sbuf(pc, [128, 128], F32)
            idxf = sbuf(pc, [128, 128], F32)
            idx = Rot([sbuf(pc, [128, 128], I32) for _ in range(2)])
            gate = Rot([sbuf(pc, [128, 8, 16], F32) for _ in range(2)])
            gsum = sbuf(pc, [128, 8], F32)
            actv = sbuf(pc, [128, 128], F32)
            tg = sbuf(pc, [128, 128], F32)
            coef = Rot([sbuf(pc, [128, 128], F32) for _ in range(1)])
            JC = 2
            uvg = Rot([sbuf(pc, [128, 2 * D], BF16) for _ in range(11)])
            prod = Rot([sbuf(pc, [128, D], BF16) for _ in range(5)])
            dgr = Rot([sbuf(pc, [128, 128], BF16) for _ in range(4)])
            tgR = [Res() for _ in range(128 // JC)]
            actvR = [Res() for _ in range(128 // JC)]
            cfR = [Res() for _ in range(128 // JC)]
            accP = [pbF[2], pbF[3]]
            pP = [pbF[0], pbF[1], pbF[4], pbF[5]]
            pQ4 = [pbF[0], pbF[1], pbF[4], pbF[5]]
            pSc = [pbF[4], pbF[5], pbF[0], pbF[1]]
            n_slots = NSLOT if not debug or "nslots" not in debug else debug["nslots"][0]
            slot_state = {}

            def front(s):
                if True:
                    pass
                    ts = slice(s * 128, (s + 1) * 128)
                    x2_ = x2.next()
                    stt = {'x2': x2_}
                    gate_ = gate.next()
                    stt['gate'] = gate_
                    slot_state[s] = stt
                    ld(x2_.t[:], x_own[ts, :], [], [x2_.r])
                    yield
                    for half in range(2):
                        for c in range(4):
                            S.op("pe", lambda e: e.matmul(pP[half].t[:], lhsT=mixsb.t[:, c, ts],
                                                          rhs=Wo.t[:, c, half * 512:(half + 1) * 512],
                                                          start=(c == 0), stop=(c == 3)), [mixsb.r, Wo.r], [pP[half].r])
                            yield
                        for c in range(4):
                            S.op("pe", lambda e: e.matmul(pP[2 + half].t[:], lhsT=mixrg.t[:, c, ts],
                                                          rhs=Wo.t[:, 4 + c, half * 512:(half + 1) * 512],
                                                          start=(c == 0), stop=(c == 3)), [mixrg.r, Wo.r],
                                 [pP[2 + half].r])
                            yield
                    for half in range(2):
                        hs_ = slice(half * 512, (half + 1) * 512)
                        S.op("dve", lambda e: e.scalar_tensor_tensor(out=x2_.t[:, hs_], in0=pP[half].t[:],
                                                                     scalar=rs_sb.t[:, s:s + 1], in1=x2_.t[:, hs_],
                                                                     op0=ALU.mult, op1=ALU.add),
                             [pP[half].r, rs_sb.r, x2_.r], [x2_.r])
                        yield
                        S.op("dve", lambda e: e.scalar_tensor_tensor(out=x2_.t[:, hs_], in0=pP[2 + half].t[:],
                                                                     scalar=rs_rg.t[:, s:s + 1], in1=x2_.t[:, hs_],
                                                                     op0=ALU.mult, op1=ALU.add),
                             [pP[2 + half].r, rs_rg.r, x2_.r], [x2_.r])
                        yield
                    if debug and "x2" in dbg:
                        ld(dbg["x2"][ts, :], x2_.t[:], [x2_.r], [])
                        yield
                    S.op("act", lambda e: e.activation(out=junk2.t[:], in_=x2_.t[:], func=AF.Square, accum_out=ss2.t[:]),
                         [x2_.r], [ss2.r])
                    yield
                    rstd_from(ss2.t[:], r2.t[:], ss2.r, r2.r, 1, 1.0 / D, t2)
                    yield
                    hqb = hqb_rot.next()
                    stt['hqb'] = hqb
                    stt['hq'] = hqb
                    S.op("dve", lambda e: e.scalar_tensor_tensor(out=hqb.t[:], in0=x2_.t[:], scalar=r2.t[:, 0:1],
                                                                 in1=gffn.t[:], op0=ALU.mult, op1=ALU.mult),
                         [x2_.r, r2.r, gffn.r], [hqb.r])
                    yield
                    for dc in range(8):
                        S.op("pe", lambda e: e.transpose(out=pbT[0].t[:, dc * 128:(dc + 1) * 128],
                                                         in_=hqb.t[:, dc * 128:(dc + 1) * 128], identity=ident.t[:]),
                             [hqb.r, ident.r], [pbT[0].r])
                        yield
                    S.op("act", lambda e: e.activation(out=hqT.t[:], in_=pbT[0].t[:].rearrange("p (a b) -> p a b", a=8),
                                                       func=AF.Copy), [pbT[0].r], [hqT.r])
                    yield
                    for c4 in range(4):
                        for dc in range(8):
                            S.op("pe", lambda e: e.matmul(pQ4[c4].t[:], lhsT=hqT.t[:, dc, :],
                                                          rhs=Wq.t[:, dc, c4 * 512:(c4 + 1) * 512],
                                                          start=(dc == 0), stop=(dc == 7)), [hqT.r, Wq.r], [pQ4[c4].r])
                            yield
                        if c4 % 2 == 0:
                            S.op("act", lambda e: e.activation(out=qb.t[:, c4 * 512:(c4 + 1) * 512], in_=pQ4[c4].t[:],
                                                               func=AF.Copy), [pQ4[c4].r], [qb.r])
                            yield
                        else:
                            S.op("dve", lambda e: e.tensor_copy(out=qb.t[:, c4 * 512:(c4 + 1) * 512], in_=pQ4[c4].t[:]),
                                 [pQ4[c4].r], [qb.r])
                            yield
                    for half in range(2):
                        pt = pbT[half]
                        for a8 in range(8):
                            S.op("pe", lambda e: e.transpose(out=pt.t[:, a8 * 128:(a8 + 1) * 128],
                                                             in_=qb.t[:, (half * 8 + a8) * 128:(half * 8 + a8 + 1) * 128],
                                                             identity=ident.t[:]), [qb.r, ident.r], [pt.r])
                            yield
                        S.op("act", lambda e: e.activation(out=qT.t[:, half * 8:(half + 1) * 8, :],
                                                           in_=pt.t[:].rearrange("p (a b) -> p a b", a=8), func=AF.Copy),
                             [pt.r], [qT.r])
                        yield
                    for c4 in range(4):
                        for a4 in range(4):
                            hpi = c4 * 4 + a4
                            S.op("pe", lambda e: e.matmul(pSc[c4].t[:, a4 * 128:(a4 + 1) * 128], lhsT=qT.t[:, hpi, :],
                                                          rhs=SKT.t[:, hpi, :], start=True, stop=True),
                                 [qT.r, SKT.r], [pSc[c4].r])
                            yield
                        S.op("act", lambda e: e.activation(out=W1.t[:, c4 * 512:(c4 + 1) * 512], in_=pSc[c4].t[:],
                                                           func=AF.Copy), [pSc[c4].r], [W1.r])
                        yield
                    for a in range(16):
                        S.op("dve", lambda e: e.max(out=tops.t[:, a, 0:8], in_=sc3[:, a, :]), [W1.r], [tops.r])
                        yield
                        S.op("dve", lambda e: e.max_index(out=topi.t[:, a, 0:8], in_max=tops.t[:, a, 0:8],
                                                          in_values=sc3[:, a, :]), [W1.r, tops.r], [topi.r])
                        yield
                        S.op("dve", lambda e: e.match_replace(out=scw3[:, a, :], in_to_replace=tops.t[:, a, 0:8],
                                                              in_values=sc3[:, a, :], imm_value=-1e30),
                             [W1.r, tops.r], [W2.r])
                        yield
                        S.op("dve", lambda e: e.max(out=tops.t[:, a, 8:16], in_=scw3[:, a, :]), [W2.r], [tops.r])
                        yield
                        S.op("dve", lambda e: e.max_index(out=topi.t[:, a, 8:16], in_max=tops.t[:, a, 8:16],
                                                          in_values=scw3[:, a, :]), [W2.r, tops.r], [topi.r])
                        yield
                    S.op("dve", lambda e: e.tensor_copy(out=topif.t[:], in_=topi.t[:]), [topi.r], [topif.r])
                    yield
                    for h in range(8):
                        S.op("dve", lambda e: e.tensor_tensor(
                            out=cand.t[:, h, :].rearrange("p (a b) -> p a b", a=16),
                            in0=tops.t[:, 2 * h, :].unsqueeze(2).to_broadcast([128, 16, 16]),
                            in1=tops.t[:, 2 * h + 1, :].unsqueeze(1).to_broadcast([128, 16, 16]), op=ALU.add),
                            [tops.r], [cand.r])
                        yield
                    for h in range(8):
                        S.op("dve", lambda e: e.max(out=best.t[:, h, 0:8], in_=cand.t[:, h, :]), [cand.r], [best.r])
                        yield
                        S.op("dve", lambda e: e.max_index(out=bpos.t[:, h, 0:8], in_max=best.t[:, h, 0:8],
                                                          in_values=cand.t[:, h, :]), [cand.r, best.r], [bpos.r])
                        yield
                        S.op("dve", lambda e: e.match_replace(out=candw3[:, h, :], in_to_replace=best.t[:, h, 0:8],
                                                              in_values=cand.t[:, h, :], imm_value=-1e30),
                             [cand.r, best.r], [W2.r])
                        yield
                        S.op("dve", lambda e: e.max(out=best.t[:, h, 8:16], in_=candw3[:, h, :]), [W2.r], [best.r])
                        yield
                        S.op("dve", lambda e: e.max_index(out=bpos.t[:, h, 8:16], in_max=best.t[:, h, 8:16],
                                                          in_values=candw3[:, h, :]), [W2.r, best.r], [bpos.r])
                        yield
                    S.op("dve", lambda e: e.tensor_copy(out=posf.t[:], in_=bpos.t[:].rearrange("p h k -> p (h k)")),
                         [bpos.r], [posf.r])
                    yield
                    pos_b = posf.t[:].unsqueeze(2).to_broadcast([128, 128, 16])
                    S.op("dve", lambda e: e.tensor_tensor(out=oh3, in0=pos_b,
                                                          in1=lo16.t[:].unsqueeze(1).to_broadcast([128, 128, 16]),
                                                          op=ALU.is_ge), [posf.r, lo16.r], [W1.r])
                    yield
                    S.op("dve", lambda e: e.tensor_tensor(out=oh2_3, in0=pos_b,
                                                          in1=hi16.t[:].unsqueeze(1).to_broadcast([128, 128, 16]),
                                                          op=ALU.is_lt), [posf.r, hi16.r], [W2.r])
                    yield
                    S.op("dve", lambda e: e.tensor_tensor(out=W1.t[:], in0=W1.t[:], in1=W2.t[:], op=ALU.mult),
                         [W1.r, W2.r], [W1.r])
                    yield
                    S.op("dve", lambda e: e.tensor_tensor(out=oh2_3, in0=oh3,
                                                           in1=iota16.t[:].unsqueeze(1).to_broadcast([128, 128, 16]),
                                                           op=ALU.mult), [W1.r, iota16.r], [W2.r])
                    yield
                    S.op("dve", lambda e: e.tensor_reduce(out=af.t[:], in_=oh2_3, axis=AX.X, op=ALU.add), [W2.r], [af.r])
                    yield
                    for h in range(8):
                        S.op("dve", lambda e: e.tensor_tensor(
                            out=oh4[:, h, :, :], in0=oh4[:, h, :, :],
                            in1=topif.t[:, 2 * h, :].unsqueeze(1).to_broadcast([128, 16, 16]), op=ALU.mult),
                            [W1.r, topif.r], [W1.r])
                        yield
                    S.op("dve", lambda e: e.tensor_reduce(out=i1f.t[:], in_=oh3, axis=AX.X, op=ALU.add), [W1.r], [i1f.r])
                    yield
                    S.op("dve", lambda e: e.scalar_tensor_tensor(out=bf.t[:], in0=af.t[:], scalar=-16.0, in1=posf.t[:],
                                                                 op0=ALU.mult, op1=ALU.add), [af.r, posf.r], [bf.r])
                    yield
                    S.op("dve", lambda e: e.tensor_tensor(out=oh3, in0=bf.t[:].unsqueeze(2).to_broadcast([128, 128, 16]),
                                                          in1=iota16.t[:].unsqueeze(1).to_broadcast([128, 128, 16]),
                                                          op=ALU.is_equal), [bf.r, iota16.r], [W1.r])
                    yield
                    for h in range(8):
                        S.op("dve", lambda e: e.tensor_tensor(
                            out=oh4[:, h, :, :], in0=oh4[:, h, :, :],
                            in1=topif.t[:, 2 * h + 1, :].unsqueeze(1).to_broadcast([128, 16, 16]), op=ALU.mult),
                            [W1.r, topif.r], [W1.r])
                        yield
                    S.op("dve", lambda e: e.tensor_reduce(out=i2f.t[:], in_=oh3, axis=AX.X, op=ALU.add), [W1.r], [i2f.r])
                    yield
                    S.op("dve", lambda e: e.scalar_tensor_tensor(out=idxf.t[:], in0=i1f.t[:], scalar=128.0, in1=i2f.t[:],
                                                                 op0=ALU.mult, op1=ALU.add), [i1f.r, i2f.r], [idxf.r])
                    yield
                    idx_ = idx.next()
                    stt['idx'] = idx_
                    S.op("dve", lambda e: e.tensor_copy(out=idx_.t[:], in_=idxf.t[:]), [idxf.r], [idx_.r])
                    yield
                    if debug and "idx" in dbg:
                        ld(dbg["idx"][ts, :], idxf.t[:], [idxf.r], [])
                        yield
                    S.op("dve", lambda e: e.tensor_tensor(out=gate_.t[:], in0=best.t[:],
                                                          in1=best.t[:, :, 0:1].to_broadcast([128, 8, 16]),
                                                          op=ALU.subtract), [best.r], [gate_.r])
                    yield
                    S.op("act", lambda e: e.activation(out=gate_.t[:], in_=gate_.t[:], func=AF.Exp), [gate_.r], [gate_.r])
                    yield
                    S.op("dve", lambda e: e.tensor_reduce(out=gsum.t[:], in_=gate_.t[:], axis=AX.X, op=ALU.add),
                         [gate_.r], [gsum.r])
                    yield
                    S.op("dve", lambda e: e.reciprocal(out=gsum.t[:], in_=gsum.t[:]), [gsum.r], [gsum.r])
                    yield
                    S.op("dve", lambda e: e.tensor_tensor(out=gate_.t[:], in0=gate_.t[:],
                                                          in1=gsum.t[:].unsqueeze(2).to_broadcast([128, 8, 16]),
                                                          op=ALU.mult), [gate_.r, gsum.r], [gate_.r])
                    yield

            def experts(s, nxt):
                if True:
                    ts = slice(s * 128, (s + 1) * 128)
                    stt = slot_state[s]
                    x2_, hq_, idx_, gate_ = stt['x2'], stt['hq'], stt['idx'], stt['gate']
                    hqb = stt['hqb']
                    ac = x2_
                    cf = coef.next()
                    NG = 128 // JC
                    grp = [None] * NG

                    def acc_group(gi, first):
                        cs = slice(gi * JC, (gi + 1) * JC)
                        S.op("dve", lambda e: e.tensor_tensor(out=cf.t[:, cs], in0=tg.t[:, cs], in1=actv.t[:, cs],
                                                              op=ALU.mult), [tgR[gi], actvR[gi]], [cfR[gi]])
                        for jj in range(JC):
                            j = gi * JC + jj
                            b_ = grp[gi][jj]
                            dg_ = dgr.next()
                            S.op("act", lambda e: e.activation(out=dg_.t[:], in_=ident.t[:], func=AF.Copy,
                                                               scale=cf.t[:, j:j + 1]), [ident.r, cfR[gi]], [dg_.r])
                            for half in range(2):
                                S.op("pe", lambda e: e.matmul(accP[half].t[:], lhsT=dg_.t[:],
                                                              rhs=b_.t[:, D + half * 512:D + (half + 1) * 512],
                                                              start=(j == 0), stop=(j == 127)),
                                     [dg_.r, b_.r], [accP[half].r])

                    def gelu1(gi):
                        cs = slice(gi * JC, (gi + 1) * JC)
                        S.op("dve", lambda e: e.scalar_tensor_tensor(out=tg.t[:, cs], in0=actv.t[:, cs], scalar=0.044715,
                                                                     in1=actv.t[:, cs], op0=ALU.mult, op1=ALU.mult),
                             [actvR[gi]], [tgR[gi]])
                        S.op("dve", lambda e: e.scalar_tensor_tensor(out=tg.t[:, cs], in0=tg.t[:, cs], scalar=1.0,
                                                                     in1=actv.t[:, cs], op0=ALU.add, op1=ALU.mult),
                             [tgR[gi], actvR[gi]], [tgR[gi]])
                        S.op("act", lambda e: e.activation(out=tg.t[:, cs], in_=tg.t[:, cs], func=AF.Sigmoid,
                                                           scale=2.0 * GC), [tgR[gi]], [tgR[gi]])
                        S.op("dve", lambda e: e.tensor_tensor(out=actv.t[:, cs], in0=actv.t[:, cs],
                                                              in1=gate_.t[:].rearrange("p h k -> p (h k)")[:, cs],
                                                              op=ALU.mult), [actvR[gi], gate_.r], [actvR[gi]])

                    for gi in range(NG):
                        grp[gi] = [uvg.next() for _ in range(JC)]
                        cs = slice(gi * JC, (gi + 1) * JC)
                        for jj in range(JC):
                            j = gi * JC + jj
                            b_ = grp[gi][jj]
                            S.dma("pool", lambda e: e.indirect_dma_start(
                                out=b_.t[:, :], out_offset=None, in_=UVb_d,
                                in_offset=bass.IndirectOffsetOnAxis(ap=idx_.t[:, j:j + 1], axis=0)),
                                [idx_.r], [b_.r])
                        for jj in range(JC):
                            j = gi * JC + jj
                            b_ = grp[gi][jj]
                            pr_ = prod.next()
                            S.op("dve", lambda e: e.tensor_tensor(out=pr_.t[:], in0=b_.t[:, 0:D], in1=hqb.t[:], op=ALU.mult),
                                 [b_.r, hqb.r], [pr_.r])
                            S.op("act", lambda e: e.activation(out=pr_.t[:], in_=pr_.t[:], func=AF.Copy,
                                                               accum_out=actv.t[:, j:j + 1]), [pr_.r], [pr_.r, actvR[gi]])
                        if gi >= 1:
                            gelu1(gi - 1)
                        if gi >= 2:
                            acc_group(gi - 2, gi == 2)
                        if nxt is not None:
                            for _ in range(FRONT_PER_GROUP):
                                next(nxt, None)
                    gelu1(NG - 1)
                    acc_group(NG - 2, False)
                    acc_group(NG - 1, False)
                    for half in range(2):
                        hs_ = slice(half * 512, (half + 1) * 512)
                        S.op("dve", lambda e: e.tensor_tensor(out=ac.t[:, hs_], in0=accP[half].t[:], in1=x2_.t[:, hs_],
                                                              op=ALU.add), [accP[half].r, x2_.r], [ac.r])
                    ld(y_own[ts, :], ac.t[:], [ac.r], [])

            FRONT_PER_GROUP = 4
            for _ in front(0):
                pass
            for s in range(n_slots):
                nxt = front(s + 1) if s + 1 < n_slots else None
                experts(s, nxt)
                if nxt is not None:
                    for _ in nxt:
                        pass
        S.finish()
    return nc


def own_blocks(c):
    return [8 * s + (c if s < 8 else 7 - c) for s in range(NSLOT)]


def make_core_inputs(c, inp):
    x = np.ascontiguousarray(inp["x"][0])
    blocks = own_blocks(c)
    rows = np.concatenate([np.arange(b * 128, (b + 1) * 128) for b in blocks])
    sel = np.zeros((128, NB), np.float32)
    sel[:, blocks] = 1.0
    masks = np.zeros((128, 2, 8, 128), np.float32)
    k = np.arange(128)[:, None]
    q = np.arange(128)[None, :]
    for hf, off in ((0, c), (1, 7 - c)):
        for jj in range(8):
            masks[:, hf, jj, :] = ((jj * 128 + k) < (off * 128 + q))
    rgp = np.zeros((512, 12), np.float32)
    rgp[:, 0:4] = inp["conv_w"][0].T
    rgp[:, 4] = inp["conv_b"][0]
    rgp[:, 5] = inp["rg_b_a"][0]
    rgp[:, 6] = inp["rg_b_x"][0]
    rgp[:, 7] = inp["rg_lambda"][0]
    rgp[:, 8] = inp["out_norm_rg"][0]
    rgp = rgp.reshape(4, 128, 12).transpose(1, 0, 2).reshape(128, 48)
    m = {
        "x_all": x,
        "x_own": np.ascontiguousarray(x[rows]),
        "sel": sel,
        "masks": masks.reshape(128, -1),
        "w_in": np.ascontiguousarray(inp["w_in"][0]),
        "nmix": np.ascontiguousarray(inp["norm_mix"][0].reshape(8, 128).T),
        "qk": np.concatenate([inp["q_norm"][0], inp["k_norm"][0]])[None, :].astype(np.float32),
        "rgp": np.ascontiguousarray(rgp),
        "rg_w_a": np.ascontiguousarray(inp["rg_w_a"][0]),
        "rg_w_x": np.ascontiguousarray(inp["rg_w_x"][0]),
        "gsb": np.ascontiguousarray(inp["out_norm_sb"][0].reshape(4, 128).T),
        "w_out": np.ascontiguousarray(inp["w_out"][0]),
        "nffn": np.ascontiguousarray(inp["norm_ffn"]),
        "wq": np.ascontiguousarray(inp["peer_w_query"][0].reshape(D, 2048)),
        "sk": np.ascontiguousarray(inp["peer_sub_keys"][0].reshape(16, 128, 128)),
        "peer_uv": inp["peer_uv"],
    }
    return m, rows


def kernel(**inputs):
    inp = {k: np.asarray(v, dtype=np.float32) for k, v in inputs.items()}
    inp["peer_uv"] = np.ascontiguousarray(np.concatenate([inp["peer_u"][0], inp["peer_v"][0]], axis=1))
    nc = build()
    in_maps, rows_all = [], []
    for c in range(8):
        m, rows = make_core_inputs(c, inp)
        in_maps.append(m)
        rows_all.append(rows)
    res = run_bass_kernel_spmd(nc, in_maps, core_ids=list(range(8)))
    out = np.zeros((1, S_LEN, D), np.float32)
    for c in range(8):
        out[0, rows_all[c]] = res.results[c]["y_own"]
    return out
```

```python
import numpy as np
from contextlib import ExitStack
import concourse.bass as bass
import concourse.mybir as mybir
from concourse.bass_utils import run_bass_kernel_spmd

F32 = mybir.dt.float32
BF16 = mybir.dt.bfloat16
U32 = mybir.dt.uint32
I32 = mybir.dt.int32
AF = mybir.ActivationFunctionType
ALU = mybir.AluOpType
AX = mybir.AxisListType

S_LEN = 16384
D = 1024
NB = 128
NSLOT = 16
EPS = 1e-6
GC = 0.7978845608028654


SAME_ENGINE_WAIT = True
NO_SELF_WAIT = set()


class Res:
    __slots__ = ("w", "r")

    def __init__(self):
        self.w = None
        self.r = {}


class Sch:
    CHUNK = 30000
    NDMA = 24

    def __init__(self, nc, stack):
        self.nc = nc
        self.stack = stack
        self.eng = {"pe": nc.tensor, "act": nc.scalar, "dve": nc.vector,
                    "pool": nc.gpsimd, "sp": nc.sync}
        self.sems = {}
        self.cnt = {k: 0 for k in self.eng}
        self.waited = {}
        self.dma_pool = {k: [] for k in self.eng}
        self.dma_rr = {k: 0 for k in self.eng}
        self.nsem = 0

    def _sem(self, key):
        s = self.sems.get(key)
        if s is None:
            s = self.stack.enter_context(self.nc.semaphore("s%d" % self.nsem))
            self.nsem += 1
            self.sems[key] = s
        return s

    def _wait(self, eng, deps):
        e = self.eng[eng]
        for key, val in deps.items():
            if key[0] == eng and (eng == "pe" or eng in NO_SELF_WAIT):
                continue
            k = (eng, key)
            if self.waited.get(k, 0) >= val:
                continue
            self.waited[k] = val
            e.wait_ge(self._sem(key), val)

    @staticmethod
    def _deps(reads, writes):
        deps = {}

        def add(k, v):
            if deps.get(k, 0) < v:
                deps[k] = v
        for r in reads:
            if r.w is not None:
                add(*r.w)
        for w in writes:
            if w.w is not None:
                add(*w.w)
            for k, v in w.r.items():
                add(k, v)
        return deps

    @staticmethod
    def _commit(tok, reads, writes):
        k, v = tok
        for r in reads:
            if r.r.get(k, 0) < v:
                r.r[k] = v
        for w in writes:
            w.w = tok
            w.r = {}

    def op(self, eng, fn, reads=(), writes=()):
        deps = self._deps(reads, writes)
        self._wait(eng, deps)
        ins = fn(self.eng[eng])
        n = self.cnt[eng]
        self.cnt[eng] = n + 1
        key = (eng, n // self.CHUNK)
        val = n % self.CHUNK + 1
        ins.then_inc(self._sem(key), 1)
        self._commit((key, val), reads, writes)
        return ins

    def dma(self, eng, fn, reads=(), writes=()):
        deps = self._deps(reads, writes)
        pool = self.dma_pool[eng]
        i = self.dma_rr[eng] % self.NDMA
        self.dma_rr[eng] += 1
        if i >= len(pool):
            pool.append([("dma", eng, len(pool)), 0])
            i = len(pool) - 1
        key, val = pool[i]
        if val > 0 and deps.get(key, 0) < val:
            deps[key] = val
        self._wait(eng, deps)
        ins = fn(self.eng[eng])
        val += 16
        pool[i][1] = val
        ins.then_inc(self._sem(key), 16)
        self._commit((key, val), reads, writes)
        return ins

    def barrier(self):
        deps = {}
        for e, pool in self.dma_pool.items():
            for key, val in pool:
                if val:
                    deps[key] = val
        for e, n in self.cnt.items():
            if n:
                deps[(e, (n - 1) // self.CHUNK)] = (n - 1) % self.CHUNK + 1
        for eng in self.eng:
            e = self.eng[eng]
            for key, val in deps.items():
                k = (eng, key)
                if self.waited.get(k, 0) >= val:
                    continue
                self.waited[k] = val
                e.wait_ge(self._sem(key), val)

    def finish(self, eng="sp"):
        deps = {}
        for e, pool in self.dma_pool.items():
            for key, val in pool:
                if val:
                    deps[key] = val
        for e, n in self.cnt.items():
            if n:
                deps[(e, (n - 1) // self.CHUNK)] = (n - 1) % self.CHUNK + 1
        e = self.eng[eng]
        for key, val in deps.items():
            e.wait_ge(self._sem(key), val)


class Buf:
    __slots__ = ("t", "r")

    def __init__(self, t):
        self.t = t
        self.r = Res()


class Rot:
    def __init__(self, bufs):
        self.bufs = bufs
        self.i = 0

    def next(self):
        b = self.bufs[self.i % len(self.bufs)]
        self.i += 1
        return b


def build(debug=None):
    nc = bass.Bass("TRN2", target_bir_lowering=False)

    def din(name, shape, dt=F32):
        return nc.dram_tensor(name, list(shape), dt, kind="ExternalInput").ap()

    x_all = din("x_all", [S_LEN, D])
    x_own = din("x_own", [2048, D])
    sel_d = din("sel", [128, NB])
    masks_d = din("masks", [128, 2 * 8 * 128])
    w_in = din("w_in", [D, 2560])
    nmix_d = din("nmix", [128, 8])
    qk_d = din("qk", [1, 128])
    rgp_d = din("rgp", [128, 4 * 12])
    rgwa_d = din("rg_w_a", [8, 64, 64])
    rgwx_d = din("rg_w_x", [8, 64, 64])
    gsb_d = din("gsb", [128, 4])
    w_out = din("w_out", [D, D])
    nffn_d = din("nffn", [1, D])
    wq_d = din("wq", [D, 2048])
    sk_d = din("sk", [16, 128, 128])
    puv_d = din("peer_uv", [16384, 2 * D])
    y_own = nc.dram_tensor("y_own", [2048, D], F32, kind="ExternalOutput").ap()
    KT_d = nc.dram_tensor("KT_scr", [4, 128, S_LEN], BF16, kind="Internal").ap()
    V_d = nc.dram_tensor("V_scr", [S_LEN, 512], BF16, kind="Internal").ap()
    UVb_d = nc.dram_tensor("UVb_scr", [16384, 2 * D], BF16, kind="Internal").ap()
    rKT = Res()
    rV = Res()
    dbg = {}
    if debug:
        for nm, shp in debug.items():
            if nm in ("nsb", "nheads", "nslots", "stop"):
                continue
            dbg[nm] = nc.dram_tensor(nm, list(shp), F32, kind="ExternalOutput").ap()

    with ExitStack() as top:
        S = Sch(nc, top)
        cnt = [0]

        def sbuf(st, shape, dt):
            cnt[0] += 1
            return Buf(st.enter_context(nc.sbuf_tensor("t%d" % cnt[0], list(shape), dt)))

        def psum(st, shape, dt):
            cnt[0] += 1
            return Buf(st.enter_context(nc.psum_tensor("p%d" % cnt[0], list(shape), dt)))

        dmaq = ["sp", "sp"]
        dq = [0]

        def ld(out, in_, reads, writes, q=None):
            if q is None:
                q = dmaq[dq[0] % 2]
                dq[0] += 1
            S.dma(q, lambda e: e.dma_start(out=out, in_=in_), reads, writes)

        ident_f = sbuf(top, [128, 128], F32)
        ident = sbuf(top, [128, 128], BF16)
        ones_b = sbuf(top, [128, 128], BF16)
        zeros_b = sbuf(top, [128, 512], BF16)
        ssrg = sbuf(top, [128, NSLOT], F32)
        sssb = sbuf(top, [128, NSLOT], F32)
        mixrg = sbuf(top, [128, 4, 2048], BF16)
        mixsb = sbuf(top, [128, 4, 2048], BF16)
        pab = top.enter_context(ExitStack())
        QT = sbuf(pab, [128, 4, 2048], BF16)

        S.op("pool", lambda e: e.memset(ident_f.t[:], 0.0), [], [ident_f.r])
        S.op("pool", lambda e: e.affine_select(out=ident_f.t[:], in_=ident_f.t[:], pattern=[[-1, 128]],
                                                compare_op=ALU.not_equal, fill=1.0, base=0,
                                                channel_multiplier=1), [ident_f.r], [ident_f.r])
        S.op("dve", lambda e: e.tensor_copy(out=ident.t[:], in_=ident_f.t[:]), [ident_f.r], [ident.r])
        S.op("pool", lambda e: e.memset(ones_b.t[:], 1.0), [], [ones_b.r])
        S.op("pool", lambda e: e.memset(zeros_b.t[:], 0.0), [], [zeros_b.r])
        S.op("pool", lambda e: e.memset(sssb.t[:], 0.0), [], [sssb.r])

        pbT = [psum(top, [128, 1024], BF16) for _ in range(2)]
        pbF = [psum(top, [128, 512], F32) for _ in range(6)]

        def rstd_from(ms_ap, out_ap, res_in, res_out, n, scale, tmp):
            S.op("dve", lambda e: e.tensor_scalar(out=tmp.t[:, 0:n], in0=ms_ap, scalar1=scale, scalar2=EPS,
                                                   op0=ALU.mult, op1=ALU.add), [res_in], [tmp.r])
            S.op("act", lambda e: e.activation(out=tmp.t[:, 0:n], in_=tmp.t[:, 0:n], func=AF.Ln), [tmp.r], [tmp.r])
            S.op("act", lambda e: e.activation(out=out_ap, in_=tmp.t[:, 0:n], func=AF.Exp, scale=-0.5),
                 [tmp.r], [res_out])

        with ExitStack() as pa:
            hso = sbuf(pa, [128, 4, 2048], F32)
            nmix = sbuf(pa, [128, 8], F32)
            ld(nmix.t[:], nmix_d, [], [nmix.r])
            w_in_v = w_in.rearrange("(dc p) e -> p dc e", p=128)

            def load_w(dst_list):
                with ExitStack() as ws:
                    stg = Rot([sbuf(ws, [128, 8, 512], F32) for _ in range(1)])
                    load_w_inner(stg, dst_list)
                S.barrier()

            def load_w_inner(stg, dst_list):
                for ci, (dst, dcol, scol) in enumerate(dst_list):
                    sg = stg.next()
                    ld(sg.t[:], w_in_v[:, :, scol:scol + 512], [], [sg.r])
                    for dc in range(8):
                        eng = "dve"
                        S.op(eng, lambda e: e.tensor_scalar(out=dst.t[:, dc, dcol:dcol + 512], in0=sg.t[:, dc, :],
                                                            scalar1=nmix.t[:, dc:dc + 1], scalar2=None,
                                                            op0=ALU.mult), [sg.r, nmix.r], [dst.r])
            qk = sbuf(pa, [128, 128], F32)
            ld(qk.t[:], qk_d.to_broadcast([128, 128]), [], [qk.r])
            gqk1 = sbuf(pa, [128, 64], F32)
            S.op("dve", lambda e: e.scalar_tensor_tensor(out=gqk1.t[:], in0=qk.t[:, 0:64], scalar=0.125,
                                                         in1=qk.t[:, 64:128], op0=ALU.mult, op1=ALU.mult),
                 [qk.r], [gqk1.r])
            gqk = sbuf(pa, [128, 8, 64], F32)
            S.op("dve", lambda e: e.tensor_copy(out=gqk.t[:], in_=gqk1.t[:].unsqueeze(1).to_broadcast([128, 8, 64])),
                 [gqk1.r], [gqk.r])
            rgp = sbuf(pa, [128, 4, 12], F32)
            ld(rgp.t[:].rearrange("p a b -> p (a b)"), rgp_d, [], [rgp.r])
            rgc = sbuf(pa, [128, 4, 4], F32)
            tmpc = sbuf(pa, [128, 4], F32)
            S.op("act", lambda e: e.activation(out=tmpc.t[:], in_=rgp.t[:, :, 7], func=AF.Exp, scale=-1.0),
                 [rgp.r], [tmpc.r])
            S.op("act", lambda e: e.activation(out=tmpc.t[:], in_=tmpc.t[:], func=AF.Ln, bias=1.0),
                 [tmpc.r], [tmpc.r])
            S.op("dve", lambda e: e.tensor_scalar(out=rgc.t[:, :, 0], in0=tmpc.t[:], scalar1=-8.0, scalar2=None,
                                                  op0=ALU.mult), [tmpc.r], [rgc.r])
            S.op("dve", lambda e: e.tensor_scalar(out=rgc.t[:, :, 1], in0=tmpc.t[:], scalar1=-16.0, scalar2=None,
                                                  op0=ALU.mult), [tmpc.r], [rgc.r])
            S.op("dve", lambda e: e.tensor_scalar(out=rgc.t[:, :, 2], in0=rgp.t[:, :, 5], scalar1=-1.0, scalar2=None,
                                                  op0=ALU.mult), [rgp.r], [rgc.r])
            S.op("dve", lambda e: e.tensor_scalar(out=rgc.t[:, :, 3], in0=rgp.t[:, :, 6], scalar1=-1.0, scalar2=None,
                                                  op0=ALU.mult), [rgp.r], [rgc.r])
            WaBD = sbuf(pa, [128, 4, 128], BF16)
            WxBD = sbuf(pa, [128, 4, 128], BF16)
            with ExitStack() as ws:
                for (dst, src) in ((WaBD, rgwa_d), (WxBD, rgwx_d)):
                    sg = sbuf(ws, [128, 4, 128], F32)
                    S.op("pool", lambda e: e.memset(sg.t[:], 0.0), [], [sg.r])
                    for ct in range(4):
                        ld(sg.t[0:64, ct, 0:64], src[2 * ct], [], [sg.r])
                        ld(sg.t[64:128, ct, 64:128], src[2 * ct + 1], [], [sg.r])
                    S.op("dve", lambda e: e.tensor_copy(out=dst.t[:], in_=sg.t[:]), [sg.r], [dst.r])
            S.barrier()
            sel = sbuf(pa, [128, NB], F32)
            ld(sel.t[:], sel_d, [], [sel.r])

            uvstg = Rot([sbuf(pa, [128, 2 * D], BF16) for _ in range(2)])
            uv_chunk = [0]

            def convert_uv(nchunks):
                for _ in range(nchunks):
                    c = uv_chunk[0]
                    if c >= 128:
                        return
                    uv_chunk[0] += 1
                    sg = uvstg.next()
                    S.dma("pool", lambda e: e.dma_start(out=sg.t[:], in_=puv_d[c * 128:(c + 1) * 128, :]), [], [sg.r])
                    ld(UVb_d[c * 128:(c + 1) * 128, :], sg.t[:], [sg.r], [])

            xt = Rot([sbuf(pa, [128, D], F32) for _ in range(3)])
            junk = sbuf(pa, [128, D], BF16)
            ss = Rot([sbuf(pa, [128, 1], F32) for _ in range(2)])
            rstd = Rot([sbuf(pa, [128, 1], F32) for _ in range(2)])
            tmp1 = Rot([sbuf(pa, [128, 8], F32) for _ in range(2)])
            tmp1k = Rot([sbuf(pa, [128, 8], F32) for _ in range(2)])
            xn = Rot([sbuf(pa, [128, D], BF16) for _ in range(2)])
            xnT = Rot([sbuf(pa, [128, 8, 512], BF16) for _ in range(2)])
            ksq = sbuf(pa, [128, 8, 64], F32)
            kms = Rot([sbuf(pa, [128, 8], F32) for _ in range(2)])
            ksc = Rot([sbuf(pa, [128, 8], F32) for _ in range(2)])
            kn = Rot([sbuf(pa, [128, 512], BF16) for _ in range(2)])

            pT, pT2 = pbT
            pK, pV, pX, pZa, pZi, pQ = pbF

            def P0(src_rows):
                x_ = xt.next()
                ld(x_.t[:], src_rows, [], [x_.r])
                return x_

            def P1a(x_):
                s_ = ss.next()
                S.op("act", lambda e: e.activation(out=junk.t[:], in_=x_.t[:], func=AF.Square, accum_out=s_.t[:]),
                     [x_.r], [s_.r])
                return s_

            def P1b(x_, s_):
                r_ = rstd.next()
                t_ = tmp1.next()
                rstd_from(s_.t[:], r_.t[:], s_.r, r_.r, 1, 1.0 / D, t_)
                n_ = xn.next()
                S.op("dve", lambda e: e.tensor_scalar(out=n_.t[:], in0=x_.t[:], scalar1=r_.t[:, 0:1], scalar2=None,
                                                      op0=ALU.mult), [x_.r, r_.r], [n_.r])
                return n_

            def P1(src_rows, x_=None):
                if x_ is None:
                    x_ = P0(src_rows)
                return P1b(x_, P1a(x_))

            def P2(n_, xnT_b, b):
                for dc in range(8):
                    S.op("pe", lambda e: e.transpose(out=pT.t[:, dc * 128:(dc + 1) * 128],
                                                     in_=n_.t[:, dc * 128:(dc + 1) * 128], identity=ident.t[:]),
                         [n_.r, ident.r], [pT.r])
                S.op("act", lambda e: e.activation(out=xnT_b.t[:, :, b * 128:(b + 1) * 128],
                                                   in_=pT.t[:].rearrange("p (a b) -> p a b", a=8), func=AF.Copy),
                     [pT.r], [xnT_b.r])

            def norm_block(src_rows, xnT_b, b):
                P2(P1(src_rows), xnT_b, b)

            def qk_norm_a(pk):
                S.op("act", lambda e: e.activation(out=ksq.t[:].rearrange("p a b -> p (a b)"), in_=pk.t[:],
                                                   func=AF.Square), [pk.r], [ksq.r])
                ms = kms.next()
                S.op("dve", lambda e: e.tensor_reduce(out=ms.t[:], in_=ksq.t[:], axis=AX.X, op=ALU.add),
                     [ksq.r], [ms.r])
                return ms

            def qk_norm_b(pk, ms, gains):
                sc = ksc.next()
                t_ = tmp1k.next()
                rstd_from(ms.t[:], sc.t[:], ms.r, sc.r, 8, 1.0 / 64, t_)
                k_ = kn.next()
                if gains:
                    S.op("dve", lambda e: e.tensor_tensor(out=ksq.t[:], in0=pk.t[:].rearrange("p (a b) -> p a b", a=8),
                                                          in1=sc.t[:].unsqueeze(2).to_broadcast([128, 8, 64]),
                                                          op=ALU.mult), [pk.r, sc.r], [ksq.r])
                    S.op("dve", lambda e: e.tensor_tensor(out=k_.t[:].rearrange("p (a b) -> p a b", a=8),
                                                          in0=ksq.t[:], in1=gqk.t[:], op=ALU.mult),
                         [ksq.r, gqk.r], [k_.r])
                else:
                    S.op("dve", lambda e: e.tensor_tensor(out=k_.t[:].rearrange("p (a b) -> p a b", a=8),
                                                          in0=pk.t[:].rearrange("p (a b) -> p a b", a=8),
                                                          in1=sc.t[:].unsqueeze(2).to_broadcast([128, 8, 64]),
                                                          op=ALU.mult), [pk.r, sc.r], [k_.r])
                return k_

            def qk_norm(pk, gains):
                return qk_norm_b(pk, qk_norm_a(pk), gains)

            pa1 = ExitStack()
            Wkvx = sbuf(pa1, [128, 8, 1536], BF16)
            load_w([(Wkvx, 0, 512), (Wkvx, 512, 1024), (Wkvx, 1024, 1536)])
            KTs = Rot([sbuf(pa1, [128, 4, 512], BF16) for _ in range(1)])
            Vs = Rot([sbuf(pa1, [128, 4, 512], BF16) for _ in range(2)])
            xr = [sbuf(pa1, [128, 515], F32) for _ in range(4)]
            hprev = [sbuf(pa1, [128, 1], F32) for _ in range(4)]
            for ct in range(4):
                S.op("pool", lambda e: e.memset(xr[ct].t[:, 0:3], 0.0), [], [xr[ct].r])
                S.op("pool", lambda e: e.memset(hprev[ct].t[:], 0.0), [], [hprev[ct].r])
            NR = 2
            ry = Rot([sbuf(pa1, [128, 512], F32) for _ in range(3)])
            ryb = Rot([sbuf(pa1, [128, 512], BF16) for _ in range(NR)])
            rea = Rot([sbuf(pa1, [128, 512], F32) for _ in range(NR)])
            rei = Rot([sbuf(pa1, [128, 512], F32) for _ in range(NR)])
            ra_ = Rot([sbuf(pa1, [128, 512], F32) for _ in range(NR)])
            rsq = Rot([sbuf(pa1, [128, 512], F32) for _ in range(NR)])
            rhs_ = Rot([sbuf(pa1, [128, 512], F32) for _ in range(NR)])

            n_sb = 32 if debug is None or "nsb" not in debug else debug["nsb"][0]
            NBLK = 4 * n_sb
            pKr = Rot([pK, pQ])
            sbst = {}
            blkst = {}
            rgst = {}

            def get_sb(sb):
                if sb not in sbst:
                    sbst[sb] = (xnT.next(), KTs.next(), Vs.next())
                return sbst[sb]

            def sP0(n):
                blkst[n] = {"x": P0(x_all[n * 128:(n + 1) * 128, :])}

            def sP1(n):
                blkst[n]["ss"] = P1a(blkst[n]["x"])

            def sP1b(n):
                blkst[n]["xn"] = P1b(blkst[n]["x"], blkst[n]["ss"])

            def sP2(n):
                xT_, kts, vs = get_sb(n // 4)
                P2(blkst[n]["xn"], xT_, n % 4)

            def sP3(n):
                sb, b = n // 4, n % 4
                xT_, kts, vs = get_sb(sb)
                pk = pKr.next()
                for dc in range(8):
                    S.op("pe", lambda e: e.matmul(pk.t[:], lhsT=xT_.t[:, dc, b * 128:(b + 1) * 128],
                                                  rhs=Wkvx.t[:, dc, 0:512], start=(dc == 0), stop=(dc == 7)),
                         [xT_.r, Wkvx.r], [pk.r])
                for dc in range(8):
                    S.op("pe", lambda e: e.matmul(pV.t[:], lhsT=xT_.t[:, dc, b * 128:(b + 1) * 128],
                                                  rhs=Wkvx.t[:, dc, 512:1024], start=(dc == 0), stop=(dc == 7)),
                         [xT_.r, Wkvx.r], [pV.r])
                blkst[n]["pk"] = pk
                blkst[n]["ms"] = qk_norm_a(pk)
                S.op("act", lambda e: e.activation(out=vs.t[:, b, :], in_=pV.t[:], func=AF.Copy), [pV.r], [vs.r])

            def sP3b(n):
                blkst[n]["kn"] = qk_norm_b(blkst[n]["pk"], blkst[n]["ms"], True)

            def sP4(n):
                sb, b = n // 4, n % 4
                xT_, kts, vs = get_sb(sb)
                k_ = blkst[n]["kn"]
                for hp in range(4):
                    S.op("pe", lambda e: e.transpose(out=pT2.t[:, hp * 128:(hp + 1) * 128],
                                                     in_=k_.t[:, hp * 128:(hp + 1) * 128], identity=ident.t[:]),
                         [k_.r, ident.r], [pT2.r])
                S.op("act", lambda e: e.activation(out=kts.t[:, :, b * 128:(b + 1) * 128],
                                                   in_=pT2.t[:, 0:512].rearrange("p (a b) -> p a b", a=4),
                                                   func=AF.Copy), [pT2.r], [kts.r])
                if b == 3:
                    for hp in range(4):
                        ld(KT_d[hp, :, sb * 512:(sb + 1) * 512], kts.t[:, hp, :], [kts.r], [rKT])
                    ld(V_d[sb * 512:(sb + 1) * 512, :].rearrange("(b p) e -> p b e", p=128), vs.t[:], [vs.r], [rV])
                    convert_uv(4)
                del blkst[n]

            def sR1(k):
                sb, ct = k // 4, k % 4
                xT_, kts, vs = get_sb(sb)
                for dc in range(8):
                    S.op("pe", lambda e: e.matmul(pX.t[:], lhsT=Wkvx.t[:, dc, 1024 + ct * 128:1024 + (ct + 1) * 128],
                                                  rhs=xT_.t[:, dc, :], start=(dc == 0), stop=(dc == 7)),
                         [xT_.r, Wkvx.r], [pX.r])
                xr_ = xr[ct]
                S.op("act", lambda e: e.activation(out=xr_.t[:, 3:515], in_=pX.t[:], func=AF.Copy),
                     [pX.r], [xr_.r])

            def sR1b(k):
                sb, ct = k // 4, k % 4
                xr_ = xr[ct]
                y_ = ry.next()
                S.op("dve", lambda e: e.tensor_scalar(out=y_.t[:], in0=xr_.t[:, 3:515], scalar1=rgp.t[:, ct, 3:4],
                                                      scalar2=rgp.t[:, ct, 4:5], op0=ALU.mult, op1=ALU.add),
                     [xr_.r, rgp.r], [y_.r])
                for j in range(3):
                    S.op("dve", lambda e: e.scalar_tensor_tensor(out=y_.t[:], in0=xr_.t[:, j:j + 512],
                                                                 scalar=rgp.t[:, ct, j:j + 1], in1=y_.t[:],
                                                                 op0=ALU.mult, op1=ALU.add),
                         [xr_.r, rgp.r, y_.r], [y_.r])
                S.op("pool", lambda e: e.tensor_copy(out=xr_.t[:, 0:3], in_=xr_.t[:, 512:515]), [xr_.r], [xr_.r])
                yb = ryb.next()
                S.op("pool", lambda e: e.tensor_copy(out=yb.t[:], in_=y_.t[:]), [y_.r], [yb.r])
                rgst[k] = {"y": y_, "yb": yb}

            def sR2(k):
                sb, ct = k // 4, k % 4
                st = rgst[k]
                yb = st["yb"]
                S.op("pe", lambda e: e.matmul(pZa.t[:], lhsT=WaBD.t[:, ct, :], rhs=yb.t[:], start=True, stop=True),
                     [yb.r, WaBD.r], [pZa.r])
                S.op("pe", lambda e: e.matmul(pZi.t[:], lhsT=WxBD.t[:, ct, :], rhs=yb.t[:], start=True, stop=True),
                     [yb.r, WxBD.r], [pZi.r])
                ea = rea.next()
                ei = rei.next()
                S.op("act", lambda e: e.activation(out=ea.t[:], in_=pZa.t[:], func=AF.Sigmoid,
                                                   bias=rgp.t[:, ct, 5:6]), [pZa.r, rgp.r], [ea.r])
                S.op("act", lambda e: e.activation(out=ei.t[:], in_=pZi.t[:], func=AF.Sigmoid,
                                                   bias=rgp.t[:, ct, 6:7]), [pZi.r, rgp.r], [ei.r])
                a_ = ra_.next()
                sq_ = rsq.next()
                S.op("act", lambda e: e.activation(out=a_.t[:], in_=ea.t[:], func=AF.Exp, scale=rgc.t[:, ct, 0:1]),
                     [ea.r, rgc.r], [a_.r])
                S.op("act", lambda e: e.activation(out=sq_.t[:], in_=ea.t[:], func=AF.Exp, scale=rgc.t[:, ct, 1:2]),
                     [ea.r, rgc.r], [sq_.r])
                S.op("act", lambda e: e.activation(out=sq_.t[:], in_=sq_.t[:], func=AF.Ln, scale=-1.0, bias=1.0),
                     [sq_.r], [sq_.r])
                S.op("act", lambda e: e.activation(out=sq_.t[:], in_=sq_.t[:], func=AF.Exp, scale=0.5),
                     [sq_.r], [sq_.r])
                st.update({"ei": ei, "a": a_, "sq": sq_})

            def sR3(k):
                sb, ct = k // 4, k % 4
                st = rgst.pop(k)
                ei, y_, sq_, a_ = st["ei"], st["y"], st["sq"], st["a"]
                b_ = ei
                S.op("dve", lambda e: e.tensor_tensor(out=b_.t[:], in0=ei.t[:], in1=y_.t[:], op=ALU.mult),
                     [ei.r, y_.r], [b_.r])
                S.op("dve", lambda e: e.tensor_tensor(out=b_.t[:], in0=b_.t[:], in1=sq_.t[:], op=ALU.mult),
                     [b_.r, sq_.r], [b_.r])
                h_ = rhs_.next()
                hp_ = hprev[ct]
                S.op("dve", lambda e: e.tensor_tensor_scan(out=h_.t[:], data0=a_.t[:], data1=b_.t[:],
                                                           initial=hp_.t[:, 0:1], op0=ALU.mult, op1=ALU.add),
                     [a_.r, b_.r, hp_.r], [h_.r])
                S.op("pool", lambda e: e.tensor_copy(out=hp_.t[:], in_=h_.t[:, 511:512]), [h_.r], [hp_.r])
                for b in range(4):
                    blk = 4 * sb + b
                    slot = blk // 8
                    dst = hso.t[:, ct, slot * 128:(slot + 1) * 128]
                    if blk % 8 == 0:
                        S.op("dve", lambda e: e.tensor_scalar(out=dst, in0=h_.t[:, b * 128:(b + 1) * 128],
                                                              scalar1=sel.t[:, blk:blk + 1], scalar2=None,
                                                              op0=ALU.mult), [h_.r, sel.r], [hso.r])
                    else:
                        S.op("dve", lambda e: e.scalar_tensor_tensor(out=dst, in0=h_.t[:, b * 128:(b + 1) * 128],
                                                                     scalar=sel.t[:, blk:blk + 1], in1=dst,
                                                                     op0=ALU.mult, op1=ALU.add),
                             [h_.r, sel.r, hso.r], [hso.r])
                if debug and "hs" in dbg and sb < dbg["hs"].shape[1] // 512:
                    ld(dbg["hs"][ct * 128:(ct + 1) * 128, sb * 512:(sb + 1) * 512], h_.t[:], [h_.r], [])

            stages = [(sP0, 0), (sP1, 1), (sP1b, 2), (sP2, 3), (sP3, 4), (sP3b, 5), (sP4, 6), (sR1, 7), (sR1b, 8), (sR2, 9),
                      (sR3, 10)]
            for i in range(NBLK + 11):
                for fn, lag in reversed(stages):
                    if 0 <= i - lag < NBLK:
                        fn(i - lag)

            convert_uv(128)
            pa1.close()
            S.barrier()
            Wqg = sbuf(pa, [128, 8, 1024], BF16)
            load_w([(Wqg, 0, 0), (Wqg, 512, 2048)])
            gl = Rot([sbuf(pa, [128, 512], F32) for _ in range(2)])
            gt = Rot([sbuf(pa, [128, 512], F32) for _ in range(2)])
            osq = Rot([sbuf(pa, [128, 512], BF16) for _ in range(2)])
            ost = {}
            osb_ = {}

            def get_og(g):
                if g not in osb_:
                    osb_[g] = xnT.next()
                return osb_[g]

            def oQ0(n):
                ost[n] = {"x": P0(x_own[n * 128:(n + 1) * 128, :])}

            def oQ1(n):
                ost[n]["ss"] = P1a(ost[n]["x"])

            def oQ1b(n):
                ost[n]["xn"] = P1b(ost[n]["x"], ost[n]["ss"])

            def oQ2(n):
                P2(ost[n]["xn"], get_og(n // 4), n % 4)

            def oQ3(n):
                xT_ = get_og(n // 4)
                b = n % 4
                for dc in range(8):
                    S.op("pe", lambda e: e.matmul(pQ.t[:], lhsT=xT_.t[:, dc, b * 128:(b + 1) * 128],
                                                  rhs=Wqg.t[:, dc, 0:512], start=(dc == 0), stop=(dc == 7)),
                         [xT_.r, Wqg.r], [pQ.r])
                ost[n]["ms"] = qk_norm_a(pQ)

            def oQ3b(n):
                ost[n]["q"] = qk_norm_b(pQ, ost[n]["ms"], False)

            def oQ4(n):
                slot = n
                q_ = ost.pop(n)["q"]
                for hp in range(4):
                    S.op("pe", lambda e: e.transpose(out=pT2.t[:, hp * 128:(hp + 1) * 128],
                                                     in_=q_.t[:, hp * 128:(hp + 1) * 128], identity=ident.t[:]),
                         [q_.r, ident.r], [pT2.r])
                S.op("act", lambda e: e.activation(out=QT.t[:, :, slot * 128:(slot + 1) * 128],
                                                   in_=pT2.t[:, 0:512].rearrange("p (a b) -> p a b", a=4),
                                                   func=AF.Copy), [pT2.r], [QT.r])

            def oG(k):
                g, ct = k // 4, k % 4
                xT_ = get_og(g)
                if ct == 0:
                    S.op("pe", lambda e: e.matmul(pZa.t[:, 0:4], lhsT=zeros_b.t[:, 0:128], rhs=zeros_b.t[:, 0:4],
                                                  start=True, stop=False), [zeros_b.r], [pZa.r])
                for dc in range(8):
                    S.op("pe", lambda e: e.matmul(pX.t[:], lhsT=Wqg.t[:, dc, 512 + ct * 128:512 + (ct + 1) * 128],
                                                  rhs=xT_.t[:, dc, :], start=(dc == 0), stop=(dc == 7)),
                         [xT_.r, Wqg.r], [pX.r])
                g_ = gl.next()
                t_ = gt.next()
                S.op("act", lambda e: e.activation(out=g_.t[:], in_=pX.t[:], func=AF.Copy), [pX.r], [g_.r])
                S.op("dve", lambda e: e.tensor_tensor(out=t_.t[:], in0=g_.t[:], in1=g_.t[:], op=ALU.mult),
                     [g_.r], [t_.r])
                S.op("dve", lambda e: e.tensor_scalar(out=t_.t[:], in0=t_.t[:], scalar1=0.044715, scalar2=1.0,
                                                      op0=ALU.mult, op1=ALU.add), [t_.r], [t_.r])
                S.op("dve", lambda e: e.tensor_tensor(out=t_.t[:], in0=t_.t[:], in1=g_.t[:], op=ALU.mult),
                     [t_.r, g_.r], [t_.r])
                S.op("act", lambda e: e.activation(out=t_.t[:], in_=t_.t[:], func=AF.Sigmoid, scale=2.0 * GC),
                     [t_.r], [t_.r])
                S.op("dve", lambda e: e.tensor_tensor(out=g_.t[:], in0=g_.t[:], in1=t_.t[:], op=ALU.mult),
                     [g_.r, t_.r], [g_.r])
                og = hso.t[:, ct, g * 512:(g + 1) * 512]
                S.op("dve", lambda e: e.tensor_tensor(out=og, in0=og, in1=g_.t[:], op=ALU.mult),
                     [hso.r, g_.r], [hso.r])
                sq_ = osq.next()
                S.op("dve", lambda e: e.tensor_tensor(out=sq_.t[:], in0=og, in1=og, op=ALU.mult),
                     [hso.r], [sq_.r])
                for b in range(4):
                    S.op("pe", lambda e: e.matmul(pZa.t[:, b:b + 1], lhsT=sq_.t[:, b * 128:(b + 1) * 128],
                                                  rhs=ones_b.t[:, 0:1], start=False, stop=(ct == 3 and b == 3)),
                         [sq_.r, ones_b.r], [pZa.r])
                S.op("dve", lambda e: e.tensor_scalar(out=mixrg.t[:, ct, g * 512:(g + 1) * 512], in0=og,
                                                      scalar1=rgp.t[:, ct, 8:9], scalar2=None, op0=ALU.mult),
                     [hso.r, rgp.r], [mixrg.r])
                if ct == 3:
                    S.op("dve", lambda e: e.tensor_copy(out=ssrg.t[:, 4 * g:4 * g + 4], in_=pZa.t[:, 0:4]),
                         [pZa.r], [ssrg.r])

            ostages = [(oQ0, 0), (oQ1, 1), (oQ1b, 2), (oQ2, 3), (oQ3, 4), (oQ3b, 5), (oQ4, 6), (oG, 8)]
            for i in range(NSLOT + 9):
                for fn, lag in reversed(ostages):
                    if 0 <= i - lag < NSLOT:
                        fn(i - lag)
            if debug and "org" in dbg:
                for ct in range(4):
                    ld(dbg["org"][ct * 128:(ct + 1) * 128, :], hso.t[:, ct, :], [hso.r], [])
            if debug and "ssrg" in dbg:
                ld(dbg["ssrg"], ssrg.t[:], [ssrg.r], [])

        S.barrier()
        if debug and debug.get("stop") == "A":
            S.finish()
            return nc

        with ExitStack() as pb:
            masks = sbuf(pb, [128, 2, 8, 128], F32)
            ld(masks.t[:].rearrange("p a b c -> p (a b c)"), masks_d, [], [masks.r])
            ntri_f = sbuf(pb, [128, 128], F32)
            ntri = sbuf(pb, [128, 128], BF16)
            nones = sbuf(pb, [128, 128], BF16)
            S.op("pool", lambda e: e.memset(ntri_f.t[:], -1.0), [], [ntri_f.r])
            S.op("pool", lambda e: e.affine_select(out=ntri_f.t[:], in_=ntri_f.t[:], pattern=[[-1, 128]],
                                                    compare_op=ALU.is_ge, fill=0.0, base=0, channel_multiplier=1),
                 [ntri_f.r], [ntri_f.r])
            S.op("dve", lambda e: e.tensor_copy(out=ntri.t[:], in_=ntri_f.t[:]), [ntri_f.r], [ntri.r])
            S.op("pool", lambda e: e.memset(nones.t[:], -1.0), [], [nones.r])
            gsb = sbuf(pb, [128, 4], F32)
            ld(gsb.t[:], gsb_d, [], [gsb.r])
            KTc2 = [[sbuf(pb, [128, 4096], BF16) for _ in range(4)] for _ in range(2)]
            Vc = [sbuf(pb, [128, 32, 128], BF16) for _ in range(4)]
            NW = 4
            be = Rot([sbuf(pb, [128, 512], F32) for _ in range(NW)])
            bsp = Rot([sbuf(pb, [128, 512], BF16) for _ in range(NW)])
            bwb = Rot([sbuf(pb, [128, 512], BF16) for _ in range(NW)])
            S32 = Rot([sbuf(pb, [128, 512], F32) for _ in range(2)])
            Sb = Rot([sbuf(pb, [128, 512], BF16) for _ in range(4)])
            osb = Rot([sbuf(pb, [128, 512], F32) for _ in range(2)])
            osq2 = Rot([sbuf(pb, [128, 512], BF16) for _ in range(2)])
            pZ = Rot([pbF[0], pbF[1], pbF[2], pbF[3]])
            pO = Rot([pbF[4], pbF[5]])
            pS = Buf(pbT[0].t[:].bitcast(F32))
            n_heads = 8 if not debug or "nheads" not in debug else debug["nheads"][0]

            class Step:
                pass
            steps = []
            for h in range(n_heads):
                for g in range(4):
                    jmax = 8 * (4 * g + 3) + 8
                    for j in range(jmax - 1, -1, -1):
                        st_ = Step()
                        st_.h, st_.g, st_.j = h, g, j
                        st_.first = (j == jmax - 1)
                        st_.last = (j == 0)
                        steps.append(st_)
            chain = {}

            def load_K(hp):
                KTc = KTc2[hp % 2]
                for c4 in range(4):
                    ld(KTc[c4].t[:], KT_d[hp, :, c4 * 4096:(c4 + 1) * 4096], [rKT], [KTc[c4].r])

            def load_V(hp):
                for c4 in range(4):
                    ld(Vc[c4].t[:],
                       V_d[c4 * 4096:(c4 + 1) * 4096, hp * 128:(hp + 1) * 128].rearrange("(j p) e -> p j e", p=128),
                       [rV], [Vc[c4].r])

            def geom(st_):
                h, g, j = st_.h, st_.g, st_.j
                s0 = max(4 * g, j // 8)
                c0 = (s0 - 4 * g) * 128
                msk = []
                for s in range(s0, 4 * g + 4):
                    if j >= 8 * s:
                        msk.append((slice((s - 4 * g) * 128, (s - 4 * g + 1) * 128), 0 if s < 8 else 1, j - 8 * s))
                return h // 2, h % 2, c0, msk

            def stageA(st_):
                h, g, j = st_.h, st_.g, st_.j
                hp, hh, c0, msk = geom(st_)
                p0 = hh * 64
                if st_.first and g == 0 and hh == 0 and hp == 0:
                    load_K(0)
                if st_.first and g == 0 and hh == 1 and hp + 1 < (n_heads + 1) // 2:
                    load_K(hp + 1)
                KTc = KTc2[hp % 2]
                if st_.first:
                    ch = Step()
                    ch.O = pO.next()
                    ch.S32 = S32.next()
                    ch.Sb = None
                    chain[(h, g)] = ch
                    S.op("pe", lambda e: e.matmul(ch.O.t[:, :], lhsT=zeros_b.t[:, 0:128], rhs=zeros_b.t[:, :],
                                                  start=True, stop=False), [zeros_b.r], [ch.O.r])
                    S.op("pool", lambda e: e.memset(ch.S32.t[:], 0.0), [], [ch.S32.r])
                st_.Z = pZ.next()
                kc = KTc[j // 32]
                jo = (j % 32) * 128
                qcols = slice(4 * g * 128 + c0, (4 * g + 4) * 128)
                S.op("pe", lambda e: e.matmul(st_.Z.t[:, c0:512], lhsT=kc.t[p0:p0 + 64, jo:jo + 128],
                                              rhs=QT.t[p0:p0 + 64, hp, qcols], start=True, stop=False),
                     [kc.r, QT.r], [st_.Z.r])
                st_.e = be.next()
                S.op("act", lambda e: e.activation(out=st_.e.t[:, c0:512], in_=st_.Z.t[:, c0:512], func=AF.Exp),
                     [st_.Z.r], [st_.e.r])

            def stageB(st_):
                hp, hh, c0, msk = geom(st_)
                st_.sp = bsp.next()
                S.op("act", lambda e: e.activation(out=st_.sp.t[:, c0:512], in_=st_.e.t[:, c0:512], func=AF.Ln,
                                                   bias=1.0), [st_.e.r], [st_.sp.r])
                for (cs, hf, jj) in msk:
                    S.op("pool", lambda e: e.tensor_tensor(out=st_.sp.t[:, cs], in0=st_.sp.t[:, cs],
                                                           in1=masks.t[:, hf, jj, :], op=ALU.mult),
                         [st_.sp.r, masks.r], [st_.sp.r])
                ch = chain[(st_.h, st_.g)]
                st_.Sb_in = ch.Sb
                st_.cp = getattr(ch, "c0_prev", None)
                if not st_.last:
                    sp = st_.sp
                    S.op("dve", lambda e: e.tensor_tensor(out=ch.S32.t[:, c0:512], in0=ch.S32.t[:, c0:512],
                                                          in1=sp.t[:, c0:512], op=ALU.add),
                         [ch.S32.r, sp.r], [ch.S32.r])
                    nsb_ = Sb.next()
                    S.op("dve", lambda e: e.tensor_copy(out=nsb_.t[:, c0:512], in_=ch.S32.t[:, c0:512]),
                         [ch.S32.r], [nsb_.r])
                    ch.Sb = nsb_
                    ch.c0_prev = c0

            def stageC(st_):
                h, g, j = st_.h, st_.g, st_.j
                hp, hh, c0, msk = geom(st_)
                p0 = hh * 64
                ch = chain[(h, g)]
                Z, sp = st_.Z, st_.sp
                has_carry = st_.Sb_in is not None
                S.op("pe", lambda e: e.matmul(Z.t[:, c0:512], lhsT=ntri.t[:], rhs=sp.t[:, c0:512],
                                              start=False, stop=not has_carry), [sp.r, ntri.r], [Z.r])
                if has_carry:
                    sbp = st_.Sb_in
                    cp = st_.cp
                    S.op("pe", lambda e: e.matmul(Z.t[:, cp:512], lhsT=nones.t[:], rhs=sbp.t[:, cp:512],
                                                  start=False, stop=True), [sbp.r, nones.r], [Z.r])
                wb = bwb.next()
                S.op("act", lambda e: e.activation(out=wb.t[:, c0:512], in_=Z.t[:, c0:512], func=AF.Exp),
                     [Z.r], [wb.r])
                for (cs, hf, jj) in msk:
                    S.op("pool", lambda e: e.tensor_tensor(out=wb.t[:, cs], in0=wb.t[:, cs],
                                                           in1=masks.t[:, hf, jj, :], op=ALU.mult),
                         [wb.r, masks.r], [wb.r])
                st_.wb = wb

            def stageD(st_):
                h, g, j = st_.h, st_.g, st_.j
                hp, hh, c0, msk = geom(st_)
                p0 = hh * 64
                ch = chain[(h, g)]
                wb = st_.wb
                if st_.first and g == 0 and hh == 0:
                    load_V(hp)
                vc = Vc[j // 32]
                MO = 64 * (hh + 1)
                O = ch.O
                S.op("pe", lambda e: e.matmul(O.t[0:MO, c0:512], lhsT=vc.t[:, j % 32, 0:MO], rhs=wb.t[:, c0:512],
                                              start=False, stop=st_.last), [vc.r, wb.r], [O.r])
                if st_.last:
                    o_ = osb.next()
                    S.op("act", lambda e: e.activation(out=o_.t[p0:p0 + 64, :], in_=O.t[p0:p0 + 64, :], func=AF.Copy),
                         [O.r], [o_.r])
                    S.op("dve", lambda e: e.tensor_scalar(out=mixsb.t[p0:p0 + 64, hp, g * 512:(g + 1) * 512],
                                                          in0=o_.t[p0:p0 + 64, :], scalar1=gsb.t[p0:p0 + 64, hp:hp + 1],
                                                          scalar2=None, op0=ALU.mult), [o_.r, gsb.r], [mixsb.r])
                    q2 = osq2.next()
                    S.op("dve", lambda e: e.tensor_tensor(out=q2.t[p0:p0 + 64, :], in0=o_.t[p0:p0 + 64, :],
                                                           in1=o_.t[p0:p0 + 64, :], op=ALU.mult), [o_.r], [q2.r])
                    for b in range(4):
                        S.op("pe", lambda e: e.matmul(pS.t[:, b:b + 1], lhsT=q2.t[p0:p0 + 64, b * 128:(b + 1) * 128],
                                                      rhs=ones_b.t[p0:p0 + 64, 0:1], start=True, stop=True),
                             [q2.r, ones_b.r], [pS.r])
                    S.op("dve", lambda e: e.tensor_tensor(out=sssb.t[:, 4 * g:4 * g + 4], in0=sssb.t[:, 4 * g:4 * g + 4],
                                                          in1=pS.t[:, 0:4], op=ALU.add), [sssb.r, pS.r], [sssb.r])
                    if debug and "osb" in dbg:
                        ld(dbg["osb"][h * 64:(h + 1) * 64, g * 512:(g + 1) * 512], o_.t[p0:p0 + 64, :], [o_.r], [])

            n = len(steps)
            for i in range(n + 3):
                if i < n:
                    stageA(steps[i])
                if 0 <= i - 1 < n:
                    stageB(steps[i - 1])
                if 0 <= i - 2 < n:
                    stageC(steps[i - 2])
                if 0 <= i - 3 < n:
                    stageD(steps[i - 3])

        pab.close()
        S.barrier()
        if debug and debug.get("stop") == "B":
            S.finish()
            return nc

        with ExitStack() as pc:
            Wo = sbuf(pc, [128, 8, D], BF16)
            Wq = sbuf(pc, [128, 8, 2048], BF16)
            SKT = sbuf(pc, [128, 16, 128], BF16)
            gffn = sbuf(pc, [128, D], F32)
            ld(gffn.t[:], nffn_d.to_broadcast([128, D]), [], [gffn.r])
            iota16 = sbuf(pc, [128, 16], F32)
            lo16 = sbuf(pc, [128, 16], F32)
            hi16 = sbuf(pc, [128, 16], F32)
            S.op("pool", lambda e: e.iota(iota16.t[:], pattern=[[1, 16]], base=0, channel_multiplier=0,
                                           allow_small_or_imprecise_dtypes=True), [], [iota16.r])
            S.op("dve", lambda e: e.tensor_scalar(out=lo16.t[:], in0=iota16.t[:], scalar1=16.0, scalar2=None,
                                                  op0=ALU.mult), [iota16.r], [lo16.r])
            S.op("dve", lambda e: e.tensor_scalar(out=hi16.t[:], in0=iota16.t[:], scalar1=16.0, scalar2=16.0,
                                                  op0=ALU.mult, op1=ALU.add), [iota16.r], [hi16.r])
            with ExitStack() as ws:
                wo_v = w_out.rearrange("(c p) e -> p c e", p=128)
                wq_v = wq_d.rearrange("(dc p) e -> p dc e", p=128)
                for half in range(2):
                    S.dma("pool", lambda e: e.dma_start(out=Wo.t[:, :, half * 512:(half + 1) * 512],
                                                        in_=wo_v[:, :, half * 512:(half + 1) * 512]), [], [Wo.r])
                for c4 in range(4):
                    S.dma("pool", lambda e: e.dma_start(out=Wq.t[:, :, c4 * 512:(c4 + 1) * 512],
                                                        in_=wq_v[:, :, c4 * 512:(c4 + 1) * 512]), [], [Wq.r])
                skf = sbuf(ws, [128, 16, 128], F32)
                skb = sbuf(ws, [128, 16, 128], BF16)
                ld(skf.t[:], sk_d.rearrange("a n k -> n a k"), [], [skf.r])
                S.op("dve", lambda e: e.tensor_copy(out=skb.t[:], in_=skf.t[:]), [skf.r], [skb.r])
                for half in range(2):
                    for a8 in range(8):
                        S.op("pe", lambda e: e.transpose(out=pbT[0].t[:, a8 * 128:(a8 + 1) * 128],
                                                         in_=skb.t[:, half * 8 + a8, :], identity=ident.t[:]),
                             [skb.r, ident.r], [pbT[0].r])
                    S.op("act", lambda e: e.activation(out=SKT.t[:, half * 8:(half + 1) * 8, :],
                                                       in_=pbT[0].t[:].rearrange("p (a b) -> p a b", a=8),
                                                       func=AF.Copy), [pbT[0].r], [SKT.r])
            S.barrier()

            rs_sb = sbuf(pc, [128, NSLOT], F32)
            rs_rg = sbuf(pc, [128, NSLOT], F32)
            tmp16 = sbuf(pc, [128, NSLOT], F32)
            rstd_from(sssb.t[:], rs_sb.t[:], sssb.r, rs_sb.r, NSLOT, 1.0 / 512, tmp16)
            rstd_from(ssrg.t[:], rs_rg.t[:], ssrg.r, rs_rg.r, NSLOT, 1.0 / 512, tmp16)

            x2 = Rot([sbuf(pc, [128, D], F32) for _ in range(2)])
            hqb_rot = Rot([sbuf(pc, [128, D], BF16) for _ in range(2)])
            hqT = sbuf(pc, [128, 8, 128], BF16)
            junk2 = sbuf(pc, [128, D], BF16)
            ss2 = sbuf(pc, [128, 1], F32)
            r2 = sbuf(pc, [128, 1], F32)
            t2 = sbuf(pc, [128, 8], F32)
            qb = sbuf(pc, [128, 2048], BF16)
            qT = sbuf(pc, [128, 16, 128], BF16)
            W1 = sbuf(pc, [128, 2048], F32)
            W2 = sbuf(pc, [128, 2048], F32)
            cand = sbuf(pc, [128, 8, 256], F32)
            sc3 = W1.t[:].rearrange("p (a n) -> p a n", a=16)
            scw3 = W2.t[:].rearrange("p (a n) -> p a n", a=16)
            candw3 = W2.t[:].rearrange("p (h n) -> p h n", h=8)
            oh3 = W1.t[:].rearrange("p (k a) -> p k a", a=16)
            oh4 = W1.t[:].rearrange("p (h k a) -> p h k a", h=8, a=16)
            oh2_3 = W2.t[:].rearrange("p (k a) -> p k a", a=16)
            tops = sbuf(pc, [128, 16, 16], F32)
            topi = sbuf(pc, [128, 16, 16], U32)
            topif = sbuf(pc, [128, 16, 16], F32)
            best = sbuf(pc, [128, 8, 16], F32)
            bpos = sbuf(pc, [128, 8, 16], U32)
            posf = sbuf(pc, [128, 128], F32)
            af = sbuf(pc, [128, 128], F32)
            bf = sbuf(pc, [128, 128], F32)
            i1f = sbuf(pc, [128, 128], F32)
            i2f = sbuf(pc, [128, 128], F32)
            idxf = sbuf(pc, [128, 128], F32)
            idx = Rot([sbuf(pc, [128, 128], I32) for _ in range(2)])
            gate = Rot([sbuf(pc, [128, 8, 16], F32) for _ in range(2)])
            gsum = sbuf(pc, [128, 8], F32)
            actv = sbuf(pc, [128, 128], F32)
            tg = sbuf(pc, [128, 128], F32)
            coef = Rot([sbuf(pc, [128, 128], F32) for _ in range(1)])
            JC = 2
            uvg = Rot([sbuf(pc, [128, 2 * D], BF16) for _ in range(11)])
            prod = Rot([sbuf(pc, [128, D], BF16) for _ in range(5)])
            dgr = Rot([sbuf(pc, [128, 128], BF16) for _ in range(4)])
            tgR = [Res() for _ in range(128 // JC)]
            actvR = [Res() for _ in range(128 // JC)]
            cfR = [Res() for _ in range(128 // JC)]
            accP = [pbF[2], pbF[3]]
            pP = [pbF[0], pbF[1], pbF[4], pbF[5]]
            pQ4 = [pbF[0], pbF[1], pbF[4], pbF[5]]
            pSc = [pbF[4], pbF[5], pbF[0], pbF[1]]
            n_slots = NSLOT if not debug or "nslots" not in debug else debug["nslots"][0]
            slot_state = {}

            def front(s):
                if True:
                    pass
                    ts = slice(s * 128, (s + 1) * 128)
                    x2_ = x2.next()
                    stt = {'x2': x2_}
                    gate_ = gate.next()
                    stt['gate'] = gate_
                    slot_state[s] = stt
                    ld(x2_.t[:], x_own[ts, :], [], [x2_.r])
                    yield
                    for half in range(2):
                        for c in range(4):
                            S.op("pe", lambda e: e.matmul(pP[half].t[:], lhsT=mixsb.t[:, c, ts],
                                                          rhs=Wo.t[:, c, half * 512:(half + 1) * 512],
                                                          start=(c == 0), stop=(c == 3)), [mixsb.r, Wo.r], [pP[half].r])
                            yield
                        for c in range(4):
                            S.op("pe", lambda e: e.matmul(pP[2 + half].t[:], lhsT=mixrg.t[:, c, ts],
                                                          rhs=Wo.t[:, 4 + c, half * 512:(half + 1) * 512],
                                                          start=(c == 0), stop=(c == 3)), [mixrg.r, Wo.r],
                                 [pP[2 + half].r])
                            yield
                    for half in range(2):
                        hs_ = slice(half * 512, (half + 1) * 512)
                        S.op("dve", lambda e: e.scalar_tensor_tensor(out=x2_.t[:, hs_], in0=pP[half].t[:],
                                                                     scalar=rs_sb.t[:, s:s + 1], in1=x2_.t[:, hs_],
                                                                     op0=ALU.mult, op1=ALU.add),
                             [pP[half].r, rs_sb.r, x2_.r], [x2_.r])
                        yield
                        S.op("dve", lambda e: e.scalar_tensor_tensor(out=x2_.t[:, hs_], in0=pP[2 + half].t[:],
                                                                     scalar=rs_rg.t[:, s:s + 1], in1=x2_.t[:, hs_],
                                                                     op0=ALU.mult, op1=ALU.add),
                             [pP[2 + half].r, rs_rg.r, x2_.r], [x2_.r])
                        yield
                    if debug and "x2" in dbg:
                        ld(dbg["x2"][ts, :], x2_.t[:], [x2_.r], [])
                        yield
                    S.op("act", lambda e: e.activation(out=junk2.t[:], in_=x2_.t[:], func=AF.Square, accum_out=ss2.t[:]),
                         [x2_.r], [ss2.r])
                    yield
                    rstd_from(ss2.t[:], r2.t[:], ss2.r, r2.r, 1, 1.0 / D, t2)
                    yield
                    hqb = hqb_rot.next()
                    stt['hqb'] = hqb
                    stt['hq'] = hqb
                    S.op("dve", lambda e: e.scalar_tensor_tensor(out=hqb.t[:], in0=x2_.t[:], scalar=r2.t[:, 0:1],
                                                                 in1=gffn.t[:], op0=ALU.mult, op1=ALU.mult),
                         [x2_.r, r2.r, gffn.r], [hqb.r])
                    yield
                    for dc in range(8):
                        S.op("pe", lambda e: e.transpose(out=pbT[0].t[:, dc * 128:(dc + 1) * 128],
                                                         in_=hqb.t[:, dc * 128:(dc + 1) * 128], identity=ident.t[:]),
                             [hqb.r, ident.r], [pbT[0].r])
                        yield
                    S.op("act", lambda e: e.activation(out=hqT.t[:], in_=pbT[0].t[:].rearrange("p (a b) -> p a b", a=8),
                                                       func=AF.Copy), [pbT[0].r], [hqT.r])
                    yield
                    for c4 in range(4):
                        for dc in range(8):
                            S.op("pe", lambda e: e.matmul(pQ4[c4].t[:], lhsT=hqT.t[:, dc, :],
                                                          rhs=Wq.t[:, dc, c4 * 512:(c4 + 1) * 512],
                                                          start=(dc == 0), stop=(dc == 7)), [hqT.r, Wq.r], [pQ4[c4].r])
                            yield
                        if c4 % 2 == 0:
                            S.op("act", lambda e: e.activation(out=qb.t[:, c4 * 512:(c4 + 1) * 512], in_=pQ4[c4].t[:],
                                                               func=AF.Copy), [pQ4[c4].r], [qb.r])
                            yield
                        else:
                            S.op("dve", lambda e: e.tensor_copy(out=qb.t[:, c4 * 512:(c4 + 1) * 512], in_=pQ4[c4].t[:]),
                                 [pQ4[c4].r], [qb.r])
                            yield
                    for half in range(2):
                        pt = pbT[half]
                        for a8 in range(8):
                            S.op("pe", lambda e: e.transpose(out=pt.t[:, a8 * 128:(a8 + 1) * 128],
                                                             in_=qb.t[:, (half * 8 + a8) * 128:(half * 8 + a8 + 1) * 128],
                                                             identity=ident.t[:]), [qb.r, ident.r], [pt.r])
                            yield
                        S.op("act", lambda e: e.activation(out=qT.t[:, half * 8:(half + 1) * 8, :],
                                                           in_=pt.t[:].rearrange("p (a b) -> p a b", a=8), func=AF.Copy),
                             [pt.r], [qT.r])
                        yield
                    for c4 in range(4):
                        for a4 in range(4):
                            hpi = c4 * 4 + a4
                            S.op("pe", lambda e: e.matmul(pSc[c4].t[:, a4 * 128:(a4 + 1) * 128], lhsT=qT.t[:, hpi, :],
                                                          rhs=SKT.t[:, hpi, :], start=True, stop=True),
                                 [qT.r, SKT.r], [pSc[c4].r])
                            yield
                        S.op("act", lambda e: e.activation(out=W1.t[:, c4 * 512:(c4 + 1) * 512], in_=pSc[c4].t[:],
                                                           func=AF.Copy), [pSc[c4].r], [W1.r])
                        yield
                    for a in range(16):
                        S.op("dve", lambda e: e.max(out=tops.t[:, a, 0:8], in_=sc3[:, a, :]), [W1.r], [tops.r])
                        yield
                        S.op("dve", lambda e: e.max_index(out=topi.t[:, a, 0:8], in_max=tops.t[:, a, 0:8],
                                                          in_values=sc3[:, a, :]), [W1.r, tops.r], [topi.r])
                        yield
                        S.op("dve", lambda e: e.match_replace(out=scw3[:, a, :], in_to_replace=tops.t[:, a, 0:8],
                                                              in_values=sc3[:, a, :], imm_value=-1e30),
                             [W1.r, tops.r], [W2.r])
                        yield
                        S.op("dve", lambda e: e.max(out=tops.t[:, a, 8:16], in_=scw3[:, a, :]), [W2.r], [tops.r])
                        yield
                        S.op("dve", lambda e: e.max_index(out=topi.t[:, a, 8:16], in_max=tops.t[:, a, 8:16],
                                                          in_values=scw3[:, a, :]), [W2.r, tops.r], [topi.r])
                        yield
                    S.op("dve", lambda e: e.tensor_copy(out=topif.t[:], in_=topi.t[:]), [topi.r], [topif.r])
                    yield
                    for h in range(8):
                        S.op("dve", lambda e: e.tensor_tensor(
                            out=cand.t[:, h, :].rearrange("p (a b) -> p a b", a=16),
                            in0=tops.t[:, 2 * h, :].unsqueeze(2).to_broadcast([128, 16, 16]),
                            in1=tops.t[:, 2 * h + 1, :].unsqueeze(1).to_broadcast([128, 16, 16]), op=ALU.add),
                            [tops.r], [cand.r])
                        yield
                    for h in range(8):
                        S.op("dve", lambda e: e.max(out=best.t[:, h, 0:8], in_=cand.t[:, h, :]), [cand.r], [best.r])
                        yield
                        S.op("dve", lambda e: e.max_index(out=bpos.t[:, h, 0:8], in_max=best.t[:, h, 0:8],
                                                          in_values=cand.t[:, h, :]), [cand.r, best.r], [bpos.r])
                        yield
                        S.op("dve", lambda e: e.match_replace(out=candw3[:, h, :], in_to_replace=best.t[:, h, 0:8],
                                                              in_values=cand.t[:, h, :], imm_value=-1e30),
                             [cand.r, best.r], [W2.r])
                        yield
                        S.op("dve", lambda e: e.max(out=best.t[:, h, 8:16], in_=candw3[:, h, :]), [W2.r], [best.r])
                        yield
                        S.op("dve", lambda e: e.max_index(out=bpos.t[:, h, 8:16], in_max=best.t[:, h, 8:16],
                                                          in_values=candw3[:, h, :]), [W2.r, best.r], [bpos.r])
                        yield
                    S.op("dve", lambda e: e.tensor_copy(out=posf.t[:], in_=bpos.t[:].rearrange("p h k -> p (h k)")),
                         [bpos.r], [posf.r])
                    yield
                    pos_b = posf.t[:].unsqueeze(2).to_broadcast([128, 128, 16])
                    S.op("dve", lambda e: e.tensor_tensor(out=oh3, in0=pos_b,
                                                          in1=lo16.t[:].unsqueeze(1).to_broadcast([128, 128, 16]),
                                                          op=ALU.is_ge), [posf.r, lo16.r], [W1.r])
                    yield
                    S.op("dve", lambda e: e.tensor_tensor(out=oh2_3, in0=pos_b,
                                                          in1=hi16.t[:].unsqueeze(1).to_broadcast([128, 128, 16]),
                                                          op=ALU.is_lt), [posf.r, hi16.r], [W2.r])
                    yield
                    S.op("dve", lambda e: e.tensor_tensor(out=W1.t[:], in0=W1.t[:], in1=W2.t[:], op=ALU.mult),
                         [W1.r, W2.r], [W1.r])
                    yield
                    S.op("dve", lambda e: e.tensor_tensor(out=oh2_3, in0=oh3,
                                                           in1=iota16.t[:].unsqueeze(1).to_broadcast([128, 128, 16]),
                                                           op=ALU.mult), [W1.r, iota16.r], [W2.r])
                    yield
                    S.op("dve", lambda e: e.tensor_reduce(out=af.t[:], in_=oh2_3, axis=AX.X, op=ALU.add), [W2.r], [af.r])
                    yield
                    for h in range(8):
                        S.op("dve", lambda e: e.tensor_tensor(
                            out=oh4[:, h, :, :], in0=oh4[:, h, :, :],
                            in1=topif.t[:, 2 * h, :].unsqueeze(1).to_broadcast([128, 16, 16]), op=ALU.mult),
                            [W1.r, topif.r], [W1.r])
                        yield
                    S.op("dve", lambda e: e.tensor_reduce(out=i1f.t[:], in_=oh3, axis=AX.X, op=ALU.add), [W1.r], [i1f.r])
                    yield
                    S.op("dve", lambda e: e.scalar_tensor_tensor(out=bf.t[:], in0=af.t[:], scalar=-16.0, in1=posf.t[:],
                                                                 op0=ALU.mult, op1=ALU.add), [af.r, posf.r], [bf.r])
                    yield
                    S.op("dve", lambda e: e.tensor_tensor(out=oh3, in0=bf.t[:].unsqueeze(2).to_broadcast([128, 128, 16]),
                                                          in1=iota16.t[:].unsqueeze(1).to_broadcast([128, 128, 16]),
                                                          op=ALU.is_equal), [bf.r, iota16.r], [W1.r])
                    yield
                    for h in range(8):
                        S.op("dve", lambda e: e.tensor_tensor(
                            out=oh4[:, h, :, :], in0=oh4[:, h, :, :],
                            in1=topif.t[:, 2 * h + 1, :].unsqueeze(1).to_broadcast([128, 16, 16]), op=ALU.mult),
                            [W1.r, topif.r], [W1.r])
                        yield
                    S.op("dve", lambda e: e.tensor_reduce(out=i2f.t[:], in_=oh3, axis=AX.X, op=ALU.add), [W1.r], [i2f.r])
                    yield
                    S.op("dve", lambda e: e.scalar_tensor_tensor(out=idxf.t[:], in0=i1f.t[:], scalar=128.0, in1=i2f.t[:],
                                                                 op0=ALU.mult, op1=ALU.add), [i1f.r, i2f.r], [idxf.r])
                    yield
                    idx_ = idx.next()
                    stt['idx'] = idx_
                    S.op("dve", lambda e: e.tensor_copy(out=idx_.t[:], in_=idxf.t[:]), [idxf.r], [idx_.r])
                    yield
                    if debug and "idx" in dbg:
                        ld(dbg["idx"][ts, :], idxf.t[:], [idxf.r], [])
                        yield
                    S.op("dve", lambda e: e.tensor_tensor(out=gate_.t[:], in0=best.t[:],
                                                          in1=best.t[:, :, 0:1].to_broadcast([128, 8, 16]),
                                                          op=ALU.subtract), [best.r], [gate_.r])
                    yield
                    S.op("act", lambda e: e.activation(out=gate_.t[:], in_=gate_.t[:], func=AF.Exp), [gate_.r], [gate_.r])
                    yield
                    S.op("dve", lambda e: e.tensor_reduce(out=gsum.t[:], in_=gate_.t[:], axis=AX.X, op=ALU.add),
                         [gate_.r], [gsum.r])
                    yield
                    S.op("dve", lambda e: e.reciprocal(out=gsum.t[:], in_=gsum.t[:]), [gsum.r], [gsum.r])
                    yield
                    S.op("dve", lambda e: e.tensor_tensor(out=gate_.t[:], in0=gate_.t[:],
                                                          in1=gsum.t[:].unsqueeze(2).to_broadcast([128, 8, 16]),
                                                          op=ALU.mult), [gate_.r, gsum.r], [gate_.r])
                    yield

            def experts(s, nxt):
                if True:
                    ts = slice(s * 128, (s + 1) * 128)
                    stt = slot_state[s]
                    x2_, hq_, idx_, gate_ = stt['x2'], stt['hq'], stt['idx'], stt['gate']
                    hqb = stt['hqb']
                    ac = x2_
                    cf = coef.next()
                    NG = 128 // JC
                    grp = [None] * NG

                    def acc_group(gi, first):
                        cs = slice(gi * JC, (gi + 1) * JC)
                        S.op("dve", lambda e: e.tensor_tensor(out=cf.t[:, cs], in0=tg.t[:, cs], in1=actv.t[:, cs],
                                                              op=ALU.mult), [tgR[gi], actvR[gi]], [cfR[gi]])
                        for jj in range(JC):
                            j = gi * JC + jj
                            b_ = grp[gi][jj]
                            dg_ = dgr.next()
                            S.op("act", lambda e: e.activation(out=dg_.t[:], in_=ident.t[:], func=AF.Copy,
                                                               scale=cf.t[:, j:j + 1]), [ident.r, cfR[gi]], [dg_.r])
                            for half in range(2):
                                S.op("pe", lambda e: e.matmul(accP[half].t[:], lhsT=dg_.t[:],
                                                              rhs=b_.t[:, D + half * 512:D + (half + 1) * 512],
                                                              start=(j == 0), stop=(j == 127)),
                                     [dg_.r, b_.r], [accP[half].r])

                    def gelu1(gi):
                        cs = slice(gi * JC, (gi + 1) * JC)
                        S.op("dve", lambda e: e.scalar_tensor_tensor(out=tg.t[:, cs], in0=actv.t[:, cs], scalar=0.044715,
                                                                     in1=actv.t[:, cs], op0=ALU.mult, op1=ALU.mult),
                             [actvR[gi]], [tgR[gi]])
                        S.op("dve", lambda e: e.scalar_tensor_tensor(out=tg.t[:, cs], in0=tg.t[:, cs], scalar=1.0,
                                                                     in1=actv.t[:, cs], op0=ALU.add, op1=ALU.mult),
                             [tgR[gi], actvR[gi]], [tgR[gi]])
                        S.op("act", lambda e: e.activation(out=tg.t[:, cs], in_=tg.t[:, cs], func=AF.Sigmoid,
                                                           scale=2.0 * GC), [tgR[gi]], [tgR[gi]])
                        S.op("dve", lambda e: e.tensor_tensor(out=actv.t[:, cs], in0=actv.t[:, cs],
                                                              in1=gate_.t[:].rearrange("p h k -> p (h k)")[:, cs],
                                                              op=ALU.mult), [actvR[gi], gate_.r], [actvR[gi]])

                    for gi in range(NG):
                        grp[gi] = [uvg.next() for _ in range(JC)]
                        cs = slice(gi * JC, (gi + 1) * JC)
                        for jj in range(JC):
                            j = gi * JC + jj
                            b_ = grp[gi][jj]
                            S.dma("pool", lambda e: e.indirect_dma_start(
                                out=b_.t[:, :], out_offset=None, in_=UVb_d,
                                in_offset=bass.IndirectOffsetOnAxis(ap=idx_.t[:, j:j + 1], axis=0)),
                                [idx_.r], [b_.r])
                        for jj in range(JC):
                            j = gi * JC + jj
                            b_ = grp[gi][jj]
                            pr_ = prod.next()
                            S.op("dve", lambda e: e.tensor_tensor(out=pr_.t[:], in0=b_.t[:, 0:D], in1=hqb.t[:], op=ALU.mult),
                                 [b_.r, hqb.r], [pr_.r])
                            S.op("act", lambda e: e.activation(out=pr_.t[:], in_=pr_.t[:], func=AF.Copy,
                                                               accum_out=actv.t[:, j:j + 1]), [pr_.r], [pr_.r, actvR[gi]])
                        if gi >= 1:
                            gelu1(gi - 1)
                        if gi >= 2:
                            acc_group(gi - 2, gi == 2)
                        if nxt is not None:
                            for _ in range(FRONT_PER_GROUP):
                                next(nxt, None)
                    gelu1(NG - 1)
                    acc_group(NG - 2, False)
                    acc_group(NG - 1, False)
                    for half in range(2):
                        hs_ = slice(half * 512, (half + 1) * 512)
                        S.op("dve", lambda e: e.tensor_tensor(out=ac.t[:, hs_], in0=accP[half].t[:], in1=x2_.t[:, hs_],
                                                              op=ALU.add), [accP[half].r, x2_.r], [ac.r])
                    ld(y_own[ts, :], ac.t[:], [ac.r], [])

            FRONT_PER_GROUP = 4
            for _ in front(0):
                pass
            for s in range(n_slots):
                nxt = front(s + 1) if s + 1 < n_slots else None
                experts(s, nxt)
                if nxt is not None:
                    for _ in nxt:
                        pass
        S.finish()
    return nc


def own_blocks(c):
    return [8 * s + (c if s < 8 else 7 - c) for s in range(NSLOT)]


def make_core_inputs(c, inp):
    x = np.ascontiguousarray(inp["x"][0])
    blocks = own_blocks(c)
    rows = np.concatenate([np.arange(b * 128, (b + 1) * 128) for b in blocks])
    sel = np.zeros((128, NB), np.float32)
    sel[:, blocks] = 1.0
    masks = np.zeros((128, 2, 8, 128), np.float32)
    k = np.arange(128)[:, None]
    q = np.arange(128)[None, :]
    for hf, off in ((0, c), (1, 7 - c)):
        for jj in range(8):
            masks[:, hf, jj, :] = ((jj * 128 + k) < (off * 128 + q))
    rgp = np.zeros((512, 12), np.float32)
    rgp[:, 0:4] = inp["conv_w"][0].T
    rgp[:, 4] = inp["conv_b"][0]
    rgp[:, 5] = inp["rg_b_a"][0]
    rgp[:, 6] = inp["rg_b_x"][0]
    rgp[:, 7] = inp["rg_lambda"][0]
    rgp[:, 8] = inp["out_norm_rg"][0]
    rgp = rgp.reshape(4, 128, 12).transpose(1, 0, 2).reshape(128, 48)
    m = {
        "x_all": x,
        "x_own": np.ascontiguousarray(x[rows]),
        "sel": sel,
        "masks": masks.reshape(128, -1),
        "w_in": np.ascontiguousarray(inp["w_in"][0]),
        "nmix": np.ascontiguousarray(inp["norm_mix"][0].reshape(8, 128).T),
        "qk": np.concatenate([inp["q_norm"][0], inp["k_norm"][0]])[None, :].astype(np.float32),
        "rgp": np.ascontiguousarray(rgp),
        "rg_w_a": np.ascontiguousarray(inp["rg_w_a"][0]),
        "rg_w_x": np.ascontiguousarray(inp["rg_w_x"][0]),
        "gsb": np.ascontiguousarray(inp["out_norm_sb"][0].reshape(4, 128).T),
        "w_out": np.ascontiguousarray(inp["w_out"][0]),
        "nffn": np.ascontiguousarray(inp["norm_ffn"]),
        "wq": np.ascontiguousarray(inp["peer_w_query"][0].reshape(D, 2048)),
        "sk": np.ascontiguousarray(inp["peer_sub_keys"][0].reshape(16, 128, 128)),
        "peer_uv": inp["peer_uv"],
    }
    return m, rows


def kernel(**inputs):
    inp = {k: np.asarray(v, dtype=np.float32) for k, v in inputs.items()}
    inp["peer_uv"] = np.ascontiguousarray(np.concatenate([inp["peer_u"][0], inp["peer_v"][0]], axis=1))
    nc = build()
    in_maps, rows_all = [], []
    for c in range(8):
        m, rows = make_core_inputs(c, inp)
        in_maps.append(m)
        rows_all.append(rows)
    res = run_bass_kernel_spmd(nc, in_maps, core_ids=list(range(8)))
    out = np.zeros((1, S_LEN, D), np.float32)
    for c in range(8):
        out[0, rows_all[c]] = res.results[c]["y_own"]
    return out
```

```python
import numpy as np
from contextlib import ExitStack
import concourse.bass as bass
import concourse.mybir as mybir
from concourse.bass_utils import run_bass_kernel_spmd

F32 = mybir.dt.float32
BF16 = mybir.dt.bfloat16
U32 = mybir.dt.uint32
I32 = mybir.dt.int32
AF = mybir.ActivationFunctionType
ALU = mybir.AluOpType
AX = mybir.AxisListType

S_LEN = 16384
D = 1024
NB = 128
NSLOT = 16
EPS = 1e-6
GC = 0.7978845608028654


SAME_ENGINE_WAIT = True
NO_SELF_WAIT = set()


class Res:
    __slots__ = ("w", "r")

    def __init__(self):
        self.w = None
        self.r = {}


class Sch:
    CHUNK = 30000
    NDMA = 24

    def __init__(self, nc, stack):
        self.nc = nc
        self.stack = stack
        self.eng = {"pe": nc.tensor, "act": nc.scalar, "dve": nc.vector,
                    "pool": nc.gpsimd, "sp": nc.sync}
        self.sems = {}
        self.cnt = {k: 0 for k in self.eng}
        self.waited = {}
        self.dma_pool = {k: [] for k in self.eng}
        self.dma_rr = {k: 0 for k in self.eng}
        self.nsem = 0

    def _sem(self, key):
        s = self.sems.get(key)
        if s is None:
            s = self.stack.enter_context(self.nc.semaphore("s%d" % self.nsem))
            self.nsem += 1
            self.sems[key] = s
        return s

    def _wait(self, eng, deps):
        e = self.eng[eng]
        for key, val in deps.items():
            if key[0] == eng and (eng == "pe" or eng in NO_SELF_WAIT):
                continue
            k = (eng, key)
            if self.waited.get(k, 0) >= val:
                continue
            self.waited[k] = val
            e.wait_ge(self._sem(key), val)

    @staticmethod
    def _deps(reads, writes):
        deps = {}

        def add(k, v):
            if deps.get(k, 0) < v:
                deps[k] = v
        for r in reads:
            if r.w is not None:
                add(*r.w)
        for w in writes:
            if w.w is not None:
                add(*w.w)
            for k, v in w.r.items():
                add(k, v)
        return deps

    @staticmethod
    def _commit(tok, reads, writes):
        k, v = tok
        for r in reads:
            if r.r.get(k, 0) < v:
                r.r[k] = v
        for w in writes:
            w.w = tok
            w.r = {}

    def op(self, eng, fn, reads=(), writes=()):
        deps = self._deps(reads, writes)
        self._wait(eng, deps)
        ins = fn(self.eng[eng])
        n = self.cnt[eng]
        self.cnt[eng] = n + 1
        key = (eng, n // self.CHUNK)
        val = n % self.CHUNK + 1
        ins.then_inc(self._sem(key), 1)
        self._commit((key, val), reads, writes)
        return ins

    def dma(self, eng, fn, reads=(), writes=()):
        deps = self._deps(reads, writes)
        pool = self.dma_pool[eng]
        i = self.dma_rr[eng] % self.NDMA
        self.dma_rr[eng] += 1
        if i >= len(pool):
            pool.append([("dma", eng, len(pool)), 0])
            i = len(pool) - 1
        key, val = pool[i]
        if val > 0 and deps.get(key, 0) < val:
            deps[key] = val
        self._wait(eng, deps)
        ins = fn(self.eng[eng])
        val += 16
        pool[i][1] = val
        ins.then_inc(self._sem(key), 16)
        self._commit((key, val), reads, writes)
        return ins

    def barrier(self):
        deps = {}
        for e, pool in self.dma_pool.items():
            for key, val in pool:
                if val:
                    deps[key] = val
        for e, n in self.cnt.items():
            if n:
                deps[(e, (n - 1) // self.CHUNK)] = (n - 1) % self.CHUNK + 1
        for eng in self.eng:
            e = self.eng[eng]
            for key, val in deps.items():
                k = (eng, key)
                if self.waited.get(k, 0) >= val:
                    continue
                self.waited[k] = val
                e.wait_ge(self._sem(key), val)

    def finish(self, eng="sp"):
        deps = {}
        for e, pool in self.dma_pool.items():
            for key, val in pool:
                if val:
                    deps[key] = val
        for e, n in self.cnt.items():
            if n:
                deps[(e, (n - 1) // self.CHUNK)] = (n - 1) % self.CHUNK + 1
        e = self.eng[eng]
        for key, val in deps.items():
            e.wait_ge(self._sem(key), val)


class Buf:
    __slots__ = ("t", "r")

    def __init__(self, t):
        self.t = t
        self.r = Res()


class Rot:
    def __init__(self, bufs):
        self.bufs = bufs
        self.i = 0

    def next(self):
        b = self.bufs[self.i % len(self.bufs)]
        self.i += 1
        return b


def build(debug=None):
    nc = bass.Bass("TRN2", target_bir_lowering=False)

    def din(name, shape, dt=F32):
        return nc.dram_tensor(name, list(shape), dt, kind="ExternalInput").ap()

    x_all = din("x_all", [S_LEN, D])
    x_own = din("x_own", [2048, D])
    sel_d = din("sel", [128, NB])
    masks_d = din("masks", [128, 2 * 8 * 128])
    w_in = din("w_in", [D, 2560])
    nmix_d = din("nmix", [128, 8])
    qk_d = din("qk", [1, 128])
    rgp_d = din("rgp", [128, 4 * 12])
    rgwa_d = din("rg_w_a", [8, 64, 64])
    rgwx_d = din("rg_w_x", [8, 64, 64])
    gsb_d = din("gsb", [128, 4])
    w_out = din("w_out", [D, D])
    nffn_d = din("nffn", [1, D])
    wq_d = din("wq", [D, 2048])
    sk_d = din("sk", [16, 128, 128])
    puv_d = din("peer_uv", [16384, 2 * D])
    y_own = nc.dram_tensor("y_own", [2048, D], F32, kind="ExternalOutput").ap()
    KT_d = nc.dram_tensor("KT_scr", [4, 128, S_LEN], BF16, kind="Internal").ap()
    V_d = nc.dram_tensor("V_scr", [S_LEN, 512], BF16, kind="Internal").ap()
    UVb_d = nc.dram_tensor("UVb_scr", [16384, 2 * D], BF16, kind="Internal").ap()
    rKT = Res()
    rV = Res()
    dbg = {}
    if debug:
        for nm, shp in debug.items():
            if nm in ("nsb", "nheads", "nslots", "stop"):
                continue
            dbg[nm] = nc.dram_tensor(nm, list(shp), F32, kind="ExternalOutput").ap()

    with ExitStack() as top:
        S = Sch(nc, top)
        cnt = [0]

        def sbuf(st, shape, dt):
            cnt[0] += 1
            return Buf(st.enter_context(nc.sbuf_tensor("t%d" % cnt[0], list(shape), dt)))

        def psum(st, shape, dt):
            cnt[0] += 1
            return Buf(st.enter_context(nc.psum_tensor("p%d" % cnt[0], list(shape), dt)))

        dmaq = ["sp", "sp"]
        dq = [0]

        def ld(out, in_, reads, writes, q=None):
            if q is None:
                q = dmaq[dq[0] % 2]
                dq[0] += 1
            S.dma(q, lambda e: e.dma_start(out=out, in_=in_), reads, writes)

        ident_f = sbuf(top, [128, 128], F32)
        ident = sbuf(top, [128, 128], BF16)
        ones_b = sbuf(top, [128, 128], BF16)
        zeros_b = sbuf(top, [128, 512], BF16)
        ssrg = sbuf(top, [128, NSLOT], F32)
        sssb = sbuf(top, [128, NSLOT], F32)
        mixrg = sbuf(top, [128, 4, 2048], BF16)
        mixsb = sbuf(top, [128, 4, 2048], BF16)
        pab = top.enter_context(ExitStack())
        QT = sbuf(pab, [128, 4, 2048], BF16)

        S.op("pool", lambda e: e.memset(ident_f.t[:], 0.0), [], [ident_f.r])
        S.op("pool", lambda e: e.affine_select(out=ident_f.t[:], in_=ident_f.t[:], pattern=[[-1, 128]],
                                                compare_op=ALU.not_equal, fill=1.0, base=0,
                                                channel_multiplier=1), [ident_f.r], [ident_f.r])
        S.op("dve", lambda e: e.tensor_copy(out=ident.t[:], in_=ident_f.t[:]), [ident_f.r], [ident.r])
        S.op("pool", lambda e: e.memset(ones_b.t[:], 1.0), [], [ones_b.r])
        S.op("pool", lambda e: e.memset(zeros_b.t[:], 0.0), [], [zeros_b.r])
        S.op("pool", lambda e: e.memset(sssb.t[:], 0.0), [], [sssb.r])

        pbT = [psum(top, [128, 1024], BF16) for _ in range(2)]
        pbF = [psum(top, [128, 512], F32) for _ in range(6)]

        def rstd_from(ms_ap, out_ap, res_in, res_out, n, scale, tmp):
            S.op("dve", lambda e: e.tensor_scalar(out=tmp.t[:, 0:n], in0=ms_ap, scalar1=scale, scalar2=EPS,
                                                   op0=ALU.mult, op1=ALU.add), [res_in], [tmp.r])
            S.op("act", lambda e: e.activation(out=tmp.t[:, 0:n], in_=tmp.t[:, 0:n], func=AF.Ln), [tmp.r], [tmp.r])
            S.op("act", lambda e: e.activation(out=out_ap, in_=tmp.t[:, 0:n], func=AF.Exp, scale=-0.5),
                 [tmp.r], [res_out])

        with ExitStack() as pa:
            hso = sbuf(pa, [128, 4, 2048], F32)
            nmix = sbuf(pa, [128, 8], F32)
            ld(nmix.t[:], nmix_d, [], [nmix.r])
            w_in_v = w_in.rearrange("(dc p) e -> p dc e", p=128)

            def load_w(dst_list):
                with ExitStack() as ws:
                    stg = Rot([sbuf(ws, [128, 8, 512], F32) for _ in range(1)])
                    load_w_inner(stg, dst_list)
                S.barrier()

            def load_w_inner(stg, dst_list):
                for ci, (dst, dcol, scol) in enumerate(dst_list):
                    sg = stg.next()
                    ld(sg.t[:], w_in_v[:, :, scol:scol + 512], [], [sg.r])
                    for dc in range(8):
                        eng = "dve"
                        S.op(eng, lambda e: e.tensor_scalar(out=dst.t[:, dc, dcol:dcol + 512], in0=sg.t[:, dc, :],
                                                            scalar1=nmix.t[:, dc:dc + 1], scalar2=None,
                                                            op0=ALU.mult), [sg.r, nmix.r], [dst.r])
            qk = sbuf(pa, [128, 128], F32)
            ld(qk.t[:], qk_d.to_broadcast([128, 128]), [], [qk.r])
            gqk1 = sbuf(pa, [128, 64], F32)
            S.op("dve", lambda e: e.scalar_tensor_tensor(out=gqk1.t[:], in0=qk.t[:, 0:64], scalar=0.125,
                                                         in1=qk.t[:, 64:128], op0=ALU.mult, op1=ALU.mult),
                 [qk.r], [gqk1.r])
            gqk = sbuf(pa, [128, 8, 64], F32)
            S.op("dve", lambda e: e.tensor_copy(out=gqk.t[:], in_=gqk1.t[:].unsqueeze(1).to_broadcast([128, 8, 64])),
                 [gqk1.r], [gqk.r])
            rgp = sbuf(pa, [128, 4, 12], F32)
            ld(rgp.t[:].rearrange("p a b -> p (a b)"), rgp_d, [], [rgp.r])
            rgc = sbuf(pa, [128, 4, 4], F32)
            tmpc = sbuf(pa, [128, 4], F32)
            S.op("act", lambda e: e.activation(out=tmpc.t[:], in_=rgp.t[:, :, 7], func=AF.Exp, scale=-1.0),
                 [rgp.r], [tmpc.r])
            S.op("act", lambda e: e.activation(out=tmpc.t[:], in_=tmpc.t[:], func=AF.Ln, bias=1.0),
                 [tmpc.r], [tmpc.r])
            S.op("dve", lambda e: e.tensor_scalar(out=rgc.t[:, :, 0], in0=tmpc.t[:], scalar1=-8.0, scalar2=None,
                                                  op0=ALU.mult), [tmpc.r], [rgc.r])
            S.op("dve", lambda e: e.tensor_scalar(out=rgc.t[:, :, 1], in0=tmpc.t[:], scalar1=-16.0, scalar2=None,
                                                  op0=ALU.mult), [tmpc.r], [rgc.r])
            S.op("dve", lambda e: e.tensor_scalar(out=rgc.t[:, :, 2], in0=rgp.t[:, :, 5], scalar1=-1.0, scalar2=None,
                                                  op0=ALU.mult), [rgp.r], [rgc.r])
            S.op("dve", lambda e: e.tensor_scalar(out=rgc.t[:, :, 3], in0=rgp.t[:, :, 6], scalar1=-1.0, scalar2=None,
                                                  op0=ALU.mult), [rgp.r], [rgc.r])
            WaBD = sbuf(pa, [128, 4, 128], BF16)
            WxBD = sbuf(pa, [128, 4, 128], BF16)
            with ExitStack() as ws:
                for (dst, src) in ((WaBD, rgwa_d), (WxBD, rgwx_d)):
                    sg = sbuf(ws, [128, 4, 128], F32)
                    S.op("pool", lambda e: e.memset(sg.t[:], 0.0), [], [sg.r])
                    for ct in range(4):
                        ld(sg.t[0:64, ct, 0:64], src[2 * ct], [], [sg.r])
                        ld(sg.t[64:128, ct, 64:128], src[2 * ct + 1], [], [sg.r])
                    S.op("dve", lambda e: e.tensor_copy(out=dst.t[:], in_=sg.t[:]), [sg.r], [dst.r])
            S.barrier()
            sel = sbuf(pa, [128, NB], F32)
            ld(sel.t[:], sel_d, [], [sel.r])

            uvstg = Rot([sbuf(pa, [128, 2 * D], BF16) for _ in range(2)])
            uv_chunk = [0]

            def convert_uv(nchunks):
                for _ in range(nchunks):
                    c = uv_chunk[0]
                    if c >= 128:
                        return
                    uv_chunk[0] += 1
                    sg = uvstg.next()
                    S.dma("pool", lambda e: e.dma_start(out=sg.t[:], in_=puv_d[c * 128:(c + 1) * 128, :]), [], [sg.r])
                    ld(UVb_d[c * 128:(c + 1) * 128, :], sg.t[:], [sg.r], [])

            xt = Rot([sbuf(pa, [128, D], F32) for _ in range(3)])
            junk = sbuf(pa, [128, D], BF16)
            ss = Rot([sbuf(pa, [128, 1], F32) for _ in range(2)])
            rstd = Rot([sbuf(pa, [128, 1], F32) for _ in range(2)])
            tmp1 = Rot([sbuf(pa, [128, 8], F32) for _ in range(2)])
            tmp1k = Rot([sbuf(pa, [128, 8], F32) for _ in range(2)])
            xn = Rot([sbuf(pa, [128, D], BF16) for _ in range(2)])
            xnT = Rot([sbuf(pa, [128, 8, 512], BF16) for _ in range(2)])
            ksq = sbuf(pa, [128, 8, 64], F32)
            kms = Rot([sbuf(pa, [128, 8], F32) for _ in range(2)])
            ksc = Rot([sbuf(pa, [128, 8], F32) for _ in range(2)])
            kn = Rot([sbuf(pa, [128, 512], BF16) for _ in range(2)])

            pT, pT2 = pbT
            pK, pV, pX, pZa, pZi, pQ = pbF

            def P0(src_rows):
                x_ = xt.next()
                ld(x_.t[:], src_rows, [], [x_.r])
                return x_

            def P1a(x_):
                s_ = ss.next()
                S.op("act", lambda e: e.activation(out=junk.t[:], in_=x_.t[:], func=AF.Square, accum_out=s_.t[:]),
                     [x_.r], [s_.r])
                return s_

            def P1b(x_, s_):
                r_ = rstd.next()
                t_ = tmp1.next()
                rstd_from(s_.t[:], r_.t[:], s_.r, r_.r, 1, 1.0 / D, t_)
                n_ = xn.next()
                S.op("dve", lambda e: e.tensor_scalar(out=n_.t[:], in0=x_.t[:], scalar1=r_.t[:, 0:1], scalar2=None,
                                                      op0=ALU.mult), [x_.r, r_.r], [n_.r])
                return n_

            def P1(src_rows, x_=None):
                if x_ is None:
                    x_ = P0(src_rows)
                return P1b(x_, P1a(x_))

            def P2(n_, xnT_b, b):
                for dc in range(8):
                    S.op("pe", lambda e: e.transpose(out=pT.t[:, dc * 128:(dc + 1) * 128],
                                                     in_=n_.t[:, dc * 128:(dc + 1) * 128], identity=ident.t[:]),
                         [n_.r, ident.r], [pT.r])
                S.op("act", lambda e: e.activation(out=xnT_b.t[:, :, b * 128:(b + 1) * 128],
                                                   in_=pT.t[:].rearrange("p (a b) -> p a b", a=8), func=AF.Copy),
                     [pT.r], [xnT_b.r])

            def norm_block(src_rows, xnT_b, b):
                P2(P1(src_rows), xnT_b, b)

            def qk_norm_a(pk):
                S.op("act", lambda e: e.activation(out=ksq.t[:].rearrange("p a b -> p (a b)"), in_=pk.t[:],
                                                   func=AF.Square), [pk.r], [ksq.r])
                ms = kms.next()
                S.op("dve", lambda e: e.tensor_reduce(out=ms.t[:], in_=ksq.t[:], axis=AX.X, op=ALU.add),
                     [ksq.r], [ms.r])
                return ms

            def qk_norm_b(pk, ms, gains):
                sc = ksc.next()
                t_ = tmp1k.next()
                rstd_from(ms.t[:], sc.t[:], ms.r, sc.r, 8, 1.0 / 64, t_)
                k_ = kn.next()
                if gains:
                    S.op("dve", lambda e: e.tensor_tensor(out=ksq.t[:], in0=pk.t[:].rearrange("p (a b) -> p a b", a=8),
                                                          in1=sc.t[:].unsqueeze(2).to_broadcast([128, 8, 64]),
                                                          op=ALU.mult), [pk.r, sc.r], [ksq.r])
                    S.op("dve", lambda e: e.tensor_tensor(out=k_.t[:].rearrange("p (a b) -> p a b", a=8),
                                                          in0=ksq.t[:], in1=gqk.t[:], op=ALU.mult),
                         [ksq.r, gqk.r], [k_.r])
                else:
                    S.op("dve", lambda e: e.tensor_tensor(out=k_.t[:].rearrange("p (a b) -> p a b", a=8),
                                                          in0=pk.t[:].rearrange("p (a b) -> p a b", a=8),
                                                          in1=sc.t[:].unsqueeze(2).to_broadcast([128, 8, 64]),
                                                          op=ALU.mult), [pk.r, sc.r], [k_.r])
                return k_

            def qk_norm(pk, gains):
                return qk_norm_b(pk, qk_norm_a(pk), gains)

            pa1 = ExitStack()
            Wkvx = sbuf(pa1, [128, 8, 1536], BF16)
            load_w([(Wkvx, 0, 512), (Wkvx, 512, 1024), (Wkvx, 1024, 1536)])
            KTs = Rot([sbuf(pa1, [128, 4, 512], BF16) for _ in range(1)])
            Vs = Rot([sbuf(pa1, [128, 4, 512], BF16) for _ in range(2)])
            xr = [sbuf(pa1, [128, 515], F32) for _ in range(4)]
            hprev = [sbuf(pa1, [128, 1], F32) for _ in range(4)]
            for ct in range(4):
                S.op("pool", lambda e: e.memset(xr[ct].t[:, 0:3], 0.0), [], [xr[ct].r])
                S.op("pool", lambda e: e.memset(hprev[ct].t[:], 0.0), [], [hprev[ct].r])
            NR = 2
            ry = Rot([sbuf(pa1, [128, 512], F32) for _ in range(3)])
            ryb = Rot([sbuf(pa1, [128, 512], BF16) for _ in range(NR)])
            rea = Rot([sbuf(pa1, [128, 512], F32) for _ in range(NR)])
            rei = Rot([sbuf(pa1, [128, 512], F32) for _ in range(NR)])
            ra_ = Rot([sbuf(pa1, [128, 512], F32) for _ in range(NR)])
            rsq = Rot([sbuf(pa1, [128, 512], F32) for _ in range(NR)])
            rhs_ = Rot([sbuf(pa1, [128, 512], F32) for _ in range(NR)])

            n_sb = 32 if debug is None or "nsb" not in debug else debug["nsb"][0]
            NBLK = 4 * n_sb
            pKr = Rot([pK, pQ])
            sbst = {}
            blkst = {}
            rgst = {}

            def get_sb(sb):
                if sb not in sbst:
                    sbst[sb] = (xnT.next(), KTs.next(), Vs.next())
                return sbst[sb]

            def sP0(n):
                blkst[n] = {"x": P0(x_all[n * 128:(n + 1) * 128, :])}

            def sP1(n):
                blkst[n]["ss"] = P1a(blkst[n]["x"])

            def sP1b(n):
                blkst[n]["xn"] = P1b(blkst[n]["x"], blkst[n]["ss"])

            def sP2(n):
                xT_, kts, vs = get_sb(n // 4)
                P2(blkst[n]["xn"], xT_, n % 4)

            def sP3(n):
                sb, b = n // 4, n % 4
                xT_, kts, vs = get_sb(sb)
                pk = pKr.next()
                for dc in range(8):
                    S.op("pe", lambda e: e.matmul(pk.t[:], lhsT=xT_.t[:, dc, b * 128:(b + 1) * 128],
                                                  rhs=Wkvx.t[:, dc, 0:512], start=(dc == 0), stop=(dc == 7)),
                         [xT_.r, Wkvx.r], [pk.r])
                for dc in range(8):
                    S.op("pe", lambda e: e.matmul(pV.t[:], lhsT=xT_.t[:, dc, b * 128:(b + 1) * 128],
                                                  rhs=Wkvx.t[:, dc, 512:1024], start=(dc == 0), stop=(dc == 7)),
                         [xT_.r, Wkvx.r], [pV.r])
                blkst[n]["pk"] = pk
                blkst[n]["ms"] = qk_norm_a(pk)
                S.op("act", lambda e: e.activation(out=vs.t[:, b, :], in_=pV.t[:], func=AF.Copy), [pV.r], [vs.r])

            def sP3b(n):
                blkst[n]["kn"] = qk_norm_b(blkst[n]["pk"], blkst[n]["ms"], True)

            def sP4(n):
                sb, b = n // 4, n % 4
                xT_, kts, vs = get_sb(sb)
                k_ = blkst[n]["kn"]
                for hp in range(4):
                    S.op("pe", lambda e: e.transpose(out=pT2.t[:, hp * 128:(hp + 1) * 128],
                                                     in_=k_.t[:, hp * 128:(hp + 1) * 128], identity=ident.t[:]),
                         [k_.r, ident.r], [pT2.r])
                S.op("act", lambda e: e.activation(out=kts.t[:, :, b * 128:(b + 1) * 128],
                                                   in_=pT2.t[:, 0:512].rearrange("p (a b) -> p a b", a=4),
                                                   func=AF.Copy), [pT2.r], [kts.r])
                if b == 3:
                    for hp in range(4):
                        ld(KT_d[hp, :, sb * 512:(sb + 1) * 512], kts.t[:, hp, :], [kts.r], [rKT])
                    ld(V_d[sb * 512:(sb + 1) * 512, :].rearrange("(b p) e -> p b e", p=128), vs.t[:], [vs.r], [rV])
                convert_uv(1)
                del blkst[n]

            def sR1(k):
                sb, ct = k // 4, k % 4
                xT_, kts, vs = get_sb(sb)
                for dc in range(8):
                    S.op("pe", lambda e: e.matmul(pX.t[:], lhsT=Wkvx.t[:, dc, 1024 + ct * 128:1024 + (ct + 1) * 128],
                                                  rhs=xT_.t[:, dc, :], start=(dc == 0), stop=(dc == 7)),
                         [xT_.r, Wkvx.r], [pX.r])
                xr_ = xr[ct]
                S.op("act", lambda e: e.activation(out=xr_.t[:, 3:515], in_=pX.t[:], func=AF.Copy),
                     [pX.r], [xr_.r])

            def sR1b(k):
                sb, ct = k // 4, k % 4
                xr_ = xr[ct]
                y_ = ry.next()
                S.op("dve", lambda e: e.tensor_scalar(out=y_.t[:], in0=xr_.t[:, 3:515], scalar1=rgp.t[:, ct, 3:4],
                                                      scalar2=rgp.t[:, ct, 4:5], op0=ALU.mult, op1=ALU.add),
                     [xr_.r, rgp.r], [y_.r])
                for j in range(3):
                    S.op("dve", lambda e: e.scalar_tensor_tensor(out=y_.t[:], in0=xr_.t[:, j:j + 512],
                                                                 scalar=rgp.t[:, ct, j:j + 1], in1=y_.t[:],
                                                                 op0=ALU.mult, op1=ALU.add),
                         [xr_.r, rgp.r, y_.r], [y_.r])
                S.op("pool", lambda e: e.tensor_copy(out=xr_.t[:, 0:3], in_=xr_.t[:, 512:515]), [xr_.r], [xr_.r])
                yb = ryb.next()
                S.op("pool", lambda e: e.tensor_copy(out=yb.t[:], in_=y_.t[:]), [y_.r], [yb.r])
                rgst[k] = {"y": y_, "yb": yb}

            def sR2(k):
                sb, ct = k // 4, k % 4
                st = rgst[k]
                yb = st["yb"]
                S.op("pe", lambda e: e.matmul(pZa.t[:], lhsT=WaBD.t[:, ct, :], rhs=yb.t[:], start=True, stop=True),
                     [yb.r, WaBD.r], [pZa.r])
                S.op("pe", lambda e: e.matmul(pZi.t[:], lhsT=WxBD.t[:, ct, :], rhs=yb.t[:], start=True, stop=True),
                     [yb.r, WxBD.r], [pZi.r])
                ea = rea.next()
                ei = rei.next()
                S.op("act", lambda e: e.activation(out=ea.t[:], in_=pZa.t[:], func=AF.Sigmoid,
                                                   bias=rgp.t[:, ct, 5:6]), [pZa.r, rgp.r], [ea.r])
                S.op("act", lambda e: e.activation(out=ei.t[:], in_=pZi.t[:], func=AF.Sigmoid,
                                                   bias=rgp.t[:, ct, 6:7]), [pZi.r, rgp.r], [ei.r])
                a_ = ra_.next()
                sq_ = rsq.next()
                S.op("act", lambda e: e.activation(out=a_.t[:], in_=ea.t[:], func=AF.Exp, scale=rgc.t[:, ct, 0:1]),
                     [ea.r, rgc.r], [a_.r])
                S.op("act", lambda e: e.activation(out=sq_.t[:], in_=ea.t[:], func=AF.Exp, scale=rgc.t[:, ct, 1:2]),
                     [ea.r, rgc.r], [sq_.r])
                S.op("act", lambda e: e.activation(out=sq_.t[:], in_=sq_.t[:], func=AF.Ln, scale=-1.0, bias=1.0),
                     [sq_.r], [sq_.r])
                S.op("act", lambda e: e.activation(out=sq_.t[:], in_=sq_.t[:], func=AF.Exp, scale=0.5),
                     [sq_.r], [sq_.r])
                st.update({"ei": ei, "a": a_, "sq": sq_})

            def sR3(k):
                sb, ct = k // 4, k % 4
                st = rgst.pop(k)
                ei, y_, sq_, a_ = st["ei"], st["y"], st["sq"], st["a"]
                b_ = ei
                S.op("dve", lambda e: e.tensor_tensor(out=b_.t[:], in0=ei.t[:], in1=y_.t[:], op=ALU.mult),
                     [ei.r, y_.r], [b_.r])
                S.op("dve", lambda e: e.tensor_tensor(out=b_.t[:], in0=b_.t[:], in1=sq_.t[:], op=ALU.mult),
                     [b_.r, sq_.r], [b_.r])
                h_ = rhs_.next()
                hp_ = hprev[ct]
                S.op("dve", lambda e: e.tensor_tensor_scan(out=h_.t[:], data0=a_.t[:], data1=b_.t[:],
                                                           initial=hp_.t[:, 0:1], op0=ALU.mult, op1=ALU.add),
                     [a_.r, b_.r, hp_.r], [h_.r])
                S.op("pool", lambda e: e.tensor_copy(out=hp_.t[:], in_=h_.t[:, 511:512]), [h_.r], [hp_.r])
                for b in range(4):
                    blk = 4 * sb + b
                    slot = blk // 8
                    dst = hso.t[:, ct, slot * 128:(slot + 1) * 128]
                    if blk % 8 == 0:
                        S.op("dve", lambda e: e.tensor_scalar(out=dst, in0=h_.t[:, b * 128:(b + 1) * 128],
                                                              scalar1=sel.t[:, blk:blk + 1], scalar2=None,
                                                              op0=ALU.mult), [h_.r, sel.r], [hso.r])
                    else:
                        S.op("dve", lambda e: e.scalar_tensor_tensor(out=dst, in0=h_.t[:, b * 128:(b + 1) * 128],
                                                                     scalar=sel.t[:, blk:blk + 1], in1=dst,
                                                                     op0=ALU.mult, op1=ALU.add),
                             [h_.r, sel.r, hso.r], [hso.r])
                if debug and "hs" in dbg and sb < dbg["hs"].shape[1] // 512:
                    ld(dbg["hs"][ct * 128:(ct + 1) * 128, sb * 512:(sb + 1) * 512], h_.t[:], [h_.r], [])

            stages = [(sP0, 0), (sP1, 1), (sP1b, 2), (sP2, 3), (sP3, 4), (sP3b, 5), (sP4, 6), (sR1, 7), (sR1b, 8), (sR2, 9),
                      (sR3, 10)]
            for i in range(NBLK + 11):
                for fn, lag in reversed(stages):
                    if 0 <= i - lag < NBLK:
                        fn(i - lag)

            convert_uv(128)
            pa1.close()
            S.barrier()
            Wqg = sbuf(pa, [128, 8, 1024], BF16)
            load_w([(Wqg, 0, 0), (Wqg, 512, 2048)])
            gl = Rot([sbuf(pa, [128, 512], F32) for _ in range(2)])
            gt = Rot([sbuf(pa, [128, 512], F32) for _ in range(2)])
            osq = Rot([sbuf(pa, [128, 512], BF16) for _ in range(2)])
            ost = {}
            osb_ = {}

            def get_og(g):
                if g not in osb_:
                    osb_[g] = xnT.next()
                return osb_[g]

            def oQ0(n):
                ost[n] = {"x": P0(x_own[n * 128:(n + 1) * 128, :])}

            def oQ1(n):
                ost[n]["ss"] = P1a(ost[n]["x"])

            def oQ1b(n):
                ost[n]["xn"] = P1b(ost[n]["x"], ost[n]["ss"])

            def oQ2(n):
                P2(ost[n]["xn"], get_og(n // 4), n % 4)

            def oQ3(n):
                xT_ = get_og(n // 4)
                b = n % 4
                for dc in range(8):
                    S.op("pe", lambda e: e.matmul(pQ.t[:], lhsT=xT_.t[:, dc, b * 128:(b + 1) * 128],
                                                  rhs=Wqg.t[:, dc, 0:512], start=(dc == 0), stop=(dc == 7)),
                         [xT_.r, Wqg.r], [pQ.r])
                ost[n]["ms"] = qk_norm_a(pQ)

            def oQ3b(n):
                ost[n]["q"] = qk_norm_b(pQ, ost[n]["ms"], False)

            def oQ4(n):
                slot = n
                q_ = ost.pop(n)["q"]
                for hp in range(4):
                    S.op("pe", lambda e: e.transpose(out=pT2.t[:, hp * 128:(hp + 1) * 128],
                                                     in_=q_.t[:, hp * 128:(hp + 1) * 128], identity=ident.t[:]),
                         [q_.r, ident.r], [pT2.r])
                S.op("act", lambda e: e.activation(out=QT.t[:, :, slot * 128:(slot + 1) * 128],
                                                   in_=pT2.t[:, 0:512].rearrange("p (a b) -> p a b", a=4),
                                                   func=AF.Copy), [pT2.r], [QT.r])

            def oG(k):
                g, ct = k // 4, k % 4
                xT_ = get_og(g)
                if ct == 0:
                    S.op("pe", lambda e: e.matmul(pZa.t[:, 0:4], lhsT=zeros_b.t[:, 0:128], rhs=zeros_b.t[:, 0:4],
                                                  start=True, stop=False), [zeros_b.r], [pZa.r])
                for dc in range(8):
                    S.op("pe", lambda e: e.matmul(pX.t[:], lhsT=Wqg.t[:, dc, 512 + ct * 128:512 + (ct + 1) * 128],
                                                  rhs=xT_.t[:, dc, :], start=(dc == 0), stop=(dc == 7)),
                         [xT_.r, Wqg.r], [pX.r])
                g_ = gl.next()
                t_ = gt.next()
                S.op("act", lambda e: e.activation(out=g_.t[:], in_=pX.t[:], func=AF.Copy), [pX.r], [g_.r])
                S.op("dve", lambda e: e.tensor_tensor(out=t_.t[:], in0=g_.t[:], in1=g_.t[:], op=ALU.mult),
                     [g_.r], [t_.r])
                S.op("dve", lambda e: e.tensor_scalar(out=t_.t[:], in0=t_.t[:], scalar1=0.044715, scalar2=1.0,
                                                      op0=ALU.mult, op1=ALU.add), [t_.r], [t_.r])
                S.op("dve", lambda e: e.tensor_tensor(out=t_.t[:], in0=t_.t[:], in1=g_.t[:], op=ALU.mult),
                     [t_.r, g_.r], [t_.r])
                S.op("act", lambda e: e.activation(out=t_.t[:], in_=t_.t[:], func=AF.Sigmoid, scale=2.0 * GC),
                     [t_.r], [t_.r])
                S.op("dve", lambda e: e.tensor_tensor(out=g_.t[:], in0=g_.t[:], in1=t_.t[:], op=ALU.mult),
                     [g_.r, t_.r], [g_.r])
                og = hso.t[:, ct, g * 512:(g + 1) * 512]
                S.op("dve", lambda e: e.tensor_tensor(out=og, in0=og, in1=g_.t[:], op=ALU.mult),
                     [hso.r, g_.r], [hso.r])
                sq_ = osq.next()
                S.op("dve", lambda e: e.tensor_tensor(out=sq_.t[:], in0=og, in1=og, op=ALU.mult),
                     [hso.r], [sq_.r])
                for b in range(4):
                    S.op("pe", lambda e: e.matmul(pZa.t[:, b:b + 1], lhsT=sq_.t[:, b * 128:(b + 1) * 128],
                                                  rhs=ones_b.t[:, 0:1], start=False, stop=(ct == 3 and b == 3)),
                         [sq_.r, ones_b.r], [pZa.r])
                S.op("dve", lambda e: e.tensor_scalar(out=mixrg.t[:, ct, g * 512:(g + 1) * 512], in0=og,
                                                      scalar1=rgp.t[:, ct, 8:9], scalar2=None, op0=ALU.mult),
                     [hso.r, rgp.r], [mixrg.r])
                if ct == 3:
                    S.op("dve", lambda e: e.tensor_copy(out=ssrg.t[:, 4 * g:4 * g + 4], in_=pZa.t[:, 0:4]),
                         [pZa.r], [ssrg.r])

            ostages = [(oQ0, 0), (oQ1, 1), (oQ1b, 2), (oQ2, 3), (oQ3, 4), (oQ3b, 5), (oQ4, 6), (oG, 8)]
            for i in range(NSLOT + 9):
                for fn, lag in reversed(ostages):
                    if 0 <= i - lag < NSLOT:
                        fn(i - lag)
            if debug and "org" in dbg:
                for ct in range(4):
                    ld(dbg["org"][ct * 128:(ct + 1) * 128, :], hso.t[:, ct, :], [hso.r], [])
            if debug and "ssrg" in dbg:
                ld(dbg["ssrg"], ssrg.t[:], [ssrg.r], [])

        S.barrier()
        if debug and debug.get("stop") == "A":
            S.finish()
            return nc

        with ExitStack() as pb:
            masks = sbuf(pb, [128, 2, 8, 128], F32)
            ld(masks.t[:].rearrange("p a b c -> p (a b c)"), masks_d, [], [masks.r])
            ntri_f = sbuf(pb, [128, 128], F32)
            ntri = sbuf(pb, [128, 128], BF16)
            nones = sbuf(pb, [128, 128], BF16)
            S.op("pool", lambda e: e.memset(ntri_f.t[:], -1.0), [], [ntri_f.r])
            S.op("pool", lambda e: e.affine_select(out=ntri_f.t[:], in_=ntri_f.t[:], pattern=[[-1, 128]],
                                                    compare_op=ALU.is_ge, fill=0.0, base=0, channel_multiplier=1),
                 [ntri_f.r], [ntri_f.r])
            S.op("dve", lambda e: e.tensor_copy(out=ntri.t[:], in_=ntri_f.t[:]), [ntri_f.r], [ntri.r])
            S.op("pool", lambda e: e.memset(nones.t[:], -1.0), [], [nones.r])
            gsb = sbuf(pb, [128, 4], F32)
            ld(gsb.t[:], gsb_d, [], [gsb.r])
            KTc2 = [[sbuf(pb, [128, 4096], BF16) for _ in range(4)] for _ in range(2)]
            Vc = [sbuf(pb, [128, 32, 128], BF16) for _ in range(4)]
            NW = 4
            be = Rot([sbuf(pb, [128, 512], F32) for _ in range(NW)])
            bsp = Rot([sbuf(pb, [128, 512], BF16) for _ in range(NW)])
            bwb = Rot([sbuf(pb, [128, 512], BF16) for _ in range(NW)])
            S32 = Rot([sbuf(pb, [128, 512], F32) for _ in range(2)])
            Sb = Rot([sbuf(pb, [128, 512], BF16) for _ in range(4)])
            osb = Rot([sbuf(pb, [128, 512], F32) for _ in range(2)])
            osq2 = Rot([sbuf(pb, [128, 512], BF16) for _ in range(2)])
            pZ = Rot([pbF[0], pbF[1], pbF[2], pbF[3]])
            pO = Rot([pbF[4], pbF[5]])
            pS = Buf(pbT[0].t[:].bitcast(F32))
            n_heads = 8 if not debug or "nheads" not in debug else debug["nheads"][0]

            class Step:
                pass
            steps = []
            for h in range(n_heads):
                for g in range(4):
                    jmax = 8 * (4 * g + 3) + 8
                    for j in range(jmax - 1, -1, -1):
                        st_ = Step()
                        st_.h, st_.g, st_.j = h, g, j
                        st_.first = (j == jmax - 1)
                        st_.last = (j == 0)
                        steps.append(st_)
            chain = {}

            def load_K(hp):
                KTc = KTc2[hp % 2]
                for c4 in range(4):
                    ld(KTc[c4].t[:], KT_d[hp, :, c4 * 4096:(c4 + 1) * 4096], [rKT], [KTc[c4].r])

            def load_V(hp):
                for c4 in range(4):
                    ld(Vc[c4].t[:],
                       V_d[c4 * 4096:(c4 + 1) * 4096, hp * 128:(hp + 1) * 128].rearrange("(j p) e -> p j e", p=128),
                       [rV], [Vc[c4].r])

            def geom(st_):
                h, g, j = st_.h, st_.g, st_.j
                s0 = max(4 * g, j // 8)
                c0 = (s0 - 4 * g) * 128
                msk = []
                for s in range(s0, 4 * g + 4):
                    if j >= 8 * s:
                        msk.append((slice((s - 4 * g) * 128, (s - 4 * g + 1) * 128), 0 if s < 8 else 1, j - 8 * s))
                return h // 2, h % 2, c0, msk

            def stageA(st_):
                h, g, j = st_.h, st_.g, st_.j
                hp, hh, c0, msk = geom(st_)
                p0 = hh * 64
                if st_.first and g == 0 and hh == 0 and hp == 0:
                    load_K(0)
                if st_.first and g == 0 and hh == 1 and hp + 1 < (n_heads + 1) // 2:
                    load_K(hp + 1)
                KTc = KTc2[hp % 2]
                if st_.first:
                    ch = Step()
                    ch.O = pO.next()
                    ch.S32 = S32.next()
                    ch.Sb = None
                    chain[(h, g)] = ch
                    S.op("pe", lambda e: e.matmul(ch.O.t[:, :], lhsT=zeros_b.t[:, 0:128], rhs=zeros_b.t[:, :],
                                                  start=True, stop=False), [zeros_b.r], [ch.O.r])
                    S.op("pool", lambda e: e.memset(ch.S32.t[:], 0.0), [], [ch.S32.r])
                st_.Z = pZ.next()
                kc = KTc[j // 32]
                jo = (j % 32) * 128
                qcols = slice(4 * g * 128 + c0, (4 * g + 4) * 128)
                S.op("pe", lambda e: e.matmul(st_.Z.t[:, c0:512], lhsT=kc.t[p0:p0 + 64, jo:jo + 128],
                                              rhs=QT.t[p0:p0 + 64, hp, qcols], start=True, stop=False),
                     [kc.r, QT.r], [st_.Z.r])
                st_.e = be.next()
                S.op("act", lambda e: e.activation(out=st_.e.t[:, c0:512], in_=st_.Z.t[:, c0:512], func=AF.Exp),
                     [st_.Z.r], [st_.e.r])

            def stageB(st_):
                hp, hh, c0, msk = geom(st_)
                st_.sp = bsp.next()
                S.op("act", lambda e: e.activation(out=st_.sp.t[:, c0:512], in_=st_.e.t[:, c0:512], func=AF.Ln,
                                                   bias=1.0), [st_.e.r], [st_.sp.r])
                for (cs, hf, jj) in msk:
                    S.op("pool", lambda e: e.tensor_tensor(out=st_.sp.t[:, cs], in0=st_.sp.t[:, cs],
                                                           in1=masks.t[:, hf, jj, :], op=ALU.mult),
                         [st_.sp.r, masks.r], [st_.sp.r])
                ch = chain[(st_.h, st_.g)]
                st_.Sb_in = ch.Sb
                st_.cp = getattr(ch, "c0_prev", None)
                if not st_.last:
                    sp = st_.sp
                    S.op("dve", lambda e: e.tensor_tensor(out=ch.S32.t[:, c0:512], in0=ch.S32.t[:, c0:512],
                                                          in1=sp.t[:, c0:512], op=ALU.add),
                         [ch.S32.r, sp.r], [ch.S32.r])
                    nsb_ = Sb.next()
                    S.op("dve", lambda e: e.tensor_copy(out=nsb_.t[:, c0:512], in_=ch.S32.t[:, c0:512]),
                         [ch.S32.r], [nsb_.r])
                    ch.Sb = nsb_
                    ch.c0_prev = c0

            def stageC(st_):
                h, g, j = st_.h, st_.g, st_.j
                hp, hh, c0, msk = geom(st_)
                p0 = hh * 64
                ch = chain[(h, g)]
                Z, sp = st_.Z, st_.sp
                has_carry = st_.Sb_in is not None
                S.op("pe", lambda e: e.matmul(Z.t[:, c0:512], lhsT=ntri.t[:], rhs=sp.t[:, c0:512],
                                              start=False, stop=not has_carry), [sp.r, ntri.r], [Z.r])
                if has_carry:
                    sbp = st_.Sb_in
                    cp = st_.cp
                    S.op("pe", lambda e: e.matmul(Z.t[:, cp:512], lhsT=nones.t[:], rhs=sbp.t[:, cp:512],
                                                  start=False, stop=True), [sbp.r, nones.r], [Z.r])
                wb = bwb.next()
                S.op("act", lambda e: e.activation(out=wb.t[:, c0:512], in_=Z.t[:, c0:512], func=AF.Exp),
                     [Z.r], [wb.r])
                for (cs, hf, jj) in msk:
                    S.op("pool", lambda e: e.tensor_tensor(out=wb.t[:, cs], in0=wb.t[:, cs],
                                                           in1=masks.t[:, hf, jj, :], op=ALU.mult),
                         [wb.r, masks.r], [wb.r])
                st_.wb = wb

            def stageD(st_):
                h, g, j = st_.h, st_.g, st_.j
                hp, hh, c0, msk = geom(st_)
                p0 = hh * 64
                ch = chain[(h, g)]
                wb = st_.wb
                if st_.first and g == 0 and hh == 0:
                    load_V(hp)
                vc = Vc[j // 32]
                MO = 64 * (hh + 1)
                O = ch.O
                S.op("pe", lambda e: e.matmul(O.t[0:MO, c0:512], lhsT=vc.t[:, j % 32, 0:MO], rhs=wb.t[:, c0:512],
                                              start=False, stop=st_.last), [vc.r, wb.r], [O.r])
                if st_.last:
                    o_ = osb.next()
                    S.op("act", lambda e: e.activation(out=o_.t[p0:p0 + 64, :], in_=O.t[p0:p0 + 64, :], func=AF.Copy),
                         [O.r], [o_.r])
                    S.op("dve", lambda e: e.tensor_scalar(out=mixsb.t[p0:p0 + 64, hp, g * 512:(g + 1) * 512],
                                                          in0=o_.t[p0:p0 + 64, :], scalar1=gsb.t[p0:p0 + 64, hp:hp + 1],
                                                          scalar2=None, op0=ALU.mult), [o_.r, gsb.r], [mixsb.r])
                    q2 = osq2.next()
                    S.op("dve", lambda e: e.tensor_tensor(out=q2.t[p0:p0 + 64, :], in0=o_.t[p0:p0 + 64, :],
                                                           in1=o_.t[p0:p0 + 64, :], op=ALU.mult), [o_.r], [q2.r])
                    for b in range(4):
                        S.op("pe", lambda e: e.matmul(pS.t[:, b:b + 1], lhsT=q2.t[p0:p0 + 64, b * 128:(b + 1) * 128],
                                                      rhs=ones_b.t[p0:p0 + 64, 0:1], start=True, stop=True),
                             [q2.r, ones_b.r], [pS.r])
                    S.op("dve", lambda e: e.tensor_tensor(out=sssb.t[:, 4 * g:4 * g + 4], in0=sssb.t[:, 4 * g:4 * g + 4],
                                                          in1=pS.t[:, 0:4], op=ALU.add), [sssb.r, pS.r], [sssb.r])
                    if debug and "osb" in dbg:
                        ld(dbg["osb"][h * 64:(h + 1) * 64, g * 512:(g + 1) * 512], o_.t[p0:p0 + 64, :], [o_.r], [])

            n = len(steps)
            for i in range(n + 3):
                if i < n:
                    stageA(steps[i])
                if 0 <= i - 1 < n:
                    stageB(steps[i - 1])
                if 0 <= i - 2 < n:
                    stageC(steps[i - 2])
                if 0 <= i - 3 < n:
                    stageD(steps[i - 3])

        pab.close()
        S.barrier()
        if debug and debug.get("stop") == "B":
            S.finish()
            return nc

        with ExitStack() as pc:
            Wo = sbuf(pc, [128, 8, D], BF16)
            Wq = sbuf(pc, [128, 8, 2048], BF16)
            SKT = sbuf(pc, [128, 16, 128], BF16)
            gffn = sbuf(pc, [128, D], F32)
            ld(gffn.t[:], nffn_d.to_broadcast([128, D]), [], [gffn.r])
            iota16 = sbuf(pc, [128, 16], F32)
            lo16 = sbuf(pc, [128, 16], F32)
            hi16 = sbuf(pc, [128, 16], F32)
            S.op("pool", lambda e: e.iota(iota16.t[:], pattern=[[1, 16]], base=0, channel_multiplier=0,
                                           allow_small_or_imprecise_dtypes=True), [], [iota16.r])
            S.op("dve", lambda e: e.tensor_scalar(out=lo16.t[:], in0=iota16.t[:], scalar1=16.0, scalar2=None,
                                                  op0=ALU.mult), [iota16.r], [lo16.r])
            S.op("dve", lambda e: e.tensor_scalar(out=hi16.t[:], in0=iota16.t[:], scalar1=16.0, scalar2=16.0,
                                                  op0=ALU.mult, op1=ALU.add), [iota16.r], [hi16.r])
            with ExitStack() as ws:
                wo_v = w_out.rearrange("(c p) e -> p c e", p=128)
                wq_v = wq_d.rearrange("(dc p) e -> p dc e", p=128)
                for half in range(2):
                    S.dma("pool", lambda e: e.dma_start(out=Wo.t[:, :, half * 512:(half + 1) * 512],
                                                        in_=wo_v[:, :, half * 512:(half + 1) * 512]), [], [Wo.r])
                for c4 in range(4):
                    S.dma("pool", lambda e: e.dma_start(out=Wq.t[:, :, c4 * 512:(c4 + 1) * 512],
                                                        in_=wq_v[:, :, c4 * 512:(c4 + 1) * 512]), [], [Wq.r])
                skf = sbuf(ws, [128, 16, 128], F32)
                skb = sbuf(ws, [128, 16, 128], BF16)
                ld(skf.t[:], sk_d.rearrange("a n k -> n a k"), [], [skf.r])
                S.op("dve", lambda e: e.tensor_copy(out=skb.t[:], in_=skf.t[:]), [skf.r], [skb.r])
                for half in range(2):
                    for a8 in range(8):
                        S.op("pe", lambda e: e.transpose(out=pbT[0].t[:, a8 * 128:(a8 + 1) * 128],
                                                         in_=skb.t[:, half * 8 + a8, :], identity=ident.t[:]),
                             [skb.r, ident.r], [pbT[0].r])
                    S.op("act", lambda e: e.activation(out=SKT.t[:, half * 8:(half + 1) * 8, :],
                                                       in_=pbT[0].t[:].rearrange("p (a b) -> p a b", a=8),
                                                       func=AF.Copy), [pbT[0].r], [SKT.r])
            S.barrier()

            rs_sb = sbuf(pc, [128, NSLOT], F32)
            rs_rg = sbuf(pc, [128, NSLOT], F32)
            tmp16 = sbuf(pc, [128, NSLOT], F32)
            rstd_from(sssb.t[:], rs_sb.t[:], sssb.r, rs_sb.r, NSLOT, 1.0 / 512, tmp16)
            rstd_from(ssrg.t[:], rs_rg.t[:], ssrg.r, rs_rg.r, NSLOT, 1.0 / 512, tmp16)

            x2 = Rot([sbuf(pc, [128, D], F32) for _ in range(2)])
            hqb_rot = Rot([sbuf(pc, [128, D], BF16) for _ in range(2)])
            hqT = sbuf(pc, [128, 8, 128], BF16)
            junk2 = sbuf(pc, [128, D], BF16)
            ss2 = sbuf(pc, [128, 1], F32)
            r2 = sbuf(pc, [128, 1], F32)
            t2 = sbuf(pc, [128, 8], F32)
            qb = sbuf(pc, [128, 2048], BF16)
            qT = sbuf(pc, [128, 16, 128], BF16)
            W1 = sbuf(pc, [128, 2048], F32)
            W2 = sbuf(pc, [128, 2048], F32)
            cand = sbuf(pc, [128, 8, 256], F32)
            sc3 = W1.t[:].rearrange("p (a n) -> p a n", a=16)
            scw3 = W2.t[:].rearrange("p (a n) -> p a n", a=16)
            candw3 = W2.t[:].rearrange("p (h n) -> p h n", h=8)
            oh3 = W1.t[:].rearrange("p (k a) -> p k a", a=16)
            oh4 = W1.t[:].rearrange("p (h k a) -> p h k a", h=8, a=16)
            oh2_3 = W2.t[:].rearrange("p (k a) -> p k a", a=16)
            tops = sbuf(pc, [128, 16, 16], F32)
            topi = sbuf(pc, [128, 16, 16], U32)
            topif = sbuf(pc, [128, 16, 16], F32)
            best = sbuf(pc, [128, 8, 16], F32)
            bpos = sbuf(pc, [128, 8, 16], U32)
            posf = sbuf(pc, [128, 128], F32)
            af = sbuf(pc, [128, 128], F32)
            bf = sbuf(pc, [128, 128], F32)
            i1f = sbuf(pc, [128, 128], F32)
            i2f = sbuf(pc, [128, 128], F32)
            idxf = sbuf(pc, [128, 128], F32)
            idx = Rot([sbuf(pc, [128, 128], I32) for _ in range(2)])
            gate = Rot([sbuf(pc, [128, 8, 16], F32) for _ in range(2)])
            gsum = sbuf(pc, [128, 8], F32)
            actv = sbuf(pc, [128, 128], F32)
            tg = sbuf(pc, [128, 128], F32)
            coef = Rot([sbuf(pc, [128, 128], F32) for _ in range(1)])
            JC = 2
            uvg = Rot([sbuf(pc, [128, 2 * D], BF16) for _ in range(11)])
            prod = Rot([sbuf(pc, [128, D], BF16) for _ in range(5)])
            dgr = Rot([sbuf(pc, [128, 128], BF16) for _ in range(4)])
            tgR = [Res() for _ in range(128 // JC)]
            actvR = [Res() for _ in range(128 // JC)]
            cfR = [Res() for _ in range(128 // JC)]
            accP = [pbF[2], pbF[3]]
            pP = [pbF[0], pbF[1], pbF[4], pbF[5]]
            pQ4 = [pbF[0], pbF[1], pbF[4], pbF[5]]
            pSc = [pbF[4], pbF[5], pbF[0], pbF[1]]
            n_slots = NSLOT if not debug or "nslots" not in debug else debug["nslots"][0]
            slot_state = {}

            def front(s):
                if True:
                    pass
                    ts = slice(s * 128, (s + 1) * 128)
                    x2_ = x2.next()
                    stt = {'x2': x2_}
                    gate_ = gate.next()
                    stt['gate'] = gate_
                    slot_state[s] = stt
                    ld(x2_.t[:], x_own[ts, :], [], [x2_.r])
                    yield
                    for half in range(2):
                        for c in range(4):
                            S.op("pe", lambda e: e.matmul(pP[half].t[:], lhsT=mixsb.t[:, c, ts],
                                                          rhs=Wo.t[:, c, half * 512:(half + 1) * 512],
                                                          start=(c == 0), stop=(c == 3)), [mixsb.r, Wo.r], [pP[half].r])
                            yield
                        for c in range(4):
                            S.op("pe", lambda e: e.matmul(pP[2 + half].t[:], lhsT=mixrg.t[:, c, ts],
                                                          rhs=Wo.t[:, 4 + c, half * 512:(half + 1) * 512],
                                                          start=(c == 0), stop=(c == 3)), [mixrg.r, Wo.r],
                                 [pP[2 + half].r])
                            yield
                    for half in range(2):
                        hs_ = slice(half * 512, (half + 1) * 512)
                        S.op("dve", lambda e: e.scalar_tensor_tensor(out=x2_.t[:, hs_], in0=pP[half].t[:],
                                                                     scalar=rs_sb.t[:, s:s + 1], in1=x2_.t[:, hs_],
                                                                     op0=ALU.mult, op1=ALU.add),
                             [pP[half].r, rs_sb.r, x2_.r], [x2_.r])
                        yield
                        S.op("dve", lambda e: e.scalar_tensor_tensor(out=x2_.t[:, hs_], in0=pP[2 + half].t[:],
                                                                     scalar=rs_rg.t[:, s:s + 1], in1=x2_.t[:, hs_],
                                                                     op0=ALU.mult, op1=ALU.add),
                             [pP[2 + half].r, rs_rg.r, x2_.r], [x2_.r])
                        yield
                    if debug and "x2" in dbg:
                        ld(dbg["x2"][ts, :], x2_.t[:], [x2_.r], [])
                        yield
                    S.op("act", lambda e: e.activation(out=junk2.t[:], in_=x2_.t[:], func=AF.Square, accum_out=ss2.t[:]),
                         [x2_.r], [ss2.r])
                    yield
                    rstd_from(ss2.t[:], r2.t[:], ss2.r, r2.r, 1, 1.0 / D, t2)
                    yield
                    hqb = hqb_rot.next()
                    stt['hqb'] = hqb
                    stt['hq'] = hqb
                    S.op("dve", lambda e: e.scalar_tensor_tensor(out=hqb.t[:], in0=x2_.t[:], scalar=r2.t[:, 0:1],
                                                                 in1=gffn.t[:], op0=ALU.mult, op1=ALU.mult),
                         [x2_.r, r2.r, gffn.r], [hqb.r])
                    yield
                    for dc in range(8):
                        S.op("pe", lambda e: e.transpose(out=pbT[0].t[:, dc * 128:(dc + 1) * 128],
                                                         in_=hqb.t[:, dc * 128:(dc + 1) * 128], identity=ident.t[:]),
                             [hqb.r, ident.r], [pbT[0].r])
                        yield
                    S.op("act", lambda e: e.activation(out=hqT.t[:], in_=pbT[0].t[:].rearrange("p (a b) -> p a b", a=8),
                                                       func=AF.Copy), [pbT[0].r], [hqT.r])
                    yield
                    for c4 in range(4):
                        for dc in range(8):
                            S.op("pe", lambda e: e.matmul(pQ4[c4].t[:], lhsT=hqT.t[:, dc, :],
                                                          rhs=Wq.t[:, dc, c4 * 512:(c4 + 1) * 512],
                                                          start=(dc == 0), stop=(dc == 7)), [hqT.r, Wq.r], [pQ4[c4].r])
                            yield
                        if c4 % 2 == 0:
                            S.op("act", lambda e: e.activation(out=qb.t[:, c4 * 512:(c4 + 1) * 512], in_=pQ4[c4].t[:],
                                                               func=AF.Copy), [pQ4[c4].r], [qb.r])
                            yield
                        else:
                            S.op("dve", lambda e: e.tensor_copy(out=qb.t[:, c4 * 512:(c4 + 1) * 512], in_=pQ4[c4].t[:]),
                                 [pQ4[c4].r], [qb.r])
                            yield
                    for half in range(2):
                        pt = pbT[half]
                        for a8 in range(8):
                            S.op("pe", lambda e: e.transpose(out=pt.t[:, a8 * 128:(a8 + 1) * 128],
                                                             in_=qb.t[:, (half * 8 + a8) * 128:(half * 8 + a8 + 1) * 128],
                                                             identity=ident.t[:]), [qb.r, ident.r], [pt.r])
                            yield
                        S.op("act", lambda e: e.activation(out=qT.t[:, half * 8:(half + 1) * 8, :],
                                                           in_=pt.t[:].rearrange("p (a b) -> p a b", a=8), func=AF.Copy),
                             [pt.r], [qT.r])
                        yield
                    for c4 in range(4):
                        for a4 in range(4):
                            hpi = c4 * 4 + a4
                            S.op("pe", lambda e: e.matmul(pSc[c4].t[:, a4 * 128:(a4 + 1) * 128], lhsT=qT.t[:, hpi, :],
                                                          rhs=SKT.t[:, hpi, :], start=True, stop=True),
                                 [qT.r, SKT.r], [pSc[c4].r])
                            yield
                        S.op("act", lambda e: e.activation(out=W1.t[:, c4 * 512:(c4 + 1) * 512], in_=pSc[c4].t[:],
                                                           func=AF.Copy), [pSc[c4].r], [W1.r])
                        yield
                    for a in range(16):
                        S.op("dve", lambda e: e.max(out=tops.t[:, a, 0:8], in_=sc3[:, a, :]), [W1.r], [tops.r])
                        yield
                        S.op("dve", lambda e: e.max_index(out=topi.t[:, a, 0:8], in_max=tops.t[:, a, 0:8],
                                                          in_values=sc3[:, a, :]), [W1.r, tops.r], [topi.r])
                        yield
                        S.op("dve", lambda e: e.match_replace(out=scw3[:, a, :], in_to_replace=tops.t[:, a, 0:8],
                                                              in_values=sc3[:, a, :], imm_value=-1e30),
                             [W1.r, tops.r], [W2.r])
                        yield
                        S.op("dve", lambda e: e.max(out=tops.t[:, a, 8:16], in_=scw3[:, a, :]), [W2.r], [tops.r])
                        yield
                        S.op("dve", lambda e: e.max_index(out=topi.t[:, a, 8:16], in_max=tops.t[:, a, 8:16],
                                                          in_values=scw3[:, a, :]), [W2.r, tops.r], [topi.r])
                        yield
                    S.op("dve", lambda e: e.tensor_copy(out=topif.t[:], in_=topi.t[:]), [topi.r], [topif.r])
                    yield
                    for h in range(8):
                        S.op("dve", lambda e: e.tensor_tensor(
                            out=cand.t[:, h, :].rearrange("p (a b) -> p a b", a=16),
                            in0=tops.t[:, 2 * h, :].unsqueeze(2).to_broadcast([128, 16, 16]),
                            in1=tops.t[:, 2 * h + 1, :].unsqueeze(1).to_broadcast([128, 16, 16]), op=ALU.add),
                            [tops.r], [cand.r])
                        yield
                    for h in range(8):
                        S.op("dve", lambda e: e.max(out=best.t[:, h, 0:8], in_=cand.t[:, h, :]), [cand.r], [best.r])
                        yield
                        S.op("dve", lambda e: e.max_index(out=bpos.t[:, h, 0:8], in_max=best.t[:, h, 0:8],
                                                          in_values=cand.t[:, h, :]), [cand.r, best.r], [bpos.r])
                        yield
                        S.op("dve", lambda e: e.match_replace(out=candw3[:, h, :], in_to_replace=best.t[:, h, 0:8],
                                                              in_values=cand.t[:, h, :], imm_value=-1e30),
                             [cand.r, best.r], [W2.r])
                        yield
                        S.op("dve", lambda e: e.max(out=best.t[:, h, 8:16], in_=candw3[:, h, :]), [W2.r], [best.r])
                        yield
                        S.op("dve", lambda e: e.max_index(out=bpos.t[:, h, 8:16], in_max=best.t[:, h, 8:16],
                                                          in_values=candw3[:, h, :]), [W2.r, best.r], [bpos.r])
                        yield
                    S.op("dve", lambda e: e.tensor_copy(out=posf.t[:], in_=bpos.t[:].rearrange("p h k -> p (h k)")),
                         [bpos.r], [posf.r])
                    yield
                    pos_b = posf.t[:].unsqueeze(2).to_broadcast([128, 128, 16])
                    S.op("dve", lambda e: e.tensor_tensor(out=oh3, in0=pos_b,
                                                          in1=lo16.t[:].unsqueeze(1).to_broadcast([128, 128, 16]),
                                                          op=ALU.is_ge), [posf.r, lo16.r], [W1.r])
                    yield
                    S.op("dve", lambda e: e.tensor_tensor(out=oh2_3, in0=pos_b,
                                                          in1=hi16.t[:].unsqueeze(1).to_broadcast([128, 128, 16]),
                                                          op=ALU.is_lt), [posf.r, hi16.r], [W2.r])
                    yield
                    S.op("dve", lambda e: e.tensor_tensor(out=W1.t[:], in0=W1.t[:], in1=W2.t[:], op=ALU.mult),
                         [W1.r, W2.r], [W1.r])
                    yield
                    S.op("dve", lambda e: e.tensor_tensor(out=oh2_3, in0=oh3,
                                                           in1=iota16.t[:].unsqueeze(1).to_broadcast([128, 128, 16]),
                                                           op=ALU.mult), [W1.r, iota16.r], [W2.r])
                    yield
                    S.op("dve", lambda e: e.tensor_reduce(out=af.t[:], in_=oh2_3, axis=AX.X, op=ALU.add), [W2.r], [af.r])
                    yield
                    for h in range(8):
                        S.op("dve", lambda e: e.tensor_tensor(
                            out=oh4[:, h, :, :], in0=oh4[:, h, :, :],
                            in1=topif.t[:, 2 * h, :].unsqueeze(1).to_broadcast([128, 16, 16]), op=ALU.mult),
                            [W1.r, topif.r], [W1.r])
                        yield
                    S.op("dve", lambda e: e.tensor_reduce(out=i1f.t[:], in_=oh3, axis=AX.X, op=ALU.add), [W1.r], [i1f.r])
                    yield
                    S.op("dve", lambda e: e.scalar_tensor_tensor(out=bf.t[:], in0=af.t[:], scalar=-16.0, in1=posf.t[:],
                                                                 op0=ALU.mult, op1=ALU.add), [af.r, posf.r], [bf.r])
                    yield
                    S.op("dve", lambda e: e.tensor_tensor(out=oh3, in0=bf.t[:].unsqueeze(2).to_broadcast([128, 128, 16]),
                                                          in1=iota16.t[:].unsqueeze(1).to_broadcast([128, 128, 16]),
                                                          op=ALU.is_equal), [bf.r, iota16.r], [W1.r])
                    yield
                    for h in range(8):
                        S.op("dve", lambda e: e.tensor_tensor(
                            out=oh4[:, h, :, :], in0=oh4[:, h, :, :],
                            in1=topif.t[:, 2 * h + 1, :].unsqueeze(1).to_broadcast([128, 16, 16]), op=ALU.mult),
                            [W1.r, topif.r], [W1.r])
                        yield
                    S.op("dve", lambda e: e.tensor_reduce(out=i2f.t[:], in_=oh3, axis=AX.X, op=ALU.add), [W1.r], [i2f.r])
                    yield
                    S.op("dve", lambda e: e.scalar_tensor_tensor(out=idxf.t[:], in0=i1f.t[:], scalar=128.0, in1=i2f.t[:],
                                                                 op0=ALU.mult, op1=ALU.add), [i1f.r, i2f.r], [idxf.r])
                    yield
                    idx_ = idx.next()
                    stt['idx'] = idx_
                    S.op("dve", lambda e: e.tensor_copy(out=idx_.t[:], in_=idxf.t[:]), [idxf.r], [idx_.r])
                    yield
                    if debug and "idx" in dbg:
                        ld(dbg["idx"][ts, :], idxf.t[:], [idxf.r], [])
                        yield
                    S.op("dve", lambda e: e.tensor_tensor(out=gate_.t[:], in0=best.t[:],
                                                          in1=best.t[:, :, 0:1].to_broadcast([128, 8, 16]),
                                                          op=ALU.subtract), [best.r], [gate_.r])
                    yield
                    S.op("act", lambda e: e.activation(out=gate_.t[:], in_=gate_.t[:], func=AF.Exp), [gate_.r], [gate_.r])
                    yield
                    S.op("dve", lambda e: e.tensor_reduce(out=gsum.t[:], in_=gate_.t[:], axis=AX.X, op=ALU.add),
                         [gate_.r], [gsum.r])
                    yield
                    S.op("dve", lambda e: e.reciprocal(out=gsum.t[:], in_=gsum.t[:]), [gsum.r], [gsum.r])
                    yield
                    S.op("dve", lambda e: e.tensor_tensor(out=gate_.t[:], in0=gate_.t[:],
                                                          in1=gsum.t[:].unsqueeze(2).to_broadcast([128, 8, 16]),
                                                          op=ALU.mult), [gate_.r, gsum.r], [gate_.r])
                    yield

            def experts(s, nxt):
                if True:
                    ts = slice(s * 128, (s + 1) * 128)
                    stt = slot_state[s]
                    x2_, hq_, idx_, gate_ = stt['x2'], stt['hq'], stt['idx'], stt['gate']
                    hqb = stt['hqb']
                    ac = x2_
                    cf = coef.next()
                    NG = 128 // JC
                    grp = [None] * NG

                    def acc_group(gi, first):
                        cs = slice(gi * JC, (gi + 1) * JC)
                        S.op("dve", lambda e: e.tensor_tensor(out=cf.t[:, cs], in0=tg.t[:, cs], in1=actv.t[:, cs],
                                                              op=ALU.mult), [tgR[gi], actvR[gi]], [cfR[gi]])
                        for jj in range(JC):
                            j = gi * JC + jj
                            b_ = grp[gi][jj]
                            dg_ = dgr.next()
                            S.op("act", lambda e: e.activation(out=dg_.t[:], in_=ident.t[:], func=AF.Copy,
                                                               scale=cf.t[:, j:j + 1]), [ident.r, cfR[gi]], [dg_.r])
                            for half in range(2):
                                S.op("pe", lambda e: e.matmul(accP[half].t[:], lhsT=dg_.t[:],
                                                              rhs=b_.t[:, D + half * 512:D + (half + 1) * 512],
                                                              start=(j == 0), stop=(j == 127)),
                                     [dg_.r, b_.r], [accP[half].r])

                    def gelu1(gi):
                        cs = slice(gi * JC, (gi + 1) * JC)
                        S.op("dve", lambda e: e.scalar_tensor_tensor(out=tg.t[:, cs], in0=actv.t[:, cs], scalar=0.044715,
                                                                     in1=actv.t[:, cs], op0=ALU.mult, op1=ALU.mult),
                             [actvR[gi]], [tgR[gi]])
                        S.op("dve", lambda e: e.scalar_tensor_tensor(out=tg.t[:, cs], in0=tg.t[:, cs], scalar=1.0,
                                                                     in1=actv.t[:, cs], op0=ALU.add, op1=ALU.mult),
                             [tgR[gi], actvR[gi]], [tgR[gi]])
                        S.op("act", lambda e: e.activation(out=tg.t[:, cs], in_=tg.t[:, cs], func=AF.Sigmoid,
                                                           scale=2.0 * GC), [tgR[gi]], [tgR[gi]])
                        S.op("dve", lambda e: e.tensor_tensor(out=actv.t[:, cs], in0=actv.t[:, cs],
                                                              in1=gate_.t[:].rearrange("p h k -> p (h k)")[:, cs],
                                                              op=ALU.mult), [actvR[gi], gate_.r], [actvR[gi]])

                    for gi in range(NG):
                        grp[gi] = [uvg.next() for _ in range(JC)]
                        cs = slice(gi * JC, (gi + 1) * JC)
                        for jj in range(JC):
                            j = gi * JC + jj
                            b_ = grp[gi][jj]
                            S.dma("pool", lambda e: e.indirect_dma_start(
                                out=b_.t[:, :], out_offset=None, in_=UVb_d,
                                in_offset=bass.IndirectOffsetOnAxis(ap=idx_.t[:, j:j + 1], axis=0)),
                                [idx_.r], [b_.r])
                        for jj in range(JC):
                            j = gi * JC + jj
                            b_ = grp[gi][jj]
                            pr_ = prod.next()
                            S.op("dve", lambda e: e.tensor_tensor(out=pr_.t[:], in0=b_.t[:, 0:D], in1=hqb.t[:], op=ALU.mult),
                                 [b_.r, hqb.r], [pr_.r])
                            S.op("act", lambda e: e.activation(out=pr_.t[:], in_=pr_.t[:], func=AF.Copy,
                                                               accum_out=actv.t[:, j:j + 1]), [pr_.r], [pr_.r, actvR[gi]])
                        if gi >= 1:
                            gelu1(gi - 1)
                        if gi >= 2:
                            acc_group(gi - 2, gi == 2)
                        if nxt is not None:
                            for _ in range(FRONT_PER_GROUP):
                                next(nxt, None)
                    gelu1(NG - 1)
                    acc_group(NG - 2, False)
                    acc_group(NG - 1, False)
                    for half in range(2):
                        hs_ = slice(half * 512, (half + 1) * 512)
                        S.op("dve", lambda e: e.tensor_tensor(out=ac.t[:, hs_], in0=accP[half].t[:], in1=x2_.t[:, hs_],
                                                              op=ALU.add), [accP[half].r, x2_.r], [ac.r])
                    ld(y_own[ts, :], ac.t[:], [ac.r], [])

            FRONT_PER_GROUP = 4
            for _ in front(0):
                pass
            for s in range(n_slots):
                nxt = front(s + 1) if s + 1 < n_slots else None
                experts(s, nxt)
                if nxt is not None:
                    for _ in nxt:
                        pass
        S.finish()
    return nc


def own_blocks(c):
    return [8 * s + (c if s < 8 else 7 - c) for s in range(NSLOT)]


def make_core_inputs(c, inp):
    x = np.ascontiguousarray(inp["x"][0])
    blocks = own_blocks(c)
    rows = np.concatenate([np.arange(b * 128, (b + 1) * 128) for b in blocks])
    sel = np.zeros((128, NB), np.float32)
    sel[:, blocks] = 1.0
    masks = np.zeros((128, 2, 8, 128), np.float32)
    k = np.arange(128)[:, None]
    q = np.arange(128)[None, :]
    for hf, off in ((0, c), (1, 7 - c)):
        for jj in range(8):
            masks[:, hf, jj, :] = ((jj * 128 + k) < (off * 128 + q))
    rgp = np.zeros((512, 12), np.float32)
    rgp[:, 0:4] = inp["conv_w"][0].T
    rgp[:, 4] = inp["conv_b"][0]
    rgp[:, 5] = inp["rg_b_a"][0]
    rgp[:, 6] = inp["rg_b_x"][0]
    rgp[:, 7] = inp["rg_lambda"][0]
    rgp[:, 8] = inp["out_norm_rg"][0]
    rgp = rgp.reshape(4, 128, 12).transpose(1, 0, 2).reshape(128, 48)
    m = {
        "x_all": x,
        "x_own": np.ascontiguousarray(x[rows]),
        "sel": sel,
        "masks": masks.reshape(128, -1),
        "w_in": np.ascontiguousarray(inp["w_in"][0]),
        "nmix": np.ascontiguousarray(inp["norm_mix"][0].reshape(8, 128).T),
        "qk": np.concatenate([inp["q_norm"][0], inp["k_norm"][0]])[None, :].astype(np.float32),
        "rgp": np.ascontiguousarray(rgp),
        "rg_w_a": np.ascontiguousarray(inp["rg_w_a"][0]),
        "rg_w_x": np.ascontiguousarray(inp["rg_w_x"][0]),
        "gsb": np.ascontiguousarray(inp["out_norm_sb"][0].reshape(4, 128).T),
        "w_out": np.ascontiguousarray(inp["w_out"][0]),
        "nffn": np.ascontiguousarray(inp["norm_ffn"]),
        "wq": np.ascontiguousarray(inp["peer_w_query"][0].reshape(D, 2048)),
        "sk": np.ascontiguousarray(inp["peer_sub_keys"][0].reshape(16, 128, 128)),
        "peer_uv": inp["peer_uv"],
    }
    return m, rows


def kernel(**inputs):
    inp = {k: np.asarray(v, dtype=np.float32) for k, v in inputs.items()}
    inp["peer_uv"] = np.ascontiguousarray(np.concatenate([inp["peer_u"][0], inp["peer_v"][0]], axis=1))
    nc = build()
    in_maps, rows_all = [], []
    for c in range(8):
        m, rows = make_core_inputs(c, inp)
        in_maps.append(m)
        rows_all.append(rows)
    res = run_bass_kernel_spmd(nc, in_maps, core_ids=list(range(8)))
    out = np.zeros((1, S_LEN, D), np.float32)
    for c in range(8):
        out[0, rows_all[c]] = res.results[c]["y_own"]
    return out
```

```python
import numpy as np
from contextlib import ExitStack
import concourse.bass as bass
import concourse.mybir as mybir
from concourse.bass_utils import run_bass_kernel_spmd

F32 = mybir.dt.float32
BF16 = mybir.dt.bfloat16
U32 = mybir.dt.uint32
I32 = mybir.dt.int32
AF = mybir.ActivationFunctionType
ALU = mybir.AluOpType
AX = mybir.AxisListType

S_LEN = 16384
D = 1024
NB = 128
NSLOT = 16
EPS = 1e-6
GC = 0.7978845608028654


SAME_ENGINE_WAIT = True
NO_SELF_WAIT = set()


class Res:
    __slots__ = ("w", "r")

    def __init__(self):
        self.w = None
        self.r = {}


class Sch:
    CHUNK = 30000
    NDMA = 24

    def __init__(self, nc, stack):
        self.nc = nc
        self.stack = stack
        self.eng = {"pe": nc.tensor, "act": nc.scalar, "dve": nc.vector,
                    "pool": nc.gpsimd, "sp": nc.sync}
        self.sems = {}
        self.cnt = {k: 0 for k in self.eng}
        self.waited = {}
        self.dma_pool = {k: [] for k in self.eng}
        self.dma_rr = {k: 0 for k in self.eng}
        self.nsem = 0

    def _sem(self, key):
        s = self.sems.get(key)
        if s is None:
            s = self.stack.enter_context(self.nc.semaphore("s%d" % self.nsem))
            self.nsem += 1
            self.sems[key] = s
        return s

    def _wait(self, eng, deps):
        e = self.eng[eng]
        for key, val in deps.items():
            if key[0] == eng and (eng == "pe" or eng in NO_SELF_WAIT):
                continue
            k = (eng, key)
            if self.waited.get(k, 0) >= val:
                continue
            self.waited[k] = val
            e.wait_ge(self._sem(key), val)

    @staticmethod
    def _deps(reads, writes):
        deps = {}

        def add(k, v):
            if deps.get(k, 0) < v:
                deps[k] = v
        for r in reads:
            if r.w is not None:
                add(*r.w)
        for w in writes:
            if w.w is not None:
                add(*w.w)
            for k, v in w.r.items():
                add(k, v)
        return deps

    @staticmethod
    def _commit(tok, reads, writes):
        k, v = tok
        for r in reads:
            if r.r.get(k, 0) < v:
                r.r[k] = v
        for w in writes:
            w.w = tok
            w.r = {}

    def op(self, eng, fn, reads=(), writes=()):
        deps = self._deps(reads, writes)
        self._wait(eng, deps)
        ins = fn(self.eng[eng])
        n = self.cnt[eng]
        self.cnt[eng] = n + 1
        key = (eng, n // self.CHUNK)
        val = n % self.CHUNK + 1
        ins.then_inc(self._sem(key), 1)
        self._commit((key, val), reads, writes)
        return ins

    def dma(self, eng, fn, reads=(), writes=()):
        deps = self._deps(reads, writes)
        pool = self.dma_pool[eng]
        i = self.dma_rr[eng] % self.NDMA
        self.dma_rr[eng] += 1
        if i >= len(pool):
            pool.append([("dma", eng, len(pool)), 0])
            i = len(pool) - 1
        key, val = pool[i]
        if val > 0 and deps.get(key, 0) < val:
            deps[key] = val
        self._wait(eng, deps)
        ins = fn(self.eng[eng])
        val += 16
        pool[i][1] = val
        ins.then_inc(self._sem(key), 16)
        self._commit((key, val), reads, writes)
        return ins

    def barrier(self):
        deps = {}
        for e, pool in self.dma_pool.items():
            for key, val in pool:
                if val:
                    deps[key] = val
        for e, n in self.cnt.items():
            if n:
                deps[(e, (n - 1) // self.CHUNK)] = (n - 1) % self.CHUNK + 1
        for eng in self.eng:
            e = self.eng[eng]
            for key, val in deps.items():
                k = (eng, key)
                if self.waited.get(k, 0) >= val:
                    continue
                self.waited[k] = val
                e.wait_ge(self._sem(key), val)

    def finish(self, eng="sp"):
        deps = {}
        for e, pool in self.dma_pool.items():
            for key, val in pool:
                if val:
                    deps[key] = val
        for e, n in self.cnt.items():
            if n:
                deps[(e, (n - 1) // self.CHUNK)] = (n - 1) % self.CHUNK + 1
        e = self.eng[eng]
        for key, val in deps.items():
            e.wait_ge(self._sem(key), val)


class Buf:
    __slots__ = ("t", "r")

    def __init__(self, t):
        self.t = t
        self.r = Res()


class Rot:
    def __init__(self, bufs):
        self.bufs = bufs
        self.i = 0

    def next(self):
        b = self.bufs[self.i % len(self.bufs)]
        self.i += 1
        return b


def build(debug=None):
    nc = bass.Bass("TRN2", target_bir_lowering=False)

    def din(name, shape, dt=F32):
        return nc.dram_tensor(name, list(shape), dt, kind="ExternalInput").ap()

    x_all = din("x_all", [S_LEN, D])
    x_own = din("x_own", [2048, D])
    sel_d = din("sel", [128, NB])
    masks_d = din("masks", [128, 2 * 8 * 128])
    w_in = din("w_in", [D, 2560])
    nmix_d = din("nmix", [128, 8])
    qk_d = din("qk", [1, 128])
    rgp_d = din("rgp", [128, 4 * 12])
    rgwa_d = din("rg_w_a", [8, 64, 64])
    rgwx_d = din("rg_w_x", [8, 64, 64])
    gsb_d = din("gsb", [128, 4])
    w_out = din("w_out", [D, D])
    nffn_d = din("nffn", [1, D])
    wq_d = din("wq", [D, 2048])
    sk_d = din("sk", [16, 128, 128])
    puv_d = din("peer_uv", [16384, 2 * D])
    y_own = nc.dram_tensor("y_own", [2048, D], F32, kind="ExternalOutput").ap()
    KT_d = nc.dram_tensor("KT_scr", [4, 128, S_LEN], BF16, kind="Internal").ap()
    V_d = nc.dram_tensor("V_scr", [S_LEN, 512], BF16, kind="Internal").ap()
    UVb_d = nc.dram_tensor("UVb_scr", [16384, 2 * D], BF16, kind="Internal").ap()
    rKT = Res()
    rV = Res()
    dbg = {}
    if debug:
        for nm, shp in debug.items():
            if nm in ("nsb", "nheads", "nslots", "stop"):
                continue
            dbg[nm] = nc.dram_tensor(nm, list(shp), F32, kind="ExternalOutput").ap()

    with ExitStack() as top:
        S = Sch(nc, top)
        cnt = [0]

        def sbuf(st, shape, dt):
            cnt[0] += 1
            return Buf(st.enter_context(nc.sbuf_tensor("t%d" % cnt[0], list(shape), dt)))

        def psum(st, shape, dt):
            cnt[0] += 1
            return Buf(st.enter_context(nc.psum_tensor("p%d" % cnt[0], list(shape), dt)))

        dmaq = ["sp", "sp"]
        dq = [0]

        def ld(out, in_, reads, writes, q=None):
            if q is None:
                q = dmaq[dq[0] % 2]
                dq[0] += 1
            S.dma(q, lambda e: e.dma_start(out=out, in_=in_), reads, writes)

        ident_f = sbuf(top, [128, 128], F32)
        ident = sbuf(top, [128, 128], BF16)
        ones_b = sbuf(top, [128, 128], BF16)
        zeros_b = sbuf(top, [128, 512], BF16)
        ssrg = sbuf(top, [128, NSLOT], F32)
        sssb = sbuf(top, [128, NSLOT], F32)
        mixrg = sbuf(top, [128, 4, 2048], BF16)
        mixsb = sbuf(top, [128, 4, 2048], BF16)
        pab = top.enter_context(ExitStack())
        QT = sbuf(pab, [128, 4, 2048], BF16)

        S.op("pool", lambda e: e.memset(ident_f.t[:], 0.0), [], [ident_f.r])
        S.op("pool", lambda e: e.affine_select(out=ident_f.t[:], in_=ident_f.t[:], pattern=[[-1, 128]],
                                                compare_op=ALU.not_equal, fill=1.0, base=0,
                                                channel_multiplier=1), [ident_f.r], [ident_f.r])
        S.op("dve", lambda e: e.tensor_copy(out=ident.t[:], in_=ident_f.t[:]), [ident_f.r], [ident.r])
        S.op("pool", lambda e: e.memset(ones_b.t[:], 1.0), [], [ones_b.r])
        S.op("pool", lambda e: e.memset(zeros_b.t[:], 0.0), [], [zeros_b.r])
        S.op("pool", lambda e: e.memset(sssb.t[:], 0.0), [], [sssb.r])

        pbT = [psum(top, [128, 1024], BF16) for _ in range(2)]
        pbF = [psum(top, [128, 512], F32) for _ in range(6)]

        def rstd_from(ms_ap, out_ap, res_in, res_out, n, scale, tmp):
            S.op("dve", lambda e: e.tensor_scalar(out=tmp.t[:, 0:n], in0=ms_ap, scalar1=scale, scalar2=EPS,
                                                   op0=ALU.mult, op1=ALU.add), [res_in], [tmp.r])
            S.op("act", lambda e: e.activation(out=tmp.t[:, 0:n], in_=tmp.t[:, 0:n], func=AF.Ln), [tmp.r], [tmp.r])
            S.op("act", lambda e: e.activation(out=out_ap, in_=tmp.t[:, 0:n], func=AF.Exp, scale=-0.5),
                 [tmp.r], [res_out])

        with ExitStack() as pa:
            hso = sbuf(pa, [128, 4, 2048], F32)
            nmix = sbuf(pa, [128, 8], F32)
            ld(nmix.t[:], nmix_d, [], [nmix.r])
            w_in_v = w_in.rearrange("(dc p) e -> p dc e", p=128)

            def load_w(dst_list):
                with ExitStack() as ws:
                    stg = Rot([sbuf(ws, [128, 8, 512], F32) for _ in range(1)])
                    load_w_inner(stg, dst_list)
                S.barrier()

            def load_w_inner(stg, dst_list):
                for ci, (dst, dcol, scol) in enumerate(dst_list):
                    sg = stg.next()
                    ld(sg.t[:], w_in_v[:, :, scol:scol + 512], [], [sg.r])
                    for dc in range(8):
                        eng = "dve"
                        S.op(eng, lambda e: e.tensor_scalar(out=dst.t[:, dc, dcol:dcol + 512], in0=sg.t[:, dc, :],
                                                            scalar1=nmix.t[:, dc:dc + 1], scalar2=None,
                                                            op0=ALU.mult), [sg.r, nmix.r], [dst.r])
            qk = sbuf(pa, [128, 128], F32)
            ld(qk.t[:], qk_d.to_broadcast([128, 128]), [], [qk.r])
            gqk1 = sbuf(pa, [128, 64], F32)
            S.op("dve", lambda e: e.scalar_tensor_tensor(out=gqk1.t[:], in0=qk.t[:, 0:64], scalar=0.125,
                                                         in1=qk.t[:, 64:128], op0=ALU.mult, op1=ALU.mult),
                 [qk.r], [gqk1.r])
            gqk = sbuf(pa, [128, 8, 64], F32)
            S.op("dve", lambda e: e.tensor_copy(out=gqk.t[:], in_=gqk1.t[:].unsqueeze(1).to_broadcast([128, 8, 64])),
                 [gqk1.r], [gqk.r])
            rgp = sbuf(pa, [128, 4, 12], F32)
            ld(rgp.t[:].rearrange("p a b -> p (a b)"), rgp_d, [], [rgp.r])
            rgc = sbuf(pa, [128, 4, 4], F32)
            tmpc = sbuf(pa, [128, 4], F32)
            S.op("act", lambda e: e.activation(out=tmpc.t[:], in_=rgp.t[:, :, 7], func=AF.Exp, scale=-1.0),
                 [rgp.r], [tmpc.r])
            S.op("act", lambda e: e.activation(out=tmpc.t[:], in_=tmpc.t[:], func=AF.Ln, bias=1.0),
                 [tmpc.r], [tmpc.r])
            S.op("dve", lambda e: e.tensor_scalar(out=rgc.t[:, :, 0], in0=tmpc.t[:], scalar1=-8.0, scalar2=None,
                                                  op0=ALU.mult), [tmpc.r], [rgc.r])
            S.op("dve", lambda e: e.tensor_scalar(out=rgc.t[:, :, 1], in0=tmpc.t[:], scalar1=-16.0, scalar2=None,
                                                  op0=ALU.mult), [tmpc.r], [rgc.r])
            S.op("dve", lambda e: e.tensor_scalar(out=rgc.t[:, :, 2], in0=rgp.t[:, :, 5], scalar1=-1.0, scalar2=None,
                                                  op0=ALU.mult), [rgp.r], [rgc.r])
            S.op("dve", lambda e: e.tensor_scalar(out=rgc.t[:, :, 3], in0=rgp.t[:, :, 6], scalar1=-1.0, scalar2=None,
                                                  op0=ALU.mult), [rgp.r], [rgc.r])
            WaBD = sbuf(pa, [128, 4, 128], BF16)
            WxBD = sbuf(pa, [128, 4, 128], BF16)
            with ExitStack() as ws:
                for (dst, src) in ((WaBD, rgwa_d), (WxBD, rgwx_d)):
                    sg = sbuf(ws, [128, 4, 128], F32)
                    S.op("pool", lambda e: e.memset(sg.t[:], 0.0), [], [sg.r])
                    for ct in range(4):
                        ld(sg.t[0:64, ct, 0:64], src[2 * ct], [], [sg.r])
                        ld(sg.t[64:128, ct, 64:128], src[2 * ct + 1], [], [sg.r])
                    S.op("dve", lambda e: e.tensor_copy(out=dst.t[:], in_=sg.t[:]), [sg.r], [dst.r])
            S.barrier()
            sel = sbuf(pa, [128, NB], F32)
            ld(sel.t[:], sel_d, [], [sel.r])

            uvstg = Rot([sbuf(pa, [128, 2 * D], BF16) for _ in range(2)])
            uv_chunk = [0]

            def convert_uv(nchunks):
                for _ in range(nchunks):
                    c = uv_chunk[0]
                    if c >= 128:
                        return
                    uv_chunk[0] += 1
                    sg = uvstg.next()
                    S.dma("pool", lambda e: e.dma_start(out=sg.t[:], in_=puv_d[c * 128:(c + 1) * 128, :]), [], [sg.r])
                    ld(UVb_d[c * 128:(c + 1) * 128, :], sg.t[:], [sg.r], [])

            xt = Rot([sbuf(pa, [128, D], F32) for _ in range(3)])
            junk = sbuf(pa, [128, D], BF16)
            ss = Rot([sbuf(pa, [128, 1], F32) for _ in range(2)])
            rstd = Rot([sbuf(pa, [128, 1], F32) for _ in range(2)])
            tmp1 = Rot([sbuf(pa, [128, 8], F32) for _ in range(2)])
            tmp1k = Rot([sbuf(pa, [128, 8], F32) for _ in range(2)])
            xn = Rot([sbuf(pa, [128, D], BF16) for _ in range(2)])
            xnT = Rot([sbuf(pa, [128, 8, 512], BF16) for _ in range(2)])
            ksq = sbuf(pa, [128, 8, 64], F32)
            kms = Rot([sbuf(pa, [128, 8], F32) for _ in range(2)])
            ksc = Rot([sbuf(pa, [128, 8], F32) for _ in range(2)])
            kn = Rot([sbuf(pa, [128, 512], BF16) for _ in range(2)])

            pT, pT2 = pbT
            pK, pV, pX, pZa, pZi, pQ = pbF

            def P0(src_rows):
                x_ = xt.next()
                ld(x_.t[:], src_rows, [], [x_.r])
                return x_

            def P1a(x_):
                s_ = ss.next()
                S.op("act", lambda e: e.activation(out=junk.t[:], in_=x_.t[:], func=AF.Square, accum_out=s_.t[:]),
                     [x_.r], [s_.r])
                return s_

            def P1b(x_, s_):
                r_ = rstd.next()
                t_ = tmp1.next()
                rstd_from(s_.t[:], r_.t[:], s_.r, r_.r, 1, 1.0 / D, t_)
                n_ = xn.next()
                S.op("dve", lambda e: e.tensor_scalar(out=n_.t[:], in0=x_.t[:], scalar1=r_.t[:, 0:1], scalar2=None,
                                                      op0=ALU.mult), [x_.r, r_.r], [n_.r])
                return n_

            def P1(src_rows, x_=None):
                if x_ is None:
                    x_ = P0(src_rows)
                return P1b(x_, P1a(x_))

            def P2(n_, xnT_b, b):
                for dc in range(8):
                    S.op("pe", lambda e: e.transpose(out=pT.t[:, dc * 128:(dc + 1) * 128],
                                                     in_=n_.t[:, dc * 128:(dc + 1) * 128], identity=ident.t[:]),
                         [n_.r, ident.r], [pT.r])
                S.op("act", lambda e: e.activation(out=xnT_b.t[:, :, b * 128:(b + 1) * 128],
                                                   in_=pT.t[:].rearrange("p (a b) -> p a b", a=8), func=AF.Copy),
                     [pT.r], [xnT_b.r])

            def norm_block(src_rows, xnT_b, b):
                P2(P1(src_rows), xnT_b, b)

            def qk_norm_a(pk):
                S.op("act", lambda e: e.activation(out=ksq.t[:].rearrange("p a b -> p (a b)"), in_=pk.t[:],
                                                   func=AF.Square), [pk.r], [ksq.r])
                ms = kms.next()
                S.op("dve", lambda e: e.tensor_reduce(out=ms.t[:], in_=ksq.t[:], axis=AX.X, op=ALU.add),
                     [ksq.r], [ms.r])
                return ms

            def qk_norm_b(pk, ms, gains):
                sc = ksc.next()
                t_ = tmp1k.next()
                rstd_from(ms.t[:], sc.t[:], ms.r, sc.r, 8, 1.0 / 64, t_)
                k_ = kn.next()
                if gains:
                    S.op("dve", lambda e: e.tensor_tensor(out=ksq.t[:], in0=pk.t[:].rearrange("p (a b) -> p a b", a=8),
                                                          in1=sc.t[:].unsqueeze(2).to_broadcast([128, 8, 64]),
                                                          op=ALU.mult), [pk.r, sc.r], [ksq.r])
                    S.op("dve", lambda e: e.tensor_tensor(out=k_.t[:].rearrange("p (a b) -> p a b", a=8),
                                                          in0=ksq.t[:], in1=gqk.t[:], op=ALU.mult),
                         [ksq.r, gqk.r], [k_.r])
                else:
                    S.op("dve", lambda e: e.tensor_tensor(out=k_.t[:].rearrange("p (a b) -> p a b", a=8),
                                                          in0=pk.t[:].rearrange("p (a b) -> p a b", a=8),
                                                          in1=sc.t[:].unsqueeze(2).to_broadcast([128, 8, 64]),
                                                          op=ALU.mult), [pk.r, sc.r], [k_.r])
                return k_

            def qk_norm(pk, gains):
                return qk_norm_b(pk, qk_norm_a(pk), gains)

            pa1 = ExitStack()
            Wkvx = sbuf(pa1, [128, 8, 1536], BF16)
            load_w([(Wkvx, 0, 512), (Wkvx, 512, 1024), (Wkvx, 1024, 1536)])
            KTs = Rot([sbuf(pa1, [128, 4, 512], BF16) for _ in range(1)])
            Vs = Rot([sbuf(pa1, [128, 4, 512], BF16) for _ in range(2)])
            xr = [sbuf(pa1, [128, 515], F32) for _ in range(4)]
            hprev = [sbuf(pa1, [128, 1], F32) for _ in range(4)]
            for ct in range(4):
                S.op("pool", lambda e: e.memset(xr[ct].t[:, 0:3], 0.0), [], [xr[ct].r])
                S.op("pool", lambda e: e.memset(hprev[ct].t[:], 0.0), [], [hprev[ct].r])
            NR = 2
            ry = Rot([sbuf(pa1, [128, 512], F32) for _ in range(3)])
            ryb = Rot([sbuf(pa1, [128, 512], BF16) for _ in range(NR)])
            rea = Rot([sbuf(pa1, [128, 512], F32) for _ in range(NR)])
            rei = Rot([sbuf(pa1, [128, 512], F32) for _ in range(NR)])
            ra_ = Rot([sbuf(pa1, [128, 512], F32) for _ in range(NR)])
            rsq = Rot([sbuf(pa1, [128, 512], F32) for _ in range(NR)])
            rhs_ = Rot([sbuf(pa1, [128, 512], F32) for _ in range(NR)])

            n_sb = 32 if debug is None or "nsb" not in debug else debug["nsb"][0]
            NBLK = 4 * n_sb
            pKr = Rot([pK, pQ])
            sbst = {}
            blkst = {}
            rgst = {}

            def get_sb(sb):
                if sb not in sbst:
                    sbst[sb] = (xnT.next(), KTs.next(), Vs.next())
                return sbst[sb]

            def sP0(n):
                blkst[n] = {"x": P0(x_all[n * 128:(n + 1) * 128, :])}

            def sP1(n):
                blkst[n]["ss"] = P1a(blkst[n]["x"])

            def sP1b(n):
                blkst[n]["xn"] = P1b(blkst[n]["x"], blkst[n]["ss"])

            def sP2(n):
                xT_, kts, vs = get_sb(n // 4)
                P2(blkst[n]["xn"], xT_, n % 4)

            def sP3(n):
                sb, b = n // 4, n % 4
                xT_, kts, vs = get_sb(sb)
                pk = pKr.next()
                for dc in range(8):
                    S.op("pe", lambda e: e.matmul(pk.t[:], lhsT=xT_.t[:, dc, b * 128:(b + 1) * 128],
                                                  rhs=Wkvx.t[:, dc, 0:512], start=(dc == 0), stop=(dc == 7)),
                         [xT_.r, Wkvx.r], [pk.r])
                for dc in range(8):
                    S.op("pe", lambda e: e.matmul(pV.t[:], lhsT=xT_.t[:, dc, b * 128:(b + 1) * 128],
                                                  rhs=Wkvx.t[:, dc, 512:1024], start=(dc == 0), stop=(dc == 7)),
                         [xT_.r, Wkvx.r], [pV.r])
                blkst[n]["pk"] = pk
                blkst[n]["ms"] = qk_norm_a(pk)
                S.op("act", lambda e: e.activation(out=vs.t[:, b, :], in_=pV.t[:], func=AF.Copy), [pV.r], [vs.r])

            def sP3b(n):
                blkst[n]["kn"] = qk_norm_b(blkst[n]["pk"], blkst[n]["ms"], True)

            def sP4(n):
                sb, b = n // 4, n % 4
                xT_, kts, vs = get_sb(sb)
                k_ = blkst[n]["kn"]
                for hp in range(4):
                    S.op("pe", lambda e: e.transpose(out=pT2.t[:, hp * 128:(hp + 1) * 128],
                                                     in_=k_.t[:, hp * 128:(hp + 1) * 128], identity=ident.t[:]),
                         [k_.r, ident.r], [pT2.r])
                S.op("act", lambda e: e.activation(out=kts.t[:, :, b * 128:(b + 1) * 128],
                                                   in_=pT2.t[:, 0:512].rearrange("p (a b) -> p a b", a=4),
                                                   func=AF.Copy), [pT2.r], [kts.r])
                if b == 3:
                    for hp in range(4):
                        ld(KT_d[hp, :, sb * 512:(sb + 1) * 512], kts.t[:, hp, :], [kts.r], [rKT])
                    ld(V_d[sb * 512:(sb + 1) * 512, :].rearrange("(b p) e -> p b e", p=128), vs.t[:], [vs.r], [rV])
                convert_uv(1)
                del blkst[n]

            def sR1(k):
                sb, ct = k // 4, k % 4
                xT_, kts, vs = get_sb(sb)
                for dc in range(8):
                    S.op("pe", lambda e: e.matmul(pX.t[:], lhsT=Wkvx.t[:, dc, 1024 + ct * 128:1024 + (ct + 1) * 128],
                                                  rhs=xT_.t[:, dc, :], start=(dc == 0), stop=(dc == 7)),
                         [xT_.r, Wkvx.r], [pX.r])
                xr_ = xr[ct]
                S.op("act", lambda e: e.activation(out=xr_.t[:, 3:515], in_=pX.t[:], func=AF.Copy),
                     [pX.r], [xr_.r])

            def sR1b(k):
                sb, ct = k // 4, k % 4
                xr_ = xr[ct]
                y_ = ry.next()
                S.op("dve", lambda e: e.tensor_scalar(out=y_.t[:], in0=xr_.t[:, 3:515], scalar1=rgp.t[:, ct, 3:4],
                                                      scalar2=rgp.t[:, ct, 4:5], op0=ALU.mult, op1=ALU.add),
                     [xr_.r, rgp.r], [y_.r])
                for j in range(3):
                    S.op("dve", lambda e: e.scalar_tensor_tensor(out=y_.t[:], in0=xr_.t[:, j:j + 512],
                                                                 scalar=rgp.t[:, ct, j:j + 1], in1=y_.t[:],
                                                                 op0=ALU.mult, op1=ALU.add),
                         [xr_.r, rgp.r, y_.r], [y_.r])
                S.op("pool", lambda e: e.tensor_copy(out=xr_.t[:, 0:3], in_=xr_.t[:, 512:515]), [xr_.r], [xr_.r])
                yb = ryb.next()
                S.op("pool", lambda e: e.tensor_copy(out=yb.t[:], in_=y_.t[:]), [y_.r], [yb.r])
                rgst[k] = {"y": y_, "yb": yb}

            def sR2(k):
                sb, ct = k // 4, k % 4
                st = rgst[k]
                yb = st["yb"]
                S.op("pe", lambda e: e.matmul(pZa.t[:], lhsT=WaBD.t[:, ct, :], rhs=yb.t[:], start=True, stop=True),
                     [yb.r, WaBD.r], [pZa.r])
                S.op("pe", lambda e: e.matmul(pZi.t[:], lhsT=WxBD.t[:, ct, :], rhs=yb.t[:], start=True, stop=True),
                     [yb.r, WxBD.r], [pZi.r])
                ea = rea.next()
                ei = rei.next()
                S.op("act", lambda e: e.activation(out=ea.t[:], in_=pZa.t[:], func=AF.Sigmoid,
                                                   bias=rgp.t[:, ct, 5:6]), [pZa.r, rgp.r], [ea.r])
                S.op("act", lambda e: e.activation(out=ei.t[:], in_=pZi.t[:], func=AF.Sigmoid,
                                                   bias=rgp.t[:, ct, 6:7]), [pZi.r, rgp.r], [ei.r])
                a_ = ra_.next()
                sq_ = rsq.next()
                S.op("act", lambda e: e.activation(out=a_.t[:], in_=ea.t[:], func=AF.Exp, scale=rgc.t[:, ct, 0:1]),
                     [ea.r, rgc.r], [a_.r])
                S.op("act", lambda e: e.activation(out=sq_.t[:], in_=ea.t[:], func=AF.Exp, scale=rgc.t[:, ct, 1:2]),
                     [ea.r, rgc.r], [sq_.r])
                S.op("act", lambda e: e.activation(out=sq_.t[:], in_=sq_.t[:], func=AF.Ln, scale=-1.0, bias=1.0),
                     [sq_.r], [sq_.r])
                S.op("act", lambda e: e.activation(out=sq_.t[:], in_=sq_.t[:], func=AF.Exp, scale=0.5),
                     [sq_.r], [sq_.r])
                st.update({"ei": ei, "a": a_, "sq": sq_})

            def sR3(k):
                sb, ct = k // 4, k % 4
                st = rgst.pop(k)
                ei, y_, sq_, a_ = st["ei"], st["y"], st["sq"], st["a"]
                b_ = ei
                S.op("dve", lambda e: e.tensor_tensor(out=b_.t[:], in0=ei.t[:], in1=y_.t[:], op=ALU.mult),
                     [ei.r, y_.r], [b_.r])
                S.op("dve", lambda e: e.tensor_tensor(out=b_.t[:], in0=b_.t[:], in1=sq_.t[:], op=ALU.mult),
                     [b_.r, sq_.r], [b_.r])
                h_ = rhs_.next()
                hp_ = hprev[ct]
                S.op("dve", lambda e: e.tensor_tensor_scan(out=h_.t[:], data0=a_.t[:], data1=b_.t[:],
                                                           initial=hp_.t[:, 0:1], op0=ALU.mult, op1=ALU.add),
                     [a_.r, b_.r, hp_.r], [h_.r])
                S.op("pool", lambda e: e.tensor_copy(out=hp_.t[:], in_=h_.t[:, 511:512]), [h_.r], [hp_.r])
                for b in range(4):
                    blk = 4 * sb + b
                    slot = blk // 8
                    dst = hso.t[:, ct, slot * 128:(slot + 1) * 128]
                    if blk % 8 == 0:
                        S.op("dve", lambda e: e.tensor_scalar(out=dst, in0=h_.t[:, b * 128:(b + 1) * 128],
                                                              scalar1=sel.t[:, blk:blk + 1], scalar2=None,
                                                              op0=ALU.mult), [h_.r, sel.r], [hso.r])
                    else:
                        S.op("dve", lambda e: e.scalar_tensor_tensor(out=dst, in0=h_.t[:, b * 128:(b + 1) * 128],
                                                                     scalar=sel.t[:, blk:blk + 1], in1=dst,
                                                                     op0=ALU.mult, op1=ALU.add),
                             [h_.r, sel.r, hso.r], [hso.r])
                if debug and "hs" in dbg and sb < dbg["hs"].shape[1] // 512:
                    ld(dbg["hs"][ct * 128:(ct + 1) * 128, sb * 512:(sb + 1) * 512], h_.t[:], [h_.r], [])

            stages = [(sP0, 0), (sP1, 1), (sP1b, 2), (sP2, 3), (sP3, 4), (sP3b, 5), (sP4, 6), (sR1, 7), (sR1b, 8), (sR2, 9),
                      (sR3, 10)]
            for i in range(NBLK + 11):
                for fn, lag in reversed(stages):
                    if 0 <= i - lag < NBLK:
                        fn(i - lag)

            convert_uv(128)
            pa1.close()
            S.barrier()
            Wqg = sbuf(pa, [128, 8, 1024], BF16)
            load_w([(Wqg, 0, 0), (Wqg, 512, 2048)])
            gl = Rot([sbuf(pa, [128, 512], F32) for _ in range(2)])
            gt = Rot([sbuf(pa, [128, 512], F32) for _ in range(2)])
            osq = Rot([sbuf(pa, [128, 512], BF16) for _ in range(2)])
            ost = {}
            osb_ = {}

            def get_og(g):
                if g not in osb_:
                    osb_[g] = xnT.next()
                return osb_[g]

            def oQ0(n):
                ost[n] = {"x": P0(x_own[n * 128:(n + 1) * 128, :])}

            def oQ1(n):
                ost[n]["ss"] = P1a(ost[n]["x"])

            def oQ1b(n):
                ost[n]["xn"] = P1b(ost[n]["x"], ost[n]["ss"])

            def oQ2(n):
                P2(ost[n]["xn"], get_og(n // 4), n % 4)

            def oQ3(n):
                xT_ = get_og(n // 4)
                b = n % 4
                for dc in range(8):
                    S.op("pe", lambda e: e.matmul(pQ.t[:], lhsT=xT_.t[:, dc, b * 128:(b + 1) * 128],
                                                  rhs=Wqg.t[:, dc, 0:512], start=(dc == 0), stop=(dc == 7)),
                         [xT_.r, Wqg.r], [pQ.r])
                ost[n]["ms"] = qk_norm_a(pQ)

            def oQ3b(n):
                ost[n]["q"] = qk_norm_b(pQ, ost[n]["ms"], False)

            def oQ4(n):
                slot = n
                q_ = ost.pop(n)["q"]
                for hp in range(4):
                    S.op("pe", lambda e: e.transpose(out=pT2.t[:, hp * 128:(hp + 1) * 128],
                                                     in_=q_.t[:, hp * 128:(hp + 1) * 128], identity=ident.t[:]),
                         [q_.r, ident.r], [pT2.r])
                S.op("act", lambda e: e.activation(out=QT.t[:, :, slot * 128:(slot + 1) * 128],
                                                   in_=pT2.t[:, 0:512].rearrange("p (a b) -> p a b", a=4),
                                                   func=AF.Copy), [pT2.r], [QT.r])

            def oG(k):
                g, ct = k // 4, k % 4
                xT_ = get_og(g)
                if ct == 0:
                    S.op("pe", lambda e: e.matmul(pZa.t[:, 0:4], lhsT=zeros_b.t[:, 0:128], rhs=zeros_b.t[:, 0:4],
                                                  start=True, stop=False), [zeros_b.r], [pZa.r])
                for dc in range(8):
                    S.op("pe", lambda e: e.matmul(pX.t[:], lhsT=Wqg.t[:, dc, 512 + ct * 128:512 + (ct + 1) * 128],
                                                  rhs=xT_.t[:, dc, :], start=(dc == 0), stop=(dc == 7)),
                         [xT_.r, Wqg.r], [pX.r])
                g_ = gl.next()
                t_ = gt.next()
                S.op("act", lambda e: e.activation(out=g_.t[:], in_=pX.t[:], func=AF.Copy), [pX.r], [g_.r])
                S.op("dve", lambda e: e.tensor_tensor(out=t_.t[:], in0=g_.t[:], in1=g_.t[:], op=ALU.mult),
                     [g_.r], [t_.r])
                S.op("dve", lambda e: e.tensor_scalar(out=t_.t[:], in0=t_.t[:], scalar1=0.044715, scalar2=1.0,
                                                      op0=ALU.mult, op1=ALU.add), [t_.r], [t_.r])
                S.op("dve", lambda e: e.tensor_tensor(out=t_.t[:], in0=t_.t[:], in1=g_.t[:], op=ALU.mult),
                     [t_.r, g_.r], [t_.r])
                S.op("act", lambda e: e.activation(out=t_.t[:], in_=t_.t[:], func=AF.Sigmoid, scale=2.0 * GC),
                     [t_.r], [t_.r])
                S.op("dve", lambda e: e.tensor_tensor(out=g_.t[:], in0=g_.t[:], in1=t_.t[:], op=ALU.mult),
                     [g_.r, t_.r], [g_.r])
                og = hso.t[:, ct, g * 512:(g + 1) * 512]
                S.op("dve", lambda e: e.tensor_tensor(out=og, in0=og, in1=g_.t[:], op=ALU.mult),
                     [hso.r, g_.r], [hso.r])
                sq_ = osq.next()
                S.op("dve", lambda e: e.tensor_tensor(out=sq_.t[:], in0=og, in1=og, op=ALU.mult),
                     [hso.r], [sq_.r])
                for b in range(4):
                    S.op("pe", lambda e: e.matmul(pZa.t[:, b:b + 1], lhsT=sq_.t[:, b * 128:(b + 1) * 128],
                                                  rhs=ones_b.t[:, 0:1], start=False, stop=(ct == 3 and b == 3)),
                         [sq_.r, ones_b.r], [pZa.r])
                S.op("dve", lambda e: e.tensor_scalar(out=mixrg.t[:, ct, g * 512:(g + 1) * 512], in0=og,
                                                      scalar1=rgp.t[:, ct, 8:9], scalar2=None, op0=ALU.mult),
                     [hso.r, rgp.r], [mixrg.r])
                if ct == 3:
                    S.op("dve", lambda e: e.tensor_copy(out=ssrg.t[:, 4 * g:4 * g + 4], in_=pZa.t[:, 0:4]),
                         [pZa.r], [ssrg.r])

            ostages = [(oQ0, 0), (oQ1, 1), (oQ1b, 2), (oQ2, 3), (oQ3, 4), (oQ3b, 5), (oQ4, 6), (oG, 8)]
            for i in range(NSLOT + 9):
                for fn, lag in reversed(ostages):
                    if 0 <= i - lag < NSLOT:
                        fn(i - lag)
            if debug and "org" in dbg:
                for ct in range(4):
                    ld(dbg["org"][ct * 128:(ct + 1) * 128, :], hso.t[:, ct, :], [hso.r], [])
            if debug and "ssrg" in dbg:
                ld(dbg["ssrg"], ssrg.t[:], [ssrg.r], [])

        S.barrier()
        if debug and debug.get("stop") == "A":
            S.finish()
            return nc

        with ExitStack() as pb:
            masks = sbuf(pb, [128, 2, 8, 128], F32)
            ld(masks.t[:].rearrange("p a b c -> p (a b c)"), masks_d, [], [masks.r])
            ntri_f = sbuf(pb, [128, 128], F32)
            ntri = sbuf(pb, [128, 128], BF16)
            nones = sbuf(pb, [128, 128], BF16)
            S.op("pool", lambda e: e.memset(ntri_f.t[:], -1.0), [], [ntri_f.r])
            S.op("pool", lambda e: e.affine_select(out=ntri_f.t[:], in_=ntri_f.t[:], pattern=[[-1, 128]],
                                                    compare_op=ALU.is_ge, fill=0.0, base=0, channel_multiplier=1),
                 [ntri_f.r], [ntri_f.r])
            S.op("dve", lambda e: e.tensor_copy(out=ntri.t[:], in_=ntri_f.t[:]), [ntri_f.r], [ntri.r])
            S.op("pool", lambda e: e.memset(nones.t[:], -1.0), [], [nones.r])
            gsb = sbuf(pb, [128, 4], F32)
            ld(gsb.t[:], gsb_d, [], [gsb.r])
            KTc2 = [[sbuf(pb, [128, 4096], BF16) for _ in range(4)] for _ in range(2)]
            Vc = [sbuf(pb, [128, 32, 128], BF16) for _ in range(4)]
            NW = 4
            be = Rot([sbuf(pb, [128, 512], F32) for _ in range(NW)])
            bsp = Rot([sbuf(pb, [128, 512], BF16) for _ in range(NW)])
            bwb = Rot([sbuf(pb, [128, 512], BF16) for _ in range(NW)])
            S32 = Rot([sbuf(pb, [128, 512], F32) for _ in range(2)])
            Sb = Rot([sbuf(pb, [128, 512], BF16) for _ in range(4)])
            osb = Rot([sbuf(pb, [128, 512], F32) for _ in range(2)])
            osq2 = Rot([sbuf(pb, [128, 512], BF16) for _ in range(2)])
            pZ = Rot([pbF[0], pbF[1], pbF[2], pbF[3]])
            pO = Rot([pbF[4], pbF[5]])
            pS = Buf(pbT[0].t[:].bitcast(F32))
            n_heads = 8 if not debug or "nheads" not in debug else debug["nheads"][0]

            class Step:
                pass
            steps = []
            for h in range(n_heads):
                for g in range(4):
                    jmax = 8 * (4 * g + 3) + 8
                    for j in range(jmax - 1, -1, -1):
                        st_ = Step()
                        st_.h, st_.g, st_.j = h, g, j
                        st_.first = (j == jmax - 1)
                        st_.last = (j == 0)
                        steps.append(st_)
            chain = {}

            def load_K(hp):
                KTc = KTc2[hp % 2]
                for c4 in range(4):
                    ld(KTc[c4].t[:], KT_d[hp, :, c4 * 4096:(c4 + 1) * 4096], [rKT], [KTc[c4].r])

            def load_V(hp):
                for c4 in range(4):
                    ld(Vc[c4].t[:],
                       V_d[c4 * 4096:(c4 + 1) * 4096, hp * 128:(hp + 1) * 128].rearrange("(j p) e -> p j e", p=128),
                       [rV], [Vc[c4].r])

            def geom(st_):
                h, g, j = st_.h, st_.g, st_.j
                s0 = max(4 * g, j // 8)
                c0 = (s0 - 4 * g) * 128
                msk = []
                for s in range(s0, 4 * g + 4):
                    if j >= 8 * s:
                        msk.append((slice((s - 4 * g) * 128, (s - 4 * g + 1) * 128), 0 if s < 8 else 1, j - 8 * s))
                return h // 2, h % 2, c0, msk

            def stageA(st_):
                h, g, j = st_.h, st_.g, st_.j
                hp, hh, c0, msk = geom(st_)
                p0 = hh * 64
                if st_.first and g == 0 and hh == 0 and hp == 0:
                    load_K(0)
                if st_.first and g == 0 and hh == 1 and hp + 1 < (n_heads + 1) // 2:
                    load_K(hp + 1)
                KTc = KTc2[hp % 2]
                if st_.first:
                    ch = Step()
                    ch.O = pO.next()
                    ch.S32 = S32.next()
                    ch.Sb = None
                    chain[(h, g)] = ch
                    S.op("pe", lambda e: e.matmul(ch.O.t[:, :], lhsT=zeros_b.t[:, 0:128], rhs=zeros_b.t[:, :],
                                                  start=True, stop=False), [zeros_b.r], [ch.O.r])
                    S.op("pool", lambda e: e.memset(ch.S32.t[:], 0.0), [], [ch.S32.r])
                st_.Z = pZ.next()
                kc = KTc[j // 32]
                jo = (j % 32) * 128
                qcols = slice(4 * g * 128 + c0, (4 * g + 4) * 128)
                S.op("pe", lambda e: e.matmul(st_.Z.t[:, c0:512], lhsT=kc.t[p0:p0 + 64, jo:jo + 128],
                                              rhs=QT.t[p0:p0 + 64, hp, qcols], start=True, stop=False),
                     [kc.r, QT.r], [st_.Z.r])
                st_.e = be.next()
                S.op("act", lambda e: e.activation(out=st_.e.t[:, c0:512], in_=st_.Z.t[:, c0:512], func=AF.Exp),
                     [st_.Z.r], [st_.e.r])

            def stageB(st_):
                hp, hh, c0, msk = geom(st_)
                st_.sp = bsp.next()
                S.op("act", lambda e: e.activation(out=st_.sp.t[:, c0:512], in_=st_.e.t[:, c0:512], func=AF.Ln,
                                                   bias=1.0), [st_.e.r], [st_.sp.r])
                for (cs, hf, jj) in msk:
                    S.op("pool", lambda e: e.tensor_tensor(out=st_.sp.t[:, cs], in0=st_.sp.t[:, cs],
                                                           in1=masks.t[:, hf, jj, :], op=ALU.mult),
                         [st_.sp.r, masks.r], [st_.sp.r])
                ch = chain[(st_.h, st_.g)]
                st_.Sb_in = ch.Sb
                st_.cp = getattr(ch, "c0_prev", None)
                if not st_.last:
                    sp = st_.sp
                    S.op("dve", lambda e: e.tensor_tensor(out=ch.S32.t[:, c0:512], in0=ch.S32.t[:, c0:512],
                                                          in1=sp.t[:, c0:512], op=ALU.add),
                         [ch.S32.r, sp.r], [ch.S32.r])
                    nsb_ = Sb.next()
                    S.op("dve", lambda e: e.tensor_copy(out=nsb_.t[:, c0:512], in_=ch.S32.t[:, c0:512]),
                         [ch.S32.r], [nsb_.r])
                    ch.Sb = nsb_
                    ch.c0_prev = c0

            def stageC(st_):
                h, g, j = st_.h, st_.g, st_.j
                hp, hh, c0, msk = geom(st_)
                p0 = hh * 64
                ch = chain[(h, g)]
                Z, sp = st_.Z, st_.sp
                has_carry = st_.Sb_in is not None
                S.op("pe", lambda e: e.matmul(Z.t[:, c0:512], lhsT=ntri.t[:], rhs=sp.t[:, c0:512],
                                              start=False, stop=not has_carry), [sp.r, ntri.r], [Z.r])
                if has_carry:
                    sbp = st_.Sb_in
                    cp = st_.cp
                    S.op("pe", lambda e: e.matmul(Z.t[:, cp:512], lhsT=nones.t[:], rhs=sbp.t[:, cp:512],
                                                  start=False, stop=True), [sbp.r, nones.r], [Z.r])
                wb = bwb.next()
                S.op("act", lambda e: e.activation(out=wb.t[:, c0:512], in_=Z.t[:, c0:512], func=AF.Exp),
                     [Z.r], [wb.r])
                for (cs, hf, jj) in msk:
                    S.op("pool", lambda e: e.tensor_tensor(out=wb.t[:, cs], in0=wb.t[:, cs],
                                                           in1=masks.t[:, hf, jj, :], op=ALU.mult),
                         [wb.r, masks.r], [wb.r])
                st_.wb = wb

            def stageD(st_):
                h, g, j = st_.h, st_.g, st_.j
                hp, hh, c0, msk = geom(st_)
                p0 = hh * 64
                ch = chain[(h, g)]
                wb = st_.wb
                if st_.first and g == 0 and hh == 0:
                    load_V(hp)
                vc = Vc[j // 32]
                MO = 64 * (hh + 1)
                O = ch.O
                S.op("pe", lambda e: e.matmul(O.t[0:MO, c0:512], lhsT=vc.t[:, j % 32, 0:MO], rhs=wb.t[:, c0:512],
                                              start=False, stop=st_.last), [vc.r, wb.r], [O.r])
                if st_.last:
                    o_ = osb.next()
                    S.op("act", lambda e: e.activation(out=o_.t[p0:p0 + 64, :], in_=O.t[p0:p0 + 64, :], func=AF.Copy),
                         [O.r], [o_.r])
                    S.op("dve", lambda e: e.tensor_scalar(out=mixsb.t[p0:p0 + 64, hp, g * 512:(g + 1) * 512],
                                                          in0=o_.t[p0:p0 + 64, :], scalar1=gsb.t[p0:p0 + 64, hp:hp + 1],
                                                          scalar2=None, op0=ALU.mult), [o_.r, gsb.r], [mixsb.r])
                    q2 = osq2.next()
                    S.op("dve", lambda e: e.tensor_tensor(out=q2.t[p0:p0 + 64, :], in0=o_.t[p0:p0 + 64, :],
                                                           in1=o_.t[p0:p0 + 64, :], op=ALU.mult), [o_.r], [q2.r])
                    for b in range(4):
                        S.op("pe", lambda e: e.matmul(pS.t[:, b:b + 1], lhsT=q2.t[p0:p0 + 64, b * 128:(b + 1) * 128],
                                                      rhs=ones_b.t[p0:p0 + 64, 0:1], start=True, stop=True),
                             [q2.r, ones_b.r], [pS.r])
                    S.op("dve", lambda e: e.tensor_tensor(out=sssb.t[:, 4 * g:4 * g + 4], in0=sssb.t[:, 4 * g:4 * g + 4],
                                                          in1=pS.t[:, 0:4], op=ALU.add), [sssb.r, pS.r], [sssb.r])
                    if debug and "osb" in dbg:
                        ld(dbg["osb"][h * 64:(h + 1) * 64, g * 512:(g + 1) * 512], o_.t[p0:p0 + 64, :], [o_.r], [])

            n = len(steps)
            for i in range(n + 3):
                if i < n:
                    stageA(steps[i])
                if 0 <= i - 1 < n:
                    stageB(steps[i - 1])
                if 0 <= i - 2 < n:
                    stageC(steps[i - 2])
                if 0 <= i - 3 < n:
                    stageD(steps[i - 3])

        pab.close()
        S.barrier()
        if debug and debug.get("stop") == "B":
            S.finish()
            return nc

        with ExitStack() as pc:
            Wo = sbuf(pc, [128, 8, D], BF16)
            Wq = sbuf(pc, [128, 8, 2048], BF16)
            SKT = sbuf(pc, [128, 16, 128], BF16)
            gffn = sbuf(pc, [128, D], F32)
            ld(gffn.t[:], nffn_d.to_broadcast([128, D]), [], [gffn.r])
            iota16 = sbuf(pc, [128, 16], F32)
            lo16 = sbuf(pc, [128, 16], F32)
            hi16 = sbuf(pc, [128, 16], F32)
            S.op("pool", lambda e: e.iota(iota16.t[:], pattern=[[1, 16]], base=0, channel_multiplier=0,
                                           allow_small_or_imprecise_dtypes=True), [], [iota16.r])
            S.op("dve", lambda e: e.tensor_scalar(out=lo16.t[:], in0=iota16.t[:], scalar1=16.0, scalar2=None,
                                                  op0=ALU.mult), [iota16.r], [lo16.r])
            S.op("dve", lambda e: e.tensor_scalar(out=hi16.t[:], in0=iota16.t[:], scalar1=16.0, scalar2=16.0,
                                                  op0=ALU.mult, op1=ALU.add), [iota16.r], [hi16.r])
            with ExitStack() as ws:
                wo_v = w_out.rearrange("(c p) e -> p c e", p=128)
                wq_v = wq_d.rearrange("(dc p) e -> p dc e", p=128)
                for half in range(2):
                    S.dma("pool", lambda e: e.dma_start(out=Wo.t[:, :, half * 512:(half + 1) * 512],
                                                        in_=wo_v[:, :, half * 512:(half + 1) * 512]), [], [Wo.r])
                for c4 in range(4):
                    S.dma("pool", lambda e: e.dma_start(out=Wq.t[:, :, c4 * 512:(c4 + 1) * 512],
                                                        in_=wq_v[:, :, c4 * 512:(c4 + 1) * 512]), [], [Wq.r])
                skf = sbuf(ws, [128, 16, 128], F32)
                skb = sbuf(ws, [128, 16, 128], BF16)
                ld(skf.t[:], sk_d.rearrange("a n k -> n a k"), [], [skf.r])
                S.op("dve", lambda e: e.tensor_copy(out=skb.t[:], in_=skf.t[:]), [skf.r], [skb.r])
                for half in range(2):
                    for a8 in range(8):
                        S.op("pe", lambda e: e.transpose(out=pbT[0].t[:, a8 * 128:(a8 + 1) * 128],
                                                         in_=skb.t[:, half * 8 + a8, :], identity=ident.t[:]),
                             [skb.r, ident.r], [pbT[0].r])
                    S.op("act", lambda e: e.activation(out=SKT.t[:, half * 8:(half + 1) * 8, :],
                                                       in_=pbT[0].t[:].rearrange("p (a b) -> p a b", a=8),
                                                       func=AF.Copy), [pbT[0].r], [SKT.r])
            S.barrier()

            rs_sb = sbuf(pc, [128, NSLOT], F32)
            rs_rg = sbuf(pc, [128, NSLOT], F32)
            tmp16 = sbuf(pc, [128, NSLOT], F32)
            rstd_from(sssb.t[:], rs_sb.t[:], sssb.r, rs_sb.r, NSLOT, 1.0 / 512, tmp16)
            rstd_from(ssrg.t[:], rs_rg.t[:], ssrg.r, rs_rg.r, NSLOT, 1.0 / 512, tmp16)

            x2 = Rot([sbuf(pc, [128, D], F32) for _ in range(2)])
            hqb_rot = Rot([sbuf(pc, [128, D], BF16) for _ in range(2)])
            hqT = sbuf(pc, [128, 8, 128], BF16)
            junk2 = sbuf(pc, [128, D], BF16)
            ss2 = sbuf(pc, [128, 1], F32)
            r2 = sbuf(pc, [128, 1], F32)
            t2 = sbuf(pc, [128, 8], F32)
            qb = sbuf(pc, [128, 2048], BF16)
            qT = sbuf(pc, [128, 16, 128], BF16)
            W1 = sbuf(pc, [128, 2048], F32)
            W2 = sbuf(pc, [128, 2048], F32)
            cand = sbuf(pc, [128, 8, 256], F32)
            sc3 = W1.t[:].rearrange("p (a n) -> p a n", a=16)
            scw3 = W2.t[:].rearrange("p (a n) -> p a n", a=16)
            candw3 = W2.t[:].rearrange("p (h n) -> p h n", h=8)
            oh3 = W1.t[:].rearrange("p (k a) -> p k a", a=16)
            oh4 = W1.t[:].rearrange("p (h k a) -> p h k a", h=8, a=16)
            oh2_3 = W2.t[:].rearrange("p (k a) -> p k a", a=16)
            tops = sbuf(pc, [128, 16, 16], F32)
            topi = sbuf(pc, [128, 16, 16], U32)
            topif = sbuf(pc, [128, 16, 16], F32)
            best = sbuf(pc, [128, 8, 16], F32)
            bpos = sbuf(pc, [128, 8, 16], U32)
            posf = sbuf(pc, [128, 128], F32)
            af = sbuf(pc, [128, 128], F32)
            bf = sbuf(pc, [128, 128], F32)
            i1f = sbuf(pc, [128, 128], F32)
            i2f = sbuf(pc, [128, 128], F32)
            idxf = sbuf(pc, [128, 128], F32)
            idx = Rot([sbuf(pc, [128, 128], I32) for _ in range(2)])
            gate = Rot([sbuf(pc, [128, 8, 16], F32) for _ in range(2)])
            gsum = sbuf(pc, [128, 8], F32)
            actv = sbuf(pc, [128, 128], F32)
            tg = sbuf(pc, [128, 128], F32)
            coef = Rot([sbuf(pc, [128, 128], F32) for _ in range(1)])
            JC = 2
            uvg = Rot([sbuf(pc, [128, 2 * D], BF16) for _ in range(11)])
            prod = Rot([sbuf(pc, [128, D], BF16) for _ in range(5)])
            dgr = Rot([sbuf(pc, [128, 128], BF16) for _ in range(4)])
            tgR = [Res() for _ in range(128 // JC)]
            actvR = [Res() for _ in range(128 // JC)]
            cfR = [Res() for _ in range(128 // JC)]
            accP = [pbF[2], pbF[3]]
            pP = [pbF[0], pbF[1], pbF[4], pbF[5]]
            pQ4 = [pbF[0], pbF[1], pbF[4], pbF[5]]
            pSc = [pbF[4], pbF[5], pbF[0], pbF[1]]
            n_slots = NSLOT if not debug or "nslots" not in debug else debug["nslots"][0]
            slot_state = {}

            def front(s):
                if True:
                    pass
                    ts = slice(s * 128, (s + 1) * 128)
                    x2_ = x2.next()
                    stt = {'x2': x2_}
                    gate_ = gate.next()
                    stt['gate'] = gate_
                    slot_state[s] = stt
                    ld(x2_.t[:], x_own[ts, :], [], [x2_.r])
                    yield
                    for half in range(2):
                        for c in range(4):
                            S.op("pe", lambda e: e.matmul(pP[half].t[:], lhsT=mixsb.t[:, c, ts],
                                                          rhs=Wo.t[:, c, half * 512:(half + 1) * 512],
                                                          start=(c == 0), stop=(c == 3)), [mixsb.r, Wo.r], [pP[half].r])
                            yield
                        for c in range(4):
                            S.op("pe", lambda e: e.matmul(pP[2 + half].t[:], lhsT=mixrg.t[:, c, ts],
                                                          rhs=Wo.t[:, 4 + c, half * 512:(half + 1) * 512],
                                                          start=(c == 0), stop=(c == 3)), [mixrg.r, Wo.r],
                                 [pP[2 + half].r])
                            yield
                    for half in range(2):
                        hs_ = slice(half * 512, (half + 1) * 512)
                        S.op("dve", lambda e: e.scalar_tensor_tensor(out=x2_.t[:, hs_], in0=pP[half].t[:],
                                                                     scalar=rs_sb.t[:, s:s + 1], in1=x2_.t[:, hs_],
                                                                     op0=ALU.mult, op1=ALU.add),
                             [pP[half].r, rs_sb.r, x2_.r], [x2_.r])
                        yield
                        S.op("dve", lambda e: e.scalar_tensor_tensor(out=x2_.t[:, hs_], in0=pP[2 + half].t[:],
                                                                     scalar=rs_rg.t[:, s:s + 1], in1=x2_.t[:, hs_],
                                                                     op0=ALU.mult, op1=ALU.add),
                             [pP[2 + half].r, rs_rg.r, x2_.r], [x2_.r])
                        yield
                    if debug and "x2" in dbg:
                        ld(dbg["x2"][ts, :], x2_.t[:], [x2_.r], [])
                        yield
                    S.op("act", lambda e: e.activation(out=junk2.t[:], in_=x2_.t[:], func=AF.Square, accum_out=ss2.t[:]),
                         [x2_.r], [ss2.r])
                    yield
                    rstd_from(ss2.t[:], r2.t[:], ss2.r, r2.r, 1, 1.0 / D, t2)
                    yield
                    hqb = hqb_rot.next()
                    stt['hqb'] = hqb
                    stt['hq'] = hqb
                    S.op("dve", lambda e: e.scalar_tensor_tensor(out=hqb.t[:], in0=x2_.t[:], scalar=r2.t[:, 0:1],
                                                                 in1=gffn.t[:], op0=ALU.mult, op1=ALU.mult),
                         [x2_.r, r2.r, gffn.r], [hqb.r])
                    yield
                    for dc in range(8):
                        S.op("pe", lambda e: e.transpose(out=pbT[0].t[:, dc * 128:(dc + 1) * 128],
                                                         in_=hqb.t[:, dc * 128:(dc + 1) * 128], identity=ident.t[:]),
                             [hqb.r, ident.r], [pbT[0].r])
                        yield
                    S.op("act", lambda e: e.activation(out=hqT.t[:], in_=pbT[0].t[:].rearrange("p (a b) -> p a b", a=8),
                                                       func=AF.Copy), [pbT[0].r], [hqT.r])
                    yield
                    for c4 in range(4):
                        for dc in range(8):
                            S.op("pe", lambda e: e.matmul(pQ4[c4].t[:], lhsT=hqT.t[:, dc, :],
                                                          rhs=Wq.t[:, dc, c4 * 512:(c4 + 1) * 512],
                                                          start=(dc == 0), stop=(dc == 7)), [hqT.r, Wq.r], [pQ4[c4].r])
                            yield
                        if c4 % 2 == 0:
                            S.op("act", lambda e: e.activation(out=qb.t[:, c4 * 512:(c4 + 1) * 512], in_=pQ4[c4].t[:],
                                                               func=AF.Copy), [pQ4[c4].r], [qb.r])
                            yield
                        else:
                            S.op("dve", lambda e: e.tensor_copy(out=qb.t[:, c4 * 512:(c4 + 1) * 512], in_=pQ4[c4].t[:]),
                                 [pQ4[c4].r], [qb.r])
                            yield
                    for half in range(2):
                        pt = pbT[half]
                        for a8 in range(8):
                            S.op("pe", lambda e: e.transpose(out=pt.t[:, a8 * 128:(a8 + 1) * 128],
                                                             in_=qb.t[:, (half * 8 + a8) * 128:(half * 8 + a8 + 1) * 128],
                                                             identity=ident.t[:]), [qb.r, ident.r], [pt.r])
                            yield
                        S.op("act", lambda e: e.activation(out=qT.t[:, half * 8:(half + 1) * 8, :],
                                                           in_=pt.t[:].rearrange("p (a b) -> p a b", a=8), func=AF.Copy),
                             [pt.r], [qT.r])
                        yield
                    for c4 in range(4):
                        for a4 in range(4):
                            hpi = c4 * 4 + a4
                            S.op("pe", lambda e: e.matmul(pSc[c4].t[:, a4 * 128:(a4 + 1) * 128], lhsT=qT.t[:, hpi, :],
                                                          rhs=SKT.t[:, hpi, :], start=True, stop=True),
                                 [qT.r, SKT.r], [pSc[c4].r])
                            yield
                        S.op("act", lambda e: e.activation(out=W1.t[:, c4 * 512:(c4 + 1) * 512], in_=pSc[c4].t[:],
                                                           func=AF.Copy), [pSc[c4].r], [W1.r])
                        yield
                    for a in range(16):
                        S.op("dve", lambda e: e.max(out=tops.t[:, a, 0:8], in_=sc3[:, a, :]), [W1.r], [tops.r])
                        yield
                        S.op("dve", lambda e: e.max_index(out=topi.t[:, a, 0:8], in_max=tops.t[:, a, 0:8],
                                                          in_values=sc3[:, a, :]), [W1.r, tops.r], [topi.r])
                        yield
                        S.op("dve", lambda e: e.match_replace(out=scw3[:, a, :], in_to_replace=tops.t[:, a, 0:8],
                                                              in_values=sc3[:, a, :], imm_value=-1e30),
                             [W1.r, tops.r], [W2.r])
                        yield
                        S.op("dve", lambda e: e.max(out=tops.t[:, a, 8:16], in_=scw3[:, a, :]), [W2.r], [tops.r])
                        yield
                        S.op("dve", lambda e: e.max_index(out=topi.t[:, a, 8:16], in_max=tops.t[:, a, 8:16],
                                                          in_values=scw3[:, a, :]), [W2.r, tops.r], [topi.r])
                        yield
                    S.op("dve", lambda e: e.tensor_copy(out=topif.t[:], in_=topi.t[:]), [topi.r], [topif.r])
                    yield
                    for h in range(8):
                        S.op("dve", lambda e: e.tensor_tensor(
                            out=cand.t[:, h, :].rearrange("p (a b) -> p a b", a=16),
                            in0=tops.t[:, 2 * h, :].unsqueeze(2).to_broadcast([128, 16, 16]),
                            in1=tops.t[:, 2 * h + 1, :].unsqueeze(1).to_broadcast([128, 16, 16]), op=ALU.add),
                            [tops.r], [cand.r])
                        yield
                    for h in range(8):
                        S.op("dve", lambda e: e.max(out=best.t[:, h, 0:8], in_=cand.t[:, h, :]), [cand.r], [best.r])
                        yield
                        S.op("dve", lambda e: e.max_index(out=bpos.t[:, h, 0:8], in_max=best.t[:, h, 0:8],
                                                          in_values=cand.t[:, h, :]), [cand.r, best.r], [bpos.r])
                        yield
                        S.op("dve", lambda e: e.match_replace(out=candw3[:, h, :], in_to_replace=best.t[:, h, 0:8],
                                                              in_values=cand.t[:, h, :], imm_value=-1e30),
                             [cand.r, best.r], [W2.r])
                        yield
                        S.op("dve", lambda e: e.max(out=best.t[:, h, 8:16], in_=candw3[:, h, :]), [W2.r], [best.r])
                        yield
                        S.op("dve", lambda e: e.max_index(out=bpos.t[:, h, 8:16], in_max=best.t[:, h, 8:16],
                                                          in_values=candw3[:, h, :]), [W2.r, best.r], [bpos.r])
                        yield
                    S.op("dve", lambda e: e.tensor_copy(out=posf.t[:], in_=bpos.t[:].rearrange("p h k -> p (h k)")),
                         [bpos.r], [posf.r])
                    yield
                    pos_b = posf.t[:].unsqueeze(2).to_broadcast([128, 128, 16])
                    S.op("dve", lambda e: e.tensor_tensor(out=oh3, in0=pos_b,
                                                          in1=lo16.t[:].unsqueeze(1).to_broadcast([128, 128, 16]),
                                                          op=ALU.is_ge), [posf.r, lo16.r], [W1.r])
                    yield
                    S.op("dve", lambda e: e.tensor_tensor(out=oh2_3, in0=pos_b,
                                                          in1=hi16.t[:].unsqueeze(1).to_broadcast([128, 128, 16]),
                                                          op=ALU.is_lt), [posf.r, hi16.r], [W2.r])
                    yield
                    S.op("dve", lambda e: e.tensor_tensor(out=W1.t[:], in0=W1.t[:], in1=W2.t[:], op=ALU.mult),
                         [W1.r, W2.r], [W1.r])
                    yield
                    S.op("dve", lambda e: e.tensor_tensor(out=oh2_3, in0=oh3,
                                                           in1=iota16.t[:].unsqueeze(1).to_broadcast([128, 128, 16]),
                                                           op=ALU.mult), [W1.r, iota16.r], [W2.r])
                    yield
                    S.op("dve", lambda e: e.tensor_reduce(out=af.t[:], in_=oh2_3, axis=AX.X, op=ALU.add), [W2.r], [af.r])
                    yield
                    for h in range(8):
                        S.op("dve", lambda e: e.tensor_tensor(
                            out=oh4[:, h, :, :], in0=oh4[:, h, :, :],
                            in1=topif.t[:, 2 * h, :].unsqueeze(1).to_broadcast([128, 16, 16]), op=ALU.mult),
                            [W1.r, topif.r], [W1.r])
                        yield
                    S.op("dve", lambda e: e.tensor_reduce(out=i1f.t[:], in_=oh3, axis=AX.X, op=ALU.add), [W1.r], [i1f.r])
                    yield
                    S.op("dve", lambda e: e.scalar_tensor_tensor(out=bf.t[:], in0=af.t[:], scalar=-16.0, in1=posf.t[:],
                                                                 op0=ALU.mult, op1=ALU.add), [af.r, posf.r], [bf.r])
                    yield
                    S.op("dve", lambda e: e.tensor_tensor(out=oh3, in0=bf.t[:].unsqueeze(2).to_broadcast([128, 128, 16]),
                                                          in1=iota16.t[:].unsqueeze(1).to_broadcast([128, 128, 16]),
                                                          op=ALU.is_equal), [bf.r, iota16.r], [W1.r])
                    yield
                    for h in range(8):
                        S.op("dve", lambda e: e.tensor_tensor(
                            out=oh4[:, h, :, :], in0=oh4[:, h, :, :],
                            in1=topif.t[:, 2 * h + 1, :].unsqueeze(1).to_broadcast([128, 16, 16]), op=ALU.mult),
                            [W1.r, topif.r], [W1.r])
                        yield
                    S.op("dve", lambda e: e.tensor_reduce(out=i2f.t[:], in_=oh3, axis=AX.X, op=ALU.add), [W1.r], [i2f.r])
                    yield
                    S.op("dve", lambda e: e.scalar_tensor_tensor(out=idxf.t[:], in0=i1f.t[:], scalar=128.0, in1=i2f.t[:],
                                                                 op0=ALU.mult, op1=ALU.add), [i1f.r, i2f.r], [idxf.r])
                    yield
                    idx_ = idx.next()
                    stt['idx'] = idx_
                    S.op("dve", lambda e: e.tensor_copy(out=idx_.t[:], in_=idxf.t[:]), [idxf.r], [idx_.r])
                    yield
                    if debug and "idx" in dbg:
                        ld(dbg["idx"][ts, :], idxf.t[:], [idxf.r], [])
                        yield
                    S.op("dve", lambda e: e.tensor_tensor(out=gate_.t[:], in0=best.t[:],
                                                          in1=best.t[:, :, 0:1].to_broadcast([128, 8, 16]),
                                                          op=ALU.subtract), [best.r], [gate_.r])
                    yield
                    S.op("act", lambda e: e.activation(out=gate_.t[:], in_=gate_.t[:], func=AF.Exp), [gate_.r], [gate_.r])
                    yield
                    S.op("dve", lambda e: e.tensor_reduce(out=gsum.t[:], in_=gate_.t[:], axis=AX.X, op=ALU.add),
                         [gate_.r], [gsum.r])
                    yield
                    S.op("dve", lambda e: e.reciprocal(out=gsum.t[:], in_=gsum.t[:]), [gsum.r], [gsum.r])
                    yield
                    S.op("dve", lambda e: e.tensor_tensor(out=gate_.t[:], in0=gate_.t[:],
                                                          in1=gsum.t[:].unsqueeze(2).to_broadcast([128, 8, 16]),
                                                          op=ALU.mult), [gate_.r, gsum.r], [gate_.r])
                    yield

            def experts(s, nxt):
                if True:
                    ts = slice(s * 128, (s + 1) * 128)
                    stt = slot_state[s]
                    x2_, hq_, idx_, gate_ = stt['x2'], stt['hq'], stt['idx'], stt['gate']
                    hqb = stt['hqb']
                    ac = x2_
                    cf = coef.next()
                    NG = 128 // JC
                    grp = [None] * NG

                    def acc_group(gi, first):
                        cs = slice(gi * JC, (gi + 1) * JC)
                        S.op("dve", lambda e: e.tensor_tensor(out=cf.t[:, cs], in0=tg.t[:, cs], in1=actv.t[:, cs],
                                                              op=ALU.mult), [tgR[gi], actvR[gi]], [cfR[gi]])
                        for jj in range(JC):
                            j = gi * JC + jj
                            b_ = grp[gi][jj]
                            dg_ = dgr.next()
                            S.op("act", lambda e: e.activation(out=dg_.t[:], in_=ident.t[:], func=AF.Copy,
                                                               scale=cf.t[:, j:j + 1]), [ident.r, cfR[gi]], [dg_.r])
                            for half in range(2):
                                S.op("pe", lambda e: e.matmul(accP[half].t[:], lhsT=dg_.t[:],
                                                              rhs=b_.t[:, D + half * 512:D + (half + 1) * 512],
                                                              start=(j == 0), stop=(j == 127)),
                                     [dg_.r, b_.r], [accP[half].r])

                    def gelu1(gi):
                        cs = slice(gi * JC, (gi + 1) * JC)
                        S.op("dve", lambda e: e.scalar_tensor_tensor(out=tg.t[:, cs], in0=actv.t[:, cs], scalar=0.044715,
                                                                     in1=actv.t[:, cs], op0=ALU.mult, op1=ALU.mult),
                             [actvR[gi]], [tgR[gi]])
                        S.op("dve", lambda e: e.scalar_tensor_tensor(out=tg.t[:, cs], in0=tg.t[:, cs], scalar=1.0,
                                                                     in1=actv.t[:, cs], op0=ALU.add, op1=ALU.mult),
                             [tgR[gi], actvR[gi]], [tgR[gi]])
                        S.op("act", lambda e: e.activation(out=tg.t[:, cs], in_=tg.t[:, cs], func=AF.Sigmoid,
                                                           scale=2.0 * GC), [tgR[gi]], [tgR[gi]])
                        S.op("dve", lambda e: e.tensor_tensor(out=actv.t[:, cs], in0=actv.t[:, cs],
                                                              in1=gate_.t[:].rearrange("p h k -> p (h k)")[:, cs],
                                                              op=ALU.mult), [actvR[gi], gate_.r], [actvR[gi]])

                    for gi in range(NG):
                        grp[gi] = [uvg.next() for _ in range(JC)]
                        cs = slice(gi * JC, (gi + 1) * JC)
                        for jj in range(JC):
                            j = gi * JC + jj
                            b_ = grp[gi][jj]
                            S.dma("pool", lambda e: e.indirect_dma_start(
                                out=b_.t[:, :], out_offset=None, in_=UVb_d,
                                in_offset=bass.IndirectOffsetOnAxis(ap=idx_.t[:, j:j + 1], axis=0)),
                                [idx_.r], [b_.r])
                        for jj in range(JC):
                            j = gi * JC + jj
                            b_ = grp[gi][jj]
                            pr_ = prod.next()
                            S.op("dve", lambda e: e.tensor_tensor(out=pr_.t[:], in0=b_.t[:, 0:D], in1=hqb.t[:], op=ALU.mult),
                                 [b_.r, hqb.r], [pr_.r])
                            S.op("act", lambda e: e.activation(out=pr_.t[:], in_=pr_.t[:], func=AF.Copy,
                                                               accum_out=actv.t[:, j:j + 1]), [pr_.r], [pr_.r, actvR[gi]])
                        if gi >= 1:
                            gelu1(gi - 1)
                        if gi >= 2:
                            acc_group(gi - 2, gi == 2)
                        if nxt is not None:
                            for _ in range(FRONT_PER_GROUP + (1 if gi >= 50 else 0)):
                                next(nxt, None)
                    gelu1(NG - 1)
                    acc_group(NG - 2, False)
                    acc_group(NG - 1, False)
                    for half in range(2):
                        hs_ = slice(half * 512, (half + 1) * 512)
                        S.op("dve", lambda e: e.tensor_tensor(out=ac.t[:, hs_], in0=accP[half].t[:], in1=x2_.t[:, hs_],
                                                              op=ALU.add), [accP[half].r, x2_.r], [ac.r])
                    ld(y_own[ts, :], ac.t[:], [ac.r], [])

            FRONT_PER_GROUP = 4
            for _ in front(0):
                pass
            for s in range(n_slots):
                nxt = front(s + 1) if s + 1 < n_slots else None
                experts(s, nxt)
                if nxt is not None:
                    for _ in nxt:
                        pass
        S.finish()
    return nc


def own_blocks(c):
    return [8 * s + (c if s < 8 else 7 - c) for s in range(NSLOT)]


def make_core_inputs(c, inp):
    x = np.ascontiguousarray(inp["x"][0])
    blocks = own_blocks(c)
    rows = np.concatenate([np.arange(b * 128, (b + 1) * 128) for b in blocks])
    sel = np.zeros((128, NB), np.float32)
    sel[:, blocks] = 1.0
    masks = np.zeros((128, 2, 8, 128), np.float32)
    k = np.arange(128)[:, None]
    q = np.arange(128)[None, :]
    for hf, off in ((0, c), (1, 7 - c)):
        for jj in range(8):
            masks[:, hf, jj, :] = ((jj * 128 + k) < (off * 128 + q))
    rgp = np.zeros((512, 12), np.float32)
    rgp[:, 0:4] = inp["conv_w"][0].T
    rgp[:, 4] = inp["conv_b"][0]
    rgp[:, 5] = inp["rg_b_a"][0]
    rgp[:, 6] = inp["rg_b_x"][0]
    rgp[:, 7] = inp["rg_lambda"][0]
    rgp[:, 8] = inp["out_norm_rg"][0]
    rgp = rgp.reshape(4, 128, 12).transpose(1, 0, 2).reshape(128, 48)
    m = {
        "x_all": x,
        "x_own": np.ascontiguousarray(x[rows]),
        "sel": sel,
        "masks": masks.reshape(128, -1),
        "w_in": np.ascontiguousarray(inp["w_in"][0]),
        "nmix": np.ascontiguousarray(inp["norm_mix"][0].reshape(8, 128).T),
        "qk": np.concatenate([inp["q_norm"][0], inp["k_norm"][0]])[None, :].astype(np.float32),
        "rgp": np.ascontiguousarray(rgp),
        "rg_w_a": np.ascontiguousarray(inp["rg_w_a"][0]),
        "rg_w_x": np.ascontiguousarray(inp["rg_w_x"][0]),
        "gsb": np.ascontiguousarray(inp["out_norm_sb"][0].reshape(4, 128).T),
        "w_out": np.ascontiguousarray(inp["w_out"][0]),
        "nffn": np.ascontiguousarray(inp["norm_ffn"]),
        "wq": np.ascontiguousarray(inp["peer_w_query"][0].reshape(D, 2048)),
        "sk": np.ascontiguousarray(inp["peer_sub_keys"][0].reshape(16, 128, 128)),
        "peer_uv": inp["peer_uv"],
    }
    return m, rows


def kernel(**inputs):
    inp = {k: np.asarray(v, dtype=np.float32) for k, v in inputs.items()}
    inp["peer_uv"] = np.ascontiguousarray(np.concatenate([inp["peer_u"][0], inp["peer_v"][0]], axis=1))
    nc = build()
    in_maps, rows_all = [], []
    for c in range(8):
        m, rows = make_core_inputs(c, inp)
        in_maps.append(m)
        rows_all.append(rows)
    res = run_bass_kernel_spmd(nc, in_maps, core_ids=list(range(8)))
    out = np.zeros((1, S_LEN, D), np.float32)
    for c in range(8):
        out[0, rows_all[c]] = res.results[c]["y_own"]
    return out
```
